# Optimizing a Trainium2 kernel written in Bass

```python
import math
import jax, jax.numpy as jnp
from jax import lax
import numpy as np

D_MODEL = 1024
BATCH = 4
SEQ = 8192
DEPTH = 2

N_EVEN = (DEPTH + 1) // 2
N_ODD = DEPTH // 2

NSA_HEADS = 8
NSA_KV_HEADS = 2
NSA_GROUP = NSA_HEADS // NSA_KV_HEADS
NSA_HEAD_DIM = 64
CMP_BLOCK = 32
CMP_STRIDE = 16
SLC_BLOCK = 64
SLC_TOP_N = 16
SLC_LOCAL = 2
WINDOW = 512
Q_BLOCK = 128
FORCED_SCORE = 1e6
GDN_HEADS = 4
GDN_HEAD_DIM = 128
GDN_CONV = 4
GDN_CHUNK = 64
POOL_SIZES = (2, 4, 8, 16)
POOL_GROUP = D_MODEL // 4
D_FF = 2816
N_EXPERTS = 8
TOP_K = 2
D_FF_EXPERT = 3584
ROPE_THETA = 10000.0
LN_EPS = 1e-5
NORM_EPS = 1e-6
DN_ALPHA = (2 * DEPTH) ** 0.25
DN_BETA = (8 * DEPTH) ** -0.25

NSA_Q = NSA_HEADS * NSA_HEAD_DIM
NSA_KV = NSA_KV_HEADS * NSA_HEAD_DIM
NSA_GATES = 3 * NSA_HEADS
GDN_W = GDN_HEADS * GDN_HEAD_DIM
IN_SPLITS = (NSA_Q,) + (NSA_KV,) * 6 + (NSA_GATES,) + (GDN_W,) * 4 + (GDN_HEADS, GDN_HEADS)
D_IN = sum(IN_SPLITS)
D_MIX = NSA_Q + GDN_W

kernel_name = "hybrid_nsa_gdn_pool_moe"

F32 = jnp.float32


def layer_norm(x, g, b):
    x32 = x.astype(F32)
    mu = jnp.mean(x32, -1, keepdims=True)
    var = jnp.mean(jnp.square(x32 - mu), -1, keepdims=True)
    return ((x32 - mu) * lax.rsqrt(var + LN_EPS) * g + b).astype(x.dtype)


def rope(x, pos):
    half = x.shape[-1] // 2
    inv = jnp.power(ROPE_THETA, -jnp.arange(half, dtype=F32) / half)
    ang = pos.astype(F32)[:, None] * inv[None, :]
    cos = jnp.cos(ang)[:, None, :]
    sin = jnp.sin(ang)[:, None, :]
    x1 = x[..., :half].astype(F32)
    x2 = x[..., half:].astype(F32)
    return jnp.concatenate([x1 * cos - x2 * sin, x2 * cos + x1 * sin], -1).astype(x.dtype)


def masked_softmax(s, mask):
    s = jnp.where(mask, s.astype(F32), -jnp.inf)
    m = jnp.max(s, -1, keepdims=True)
    m = jnp.where(jnp.isfinite(m), m, 0.0)
    e = jnp.exp(s - m)
    d = jnp.sum(e, -1, keepdims=True)
    return e / jnp.where(d > 0, d, 1.0)


def nsa_mixer(q, k_cmp, v_cmp, k_slc, v_slc, k_win, v_win, gate_logits, pe_k, w_k, pe_v, w_v):
    B, S = q.shape[:2]
    H, HKV, G, D = NSA_HEADS, NSA_KV_HEADS, NSA_GROUP, NSA_HEAD_DIM
    scale = D ** -0.5
    kv = lambda t: t.reshape(B, S, HKV, D)
    k_cmp, v_cmp, k_slc, v_slc, k_win, v_win = map(kv, (k_cmp, v_cmp, k_slc, v_slc, k_win, v_win))
    pos = jnp.arange(S, dtype=F32)
    q = rope(q.reshape(B, S, H, D), pos)
    k_slc = rope(k_slc, pos)
    k_win = rope(k_win, pos)

    n_cmp = (S - CMP_BLOCK) // CMP_STRIDE + 1
    starts = jnp.arange(n_cmp) * CMP_STRIDE
    blk_idx = starts[:, None] + jnp.arange(CMP_BLOCK)[None, :]
    cmp_end = starts + CMP_BLOCK - 1

    def compress(t, pe, w):
        tb = t[:, blk_idx] + pe[None, None, :, None, :]
        return jnp.einsum('bnlhd,lde->bnhe', tb, w)

    kc = rope(compress(k_cmp, pe_k, w_k), starts.astype(F32) + (CMP_BLOCK - 1) / 2)
    vc = compress(v_cmp, pe_v, w_v)
    kc = kc.transpose(0, 2, 1, 3)
    vc = vc.transpose(0, 2, 1, 3)

    n_slc = S // SLC_BLOCK
    n_sel = min(SLC_TOP_N, n_slc)
    slc_start = jnp.arange(n_slc) * SLC_BLOCK
    overlap = ((starts[:, None] <= slc_start[None, :] + SLC_BLOCK - 1)
               & (cmp_end[:, None] >= slc_start[None, :])).astype(F32)
    ks_blk = k_slc.reshape(B, n_slc, SLC_BLOCK, HKV, D).transpose(0, 3, 1, 2, 4)
    vs_blk = v_slc.reshape(B, n_slc, SLC_BLOCK, HKV, D).transpose(0, 3, 1, 2, 4)

    pad = ((0, 0), (0, 0), (WINDOW, 0), (0, 0))
    kw_pad = jnp.pad(k_win.transpose(0, 2, 1, 3), pad)
    vw_pad = jnp.pad(v_win.transpose(0, 2, 1, 3), pad)

    qh = q.reshape(B, S, HKV, G, D).transpose(0, 2, 3, 1, 4)
    bi = jnp.arange(B)[:, None, None, None]
    hi = jnp.arange(HKV)[None, :, None, None]
    j = jnp.arange(n_slc)

    def query_block(qb):
        s0 = qb * Q_BLOCK
        t = s0 + jnp.arange(Q_BLOCK)
        qblk = lax.dynamic_slice_in_dim(qh, s0, Q_BLOCK, axis=3)

        s_c = jnp.einsum('bhgqd,bhnd->bhgqn', qblk, kc).astype(F32) * scale
        p_c = masked_softmax(s_c, cmp_end[None, :] <= t[:, None])
        o_c = jnp.einsum('bhgqn,bhnd->bhgqd', p_c.astype(vc.dtype), vc)

        imp = jnp.einsum('bhgqn,nm->bhqm', p_c, overlap)
        cur = (t // SLC_BLOCK)[:, None]
        forced = (j[None, :] == 0) | ((j[None, :] <= cur) & (j[None, :] > cur - SLC_LOCAL))
        causal_blk = slc_start[None, :] <= t[:, None]
        score = jnp.where(causal_blk, jnp.where(forced, FORCED_SCORE, imp), -1.0)
        _, sel = lax.top_k(score, n_sel)

        kg = ks_blk[bi, hi, sel].reshape(B, HKV, Q_BLOCK, n_sel * SLC_BLOCK, D)
        vg = vs_blk[bi, hi, sel].reshape(B, HKV, Q_BLOCK, n_sel * SLC_BLOCK, D)
        kpos = (sel[..., None] * SLC_BLOCK + jnp.arange(SLC_BLOCK)).reshape(B, HKV, Q_BLOCK, n_sel * SLC_BLOCK)
        s_s = jnp.einsum('bhgqd,bhqkd->bhgqk', qblk, kg).astype(F32) * scale
        p_s = masked_softmax(s_s, (kpos <= t[:, None])[:, :, None])
        o_s = jnp.einsum('bhgqk,bhqkd->bhgqd', p_s.astype(vg.dtype), vg)

        kwb = lax.dynamic_slice_in_dim(kw_pad, s0, WINDOW + Q_BLOCK, axis=2)
        vwb = lax.dynamic_slice_in_dim(vw_pad, s0, WINDOW + Q_BLOCK, axis=2)
        wpos = s0 - WINDOW + jnp.arange(WINDOW + Q_BLOCK)
        rel = t[:, None] - wpos[None, :]
        m_w = (rel >= 0) & (rel < WINDOW) & (wpos[None, :] >= 0)
        s_w = jnp.einsum('bhgqd,bhkd->bhgqk', qblk, kwb).astype(F32) * scale
        p_w = masked_softmax(s_w, m_w)
        o_w = jnp.einsum('bhgqk,bhkd->bhgqd', p_w.astype(vwb.dtype), vwb)

        return jnp.stack([o_c.astype(F32), o_s.astype(F32), o_w.astype(F32)], axis=-2)

    o = lax.map(query_block, jnp.arange(S // Q_BLOCK))
    o = o.transpose(1, 0, 4, 2, 3, 5, 6).reshape(B, S, H, 3, D)
    gates = jax.nn.sigmoid(gate_logits.astype(F32)).reshape(B, S, H, 3)
    out = jnp.einsum('bshc,bshcd->bshd', gates, o)
    return out.reshape(B, S, H * D).astype(q.dtype)


def causal_dwconv(x, w):
    C = x.shape[-1]
    return lax.conv_general_dilated(x, w[:, None, :].astype(x.dtype), window_strides=(1,),
                                    padding=[(w.shape[0] - 1, 0)],
                                    dimension_numbers=('NWC', 'WIO', 'NWC'),
                                    feature_group_count=C)


def l2norm(x):
    return x * lax.rsqrt(jnp.sum(x * x, -1, keepdims=True) + NORM_EPS)


def gdn_mixer(q, k, v, z, b, a, conv_w, a_log, dt_bias, norm_w):
    B, S = q.shape[:2]
    H, D, C = GDN_HEADS, GDN_HEAD_DIM, GDN_CHUNK
    NC = S // C
    qkv = jax.nn.silu(causal_dwconv(jnp.concatenate([q, k, v], -1), conv_w)).astype(F32)
    q, k, v = jnp.split(qkv, 3, axis=-1)
    q = l2norm(q.reshape(B, S, H, D)) * (D ** -0.5)
    k = l2norm(k.reshape(B, S, H, D))
    v = v.reshape(B, S, H, D)
    beta = jax.nn.sigmoid(b.astype(F32))
    g = -jnp.exp(a_log.astype(F32)) * jax.nn.softplus(a.astype(F32) + dt_bias.astype(F32))

    chunk4 = lambda t: t.reshape(B, NC, C, H, D).transpose(0, 3, 1, 2, 4)
    chunk3 = lambda t: t.reshape(B, NC, C, H).transpose(0, 3, 1, 2)
    q, k, v = chunk4(q), chunk4(k), chunk4(v)
    beta = chunk3(beta)
    gc = jnp.cumsum(chunk3(g), axis=-1)

    idx = jnp.arange(C)
    tril = idx[:, None] >= idx[None, :]
    strict = idx[:, None] > idx[None, :]
    decay = jnp.exp(jnp.where(tril, gc[..., :, None] - gc[..., None, :], -jnp.inf))
    kb = k * beta[..., None]
    lmat = jnp.where(strict, jnp.einsum('bhnid,bhnjd->bhnij', kb, k) * decay, 0.0)
    amat = lmat + jnp.eye(C, dtype=F32)
    u = lax.linalg.triangular_solve(amat, v * beta[..., None], left_side=True, lower=True, unit_diagonal=True)
    w = lax.linalg.triangular_solve(amat, kb * jnp.exp(gc)[..., None], left_side=True, lower=True, unit_diagonal=True)
    intra = jnp.einsum('bhnid,bhnjd->bhnij', q, k) * decay

    def step(state, inp):
        qi, ki, ui, wi, gi, ai = inp
        v_new = ui - jnp.einsum('bhck,bhkv->bhcv', wi, state)
        o = (jnp.einsum('bhck,bhkv->bhcv', qi * jnp.exp(gi)[..., None], state)
             + jnp.einsum('bhij,bhjv->bhiv', ai, v_new))
        g_last = gi[..., -1]
        state = (state * jnp.exp(g_last)[..., None, None]
                 + jnp.einsum('bhck,bhcv->bhkv', ki * jnp.exp(g_last[..., None] - gi)[..., None], v_new))
        return state, o

    mv = lambda t: jnp.moveaxis(t, 2, 0)
    state0 = jnp.zeros((B, H, D, D), F32)
    _, o = lax.scan(step, state0, (mv(q), mv(k), mv(u), mv(w), mv(gc), mv(intra)))
    o = o.transpose(1, 0, 3, 2, 4).reshape(B, S, H, D)
    o = o * lax.rsqrt(jnp.mean(o * o, -1, keepdims=True) + NORM_EPS) * norm_w.astype(F32)
    o = o * jax.nn.silu(z.reshape(B, S, H, D).astype(F32))
    return o.reshape(B, S, H * D).astype(z.dtype)


def hybrid_attention_mixer(x, w_in, pe_k, w_k, pe_v, w_v, conv_w, a_log, dt_bias, gdn_norm, w_out):
    h = jnp.einsum('bsd,de->bse', x, w_in)
    offs = [sum(IN_SPLITS[:i + 1]) for i in range(len(IN_SPLITS) - 1)]
    (nq, kc, vc, ks, vs, kw, vw, gl, gq, gk, gv, gz, gb, ga) = jnp.split(h, offs, axis=-1)
    o_a = nsa_mixer(nq, kc, vc, ks, vs, kw, vw, gl, pe_k, w_k, pe_v, w_v)
    o_b = gdn_mixer(gq, gk, gv, gz, gb, ga, conv_w, a_log, dt_bias, gdn_norm)
    return jnp.einsum('bse,ed->bsd', jnp.concatenate([o_a, o_b], -1), w_out)


def pool_mixer(x, pool_w, pool_scale):
    B, S, _ = x.shape
    x32 = x.astype(F32)
    csum = jnp.concatenate([jnp.zeros((B, 1, D_MODEL), F32), jnp.cumsum(x32, axis=1)], axis=1)
    t = jnp.arange(S)
    outs = []
    for gi, win in enumerate(POOL_SIZES):
        sl = slice(gi * POOL_GROUP, (gi + 1) * POOL_GROUP)
        c = csum[:, :, sl]
        lo = jnp.maximum(t + 1 - win, 0)
        cnt = jnp.minimum(t + 1, win).astype(F32)[None, :, None]
        mean = (c[:, 1:] - c[:, lo]) / cnt
        outs.append(jnp.einsum('bsc,cd->bsd', mean - x32[:, :, sl], pool_w[gi].astype(F32)))
    return (jnp.concatenate(outs, -1) * pool_scale.astype(F32)).astype(x.dtype)


def swiglu(x, wg, wu, wd):
    return jnp.dot(jax.nn.silu(jnp.dot(x, wg)) * jnp.dot(x, wu), wd)


def moe_ffn(x, router_w, wg, wu, wd):
    logits = jnp.einsum('bsd,de->bse', x, router_w).astype(F32)
    probs = jax.nn.softmax(logits, -1)
    top_p, top_i = lax.top_k(probs, TOP_K)
    top_p = top_p / jnp.sum(top_p, -1, keepdims=True)
    gate = jnp.sum(jax.nn.one_hot(top_i, N_EXPERTS, dtype=F32) * top_p[..., None], axis=-2)
    y = jnp.zeros(x.shape, F32)
    for e in range(N_EXPERTS):
        y = y + gate[..., e:e + 1] * swiglu(x, wg[e], wu[e], wd[e]).astype(F32)
    return y.astype(x.dtype)


def setup_inputs(seed: int = 0) -> dict:
    key = jax.random.key(seed)
    ks = jax.random.split(key, 32)
    nrm = lambda i, shape, s: jax.random.normal(ks[i], shape, F32) * s
    gain = lambda i, shape: 1.0 + nrm(i, shape, 0.02)
    NE, NO = N_EVEN, N_ODD
    dt = jnp.exp(jax.random.uniform(ks[8], (NE, GDN_HEADS), F32, math.log(1e-3), math.log(1e-1)))
    return {
        'x': nrm(0, (BATCH, SEQ, D_MODEL), 1.0),
        'ev_w_in': nrm(1, (NE, D_MODEL, D_IN), D_MODEL ** -0.5),
        'ev_cmp_pe_k': nrm(2, (NE, CMP_BLOCK, NSA_HEAD_DIM), 0.02),
        'ev_cmp_w_k': nrm(3, (NE, CMP_BLOCK, NSA_HEAD_DIM, NSA_HEAD_DIM), (CMP_BLOCK * NSA_HEAD_DIM) ** -0.5),
        'ev_cmp_pe_v': nrm(4, (NE, CMP_BLOCK, NSA_HEAD_DIM), 0.02),
        'ev_cmp_w_v': nrm(5, (NE, CMP_BLOCK, NSA_HEAD_DIM, NSA_HEAD_DIM), (CMP_BLOCK * NSA_HEAD_DIM) ** -0.5),
        'ev_conv_w': nrm(6, (NE, GDN_CONV, 3 * GDN_W), GDN_CONV ** -0.5),
        'ev_a_log': jnp.log(jax.random.uniform(ks[7], (NE, GDN_HEADS), F32, 1.0, 16.0)),
        'ev_dt_bias': dt + jnp.log(-jnp.expm1(-dt)),
        'ev_gdn_norm': gain(9, (NE, GDN_HEAD_DIM)),
        'ev_w_out': nrm(10, (NE, D_MIX, D_MODEL), DN_BETA * D_MIX ** -0.5),
        'ev_ln1_g': gain(11, (NE, D_MODEL)),
        'ev_ln1_b': nrm(12, (NE, D_MODEL), 0.02),
        'ev_ffn_wg': nrm(13, (NE, D_MODEL, D_FF), D_MODEL ** -0.5),
        'ev_ffn_wu': nrm(14, (NE, D_MODEL, D_FF), D_MODEL ** -0.5),
        'ev_ffn_wd': nrm(15, (NE, D_FF, D_MODEL), DN_BETA * D_FF ** -0.5),
        'ev_ln2_g': gain(16, (NE, D_MODEL)),
        'ev_ln2_b': nrm(17, (NE, D_MODEL), 0.02),
        'od_pool_w': nrm(18, (NO, len(POOL_SIZES), POOL_GROUP, POOL_GROUP), DN_BETA * POOL_GROUP ** -0.5),
        'od_pool_scale': gain(19, (NO, D_MODEL)),
        'od_ln1_g': gain(20, (NO, D_MODEL)),
        'od_ln1_b': nrm(21, (NO, D_MODEL), 0.02),
        'od_router_w': nrm(22, (NO, D_MODEL, N_EXPERTS), D_MODEL ** -0.5),
        'od_exp_wg': nrm(23, (NO, N_EXPERTS, D_MODEL, D_FF_EXPERT), D_MODEL ** -0.5),
        'od_exp_wu': nrm(24, (NO, N_EXPERTS, D_MODEL, D_FF_EXPERT), D_MODEL ** -0.5),
        'od_exp_wd': nrm(25, (NO, N_EXPERTS, D_FF_EXPERT, D_MODEL), DN_BETA * D_FF_EXPERT ** -0.5),
        'od_ln2_g': gain(26, (NO, D_MODEL)),
        'od_ln2_b': nrm(27, (NO, D_MODEL), 0.02),
    }


def reference(x, ev_w_in, ev_cmp_pe_k, ev_cmp_w_k, ev_cmp_pe_v, ev_cmp_w_v, ev_conv_w, ev_a_log,
              ev_dt_bias, ev_gdn_norm, ev_w_out, ev_ln1_g, ev_ln1_b, ev_ffn_wg, ev_ffn_wu, ev_ffn_wd,
              ev_ln2_g, ev_ln2_b, od_pool_w, od_pool_scale, od_ln1_g, od_ln1_b, od_router_w,
              od_exp_wg, od_exp_wu, od_exp_wd, od_ln2_g, od_ln2_b):
    for layer in range(DEPTH):
        i = layer // 2
        if layer % 2 == 0:
            mix = hybrid_attention_mixer(x, ev_w_in[i], ev_cmp_pe_k[i], ev_cmp_w_k[i], ev_cmp_pe_v[i],
                                         ev_cmp_w_v[i], ev_conv_w[i], ev_a_log[i], ev_dt_bias[i],
                                         ev_gdn_norm[i], ev_w_out[i])
            x = layer_norm(DN_ALPHA * x + mix, ev_ln1_g[i], ev_ln1_b[i])
            ffn = swiglu(x, ev_ffn_wg[i], ev_ffn_wu[i], ev_ffn_wd[i])
            x = layer_norm(DN_ALPHA * x + ffn, ev_ln2_g[i], ev_ln2_b[i])
        else:
            mix = pool_mixer(x, od_pool_w[i], od_pool_scale[i])
            x = layer_norm(DN_ALPHA * x + mix, od_ln1_g[i], od_ln1_b[i])
            ffn = moe_ffn(x, od_router_w[i], od_exp_wg[i], od_exp_wu[i], od_exp_wd[i])
            x = layer_norm(DN_ALPHA * x + ffn, od_ln2_g[i], od_ln2_b[i])
    return x
```

```python
import os
import numpy as np
import concourse.bass as bass
import concourse.mybir as mybir
from concourse.bass_utils import run_bass_kernel_spmd

F32 = mybir.dt.float32
BF16 = mybir.dt.bfloat16
AF = mybir.ActivationFunctionType
ALU = mybir.AluOpType
AX = mybir.AxisListType

NCORES = 8
D = 1024
ALPHA = float((2 * 2) ** 0.25)
LN_EPS = 1e-5


class Buf:
    __slots__ = ("name", "lw", "rs")

    def __init__(self, name):
        self.name = name
        self.lw = None
        self.rs = []


class Sched:
    NDMA = 24

    def __init__(self, nc, shared=None):
        self.nc = nc
        self.ops = []
        self.shared = shared

    def add(self, eng, fn, reads=(), writes=(), dma=False):
        i = len(self.ops)
        deps = set()
        for b in reads:
            if b.lw is not None:
                deps.add(b.lw)
        for b in writes:
            if b.lw is not None:
                deps.add(b.lw)
            deps.update(b.rs)
        for b in reads:
            b.rs.append(i)
        for b in writes:
            b.lw = i
            b.rs = []
        self.ops.append([eng, fn, deps, dma])
        return i

    def pe(self, fn, reads=(), writes=()):
        return self.add("pe", fn, reads, writes)

    def act(self, fn, reads=(), writes=()):
        return self.add("act", fn, reads, writes)

    def dve(self, fn, reads=(), writes=()):
        return self.add("dve", fn, reads, writes)

    def pool(self, fn, reads=(), writes=()):
        return self.add("pool", fn, reads, writes)

    def dma(self, fn, reads=(), writes=(), q="sp"):
        return self.add(q, fn, reads, writes, dma=True)

    def emit(self, stack):
        nc = self.nc
        ops = self.ops
        n = len(ops)
        engs = ["pe", "act", "dve", "pool", "sp"]
        has_dep = [False] * n
        red = []
        pos = [0] * n
        _c = {}
        for i, op in enumerate(ops):
            _c[op[0]] = _c.get(op[0], 0) + 1
            pos[i] = _c[op[0]]
        for i, (eng, fn, deps, dma) in enumerate(ops):
            latest = {}
            dl = []
            for j in deps:
                if ops[j][3]:
                    dl.append(j)
                else:
                    e = ops[j][0]
                    if e not in latest or latest[e] < j:
                        latest[e] = j
            for e, j in latest.items():
                if e == eng and not dma:
                    if eng == "pe":
                        continue
                    if eng in ("act", "dve") and pos[i] - pos[j] >= 2:
                        continue
                dl.append(j)
            red.append(dl)
            for j in dl:
                has_dep[j] = True
        last = {}
        for i, op in enumerate(ops):
            last[op[0]] = i
        for e, i in last.items():
            has_dep[i] = True
        sh = self.shared
        if sh is None:
            sh = {}
        if "sem_eng" not in sh:
            stack = sh.get("stack", stack)
            sh["sem_eng"] = {e: stack.enter_context(nc.semaphore("s_" + e)) for e in engs}
            sh["sem_dma"] = [stack.enter_context(nc.semaphore("s_dma%d" % k)) for k in range(self.NDMA)]
            sh["cnt"] = {e: 0 for e in engs}
            sh["kd"] = 0
            sh["dma_final"] = {}
        sem_eng, sem_dma = sh["sem_eng"], sh["sem_dma"]
        sig = [None] * n
        prevw = [None] * n
        cnt = dict(sh["cnt"])
        start_cnt = dict(sh["cnt"])
        kd = sh["kd"]
        for i, (eng, fn, deps, dma) in enumerate(ops):
            if dma:
                s = kd % self.NDMA
                g = kd // self.NDMA + 1
                sig[i] = (("d", s), 16 * g)
                if g > 1:
                    prevw[i] = (("d", s), 16 * (g - 1))
                kd += 1
            elif has_dep[i]:
                cnt[eng] += 1
                sig[i] = (("e", eng), cnt[eng])
        dma_final = dict(sh["dma_final"])
        start_dma = dict(sh["dma_final"])
        for i in range(n):
            if ops[i][3]:
                dma_final[sig[i][0]] = sig[i][1]
        sh["cnt"] = dict(cnt)
        sh["kd"] = kd
        sh["dma_final"] = dict(dma_final)

        def semh(key):
            return sem_dma[key[1]] if key[0] == "d" else sem_eng[key[1]]

        per_eng = {e: [] for e in engs}
        for i, op in enumerate(ops):
            per_eng[op[0]].append(i)

        def run_engine(ename, e):
            waited = {("e", en): v for en, v in start_cnt.items()}
            waited.update(start_dma)
            for i in per_eng[ename]:
                _, fn, _, dma = ops[i]
                need = {}
                for j in red[i]:
                    k, v = sig[j]
                    if need.get(k, 0) < v:
                        need[k] = v
                if prevw[i] is not None:
                    k, v = prevw[i]
                    if need.get(k, 0) < v:
                        need[k] = v
                for k, v in need.items():
                    if waited.get(k, 0) < v:
                        e.wait_ge(semh(k), v)
                        waited[k] = v
                ins = fn(e)
                if sig[i] is not None:
                    k, v = sig[i]
                    ins.then_inc(semh(k), 16 if dma else 1)
            for k, v in dma_final.items():
                if waited.get(k, 0) < v:
                    e.wait_ge(semh(k), v)
            for en in engs:
                if en != ename and cnt[en] > waited.get(("e", en), 0):
                    e.wait_ge(sem_eng[en], cnt[en])

        with nc.Block() as block:
            @block.tensor
            def _(e):
                run_engine("pe", e)

            @block.scalar
            def _(e):
                run_engine("act", e)

            @block.vector
            def _(e):
                run_engine("dve", e)

            @block.gpsimd
            def _(e):
                run_engine("pool", e)

            @block.sync
            def _(e):
                run_engine("sp", e)


class Ctx:
    def __init__(self, nc, stack, shared=None, pfx="", io=None):
        self.nc = nc
        self.stack = stack
        self.S = Sched(nc, shared)
        self.n = 0
        self.pfx = pfx
        self.io = io or {}

    def sb(self, shape, dt, name=None):
        self.n += 1
        name = "sb_" + self.pfx + (name or ("t%d" % self.n))
        t = self.stack.enter_context(self.nc.sbuf_tensor(name, list(shape), dt))
        return t, Buf(name)

    def ps(self, shape, dt, name=None):
        self.n += 1
        name = "ps_" + self.pfx + (name or ("p%d" % self.n))
        t = self.stack.enter_context(self.nc.psum_tensor(name, list(shape), dt))
        return t, Buf(name)

    def din(self, name, shape, dt=F32):
        if name in self.io:
            return self.io[name], Buf(name)
        return self.nc.dram_tensor(self.pfx + name, list(shape), dt, kind="ExternalInput").ap(), Buf(name)

    def dout(self, name, shape, dt=F32):
        if name in self.io:
            return self.io[name], Buf(name)
        return self.nc.dram_tensor(self.pfx + name, list(shape), dt, kind="ExternalOutput").ap(), Buf(name)

    def dscr(self, name, shape, dt=F32):
        return self.nc.dram_tensor(name, list(shape), dt, kind="Internal").ap(), Buf(name)


NT_TAIL = 33
DFF = 2816
DFE = 3584
NEXP = 8


def ln_tile(C, src, srcb, dst, dstb, gt, gtb, bt, btb, tmp):
    S = C.S
    st6, st6b, mv, mvb, rstd, rstdb = tmp
    for h in range(2):
        S.dve(lambda e, h=h: e.bn_stats(out=st6[:, h, :], in_=src[:, h * 512:(h + 1) * 512]), [srcb], [st6b])
    S.dve(lambda e: e.bn_aggr(out=mv[:], in_=st6[:].rearrange("p a b -> p (a b)")), [st6b], [mvb])
    S.act(lambda e: e.activation(out=rstd[:], in_=mv[:, 1:2], func=AF.Ln, bias=LN_EPS, scale=1.0), [mvb], [rstdb])
    S.act(lambda e: e.activation(out=rstd[:], in_=rstd[:], func=AF.Exp, scale=-0.5), [rstdb], [rstdb])
    S.dve(lambda e: e.tensor_scalar(out=dst, in0=src, scalar1=mv[:, 0:1], scalar2=rstd[:, 0:1],
                                    op0=ALU.subtract, op1=ALU.mult), [srcb, mvb, rstdb], [dstb])
    S.pool(lambda e: e.tensor_tensor(out=dst, in0=dst, in1=gt[:], op=ALU.mult), [dstb, gtb], [dstb])
    S.pool(lambda e: e.tensor_tensor(out=dst, in0=dst, in1=bt[:], op=ALU.add), [dstb, btb], [dstb])


def build_tail(nc, stack, stop=None, shared=None, pfx="", io=None, dyn=False):
    C = Ctx(nc, stack, shared, pfx, io)
    S = C.S
    NT = NT_TAIL
    if dyn:
        xpad, xpadb = C.din("xpad", [SEQ + 128, D])
        shmix, shmixb = C.din("shmix", [2, 2, SEQ + 128, 256])
        xin, xinb = C.dscr(pfx + "xts", [NT * 128, D])
        mxs, omixb = C.dscr(pfx + "mxs", [4, NT * 128, 256])
        omix = None

        def dyn_rows(ap):
            return ap[bass.ds((nc.partition_id() % 2) * 4096, NT * 128), :]
        S.dma(lambda e: e.dma_start(out=xin, in_=dyn_rows(xpad)), [xpadb], [xinb], q="act")
        for k in range(4):
            S.dma(lambda e, k=k: e.dma_start(out=mxs[k], in_=dyn_rows(shmix[k % 2, k // 2])), [shmixb], [omixb], q=("act" if k == 0 else "pool"))
    else:
        xin, xinb = C.din("xin", [NT * 128, D])
        omix, omixb = C.din("omix", [NT * 128, D])
    wout_d, woutb = C.din("w_out", [D, D])
    lnp_d, lnpb = C.din("lnp", [9, D])
    wg_d, wgb = C.din("ffn_wg", [D, DFF])
    wu_d, wub = C.din("ffn_wu", [D, DFF])
    wd_d, wdb = C.din("ffn_wd", [DFF, D])
    pw_d, pwb = C.din("pool_w", [4, 256, 256])
    rw_d, rwb = C.din("router_w", [D, NEXP])
    eg_d, egb = C.din("exp_wg", [NEXP, D, DFE])
    eu_d, eub = C.din("exp_wu", [NEXP, D, DFE])
    ed_d, edb = C.din("exp_wd", [NEXP, DFE, D])
    ap_d, apb = C.din("apool", [4, 4, 128, 128])
    id_d, idb = C.din("ident", [128, 128])
    x2s, x2sb = C.dout("x2s", [NT * 128, D])
    out_d, outb = C.dout("out", [(NT - 1) * 128, D])

    ident, identb = C.sb([128, 128], F32, "ident")
    S.dma(lambda e: e.dma_start(out=ident[:], in_=id_d), [idb], [identb])
    lnpt = [C.sb([128, D], F32, "lnp%d" % i) for i in range(5)]
    lnp = {}

    def load_lnp(rows):
        for j, i in enumerate(rows):
            t, b = lnpt[j]
            S.dma(lambda e, t=t, i=i: e.dma_start(out=t[:], in_=lnp_d[i:i + 1, :].to_broadcast([128, D])), [lnpb], [b])
            lnp[i] = (t, b)
    load_lnp([0, 1, 2, 3])
    wout, woutsb = C.sb([128, 8, D], BF16, "wout")
    S.dma(lambda e: e.dma_start(out=wout[:], in_=wout_d.rearrange("(k p) n -> p k n", p=128)), [woutb], [woutsb], q="pool")
    apool, apoolb = C.sb([128, 16, 128], F32, "apool")
    S.dma(lambda e: e.dma_start(out=apool[:], in_=ap_d.rearrange("a w p t -> p (a w) t")), [apb], [apoolb])
    poolw, poolwb = C.sb([128, 8, 256], F32, "poolw")
    S.dma(lambda e: e.dma_start(out=poolw[:], in_=pw_d.rearrange("g (k p) d -> p (g k) d", p=128)), [pwb], [poolwb])
    rw, rwsb = C.sb([128, 8, NEXP], F32, "rw")
    S.dma(lambda e: e.dma_start(out=rw[:], in_=rw_d.rearrange("(k p) n -> p k n", p=128)), [rwb], [rwsb])

    NP = 11
    yacc, yaccb = [], []
    for t in range(NP):
        a, b = C.sb([128, D], F32, "yacc%d" % t)
        yacc.append(a)
        yaccb.append(b)
    xT, xTb = C.sb([128, 8, NP * 128], BF16, "xT")
    gates, gatesb = C.sb([128, NP, NEXP], F32, "gates")
    hT = [C.sb([128, 4, NP * 128], BF16, "hT%d" % i) for i in range(1)]
    wgs = [C.sb([128, 8, 512], BF16, "wgs%d" % i) for i in range(2)]
    wus = [C.sb([128, 8, 512], BF16, "wus%d" % i) for i in range(2)]
    wds = [C.sb([128, 4, D], BF16, "wds%d" % i) for i in range(2)]
    sgt = [C.sb([128, 512], BF16, "sg%d" % i) for i in range(2)]
    NSL = 1
    ta = [C.sb([128, D], F32, "ta%d" % i) for i in range(NSL)]
    tb = [C.sb([128, D], F32, "tb%d" % i) for i in range(NSL)]
    tc_ = [C.sb([128, D], F32, "tc%d" % i) for i in range(NSL)]
    tTb = [C.sb([128, 8, 128], BF16, "tTb%d" % i) for i in range(NSL)]
    tTf = [C.sb([128, 8, 128], F32, "tTf%d" % i) for i in range(NSL)]
    lntmp = []
    for i in range(NSL):
        a = C.sb([128, 2, 6], F32)
        b = C.sb([128, 2], F32)
        c = C.sb([128, 1], F32)
        lntmp.append((a[0], a[1], b[0], b[1], c[0], c[1]))
    sm = [dict(mx=C.sb([128, 8], F32), e=C.sb([128, 8], F32), m=C.sb([128, 8], F32), d=C.sb([128, 1], F32),
               nb=C.sb([128, 1], F32), lg=C.sb([128, 8], F32)) for i in range(NSL)]
    psA = [C.ps([128, 512], F32, "psA%d" % i) for i in range(2)]
    psB = [C.ps([128, 512], F32, "psB%d" % i) for i in range(2)]
    psD = [C.ps([128, 512], F32, "psD%d" % i) for i in range(4)]
    cnt = {"t": 0, "g": 0, "ab": 0, "d": 0}

    def transpose_to(src, srcb, dst_bf=None, dst_bfb=None, dst_f=None, dst_fb=None, dst_bf_ap=None):
        for half in range(2):
            p, pb = psD[cnt["d"] % 4]
            cnt["d"] += 1
            for k in range(4):
                kk = half * 4 + k
                S.pe(lambda e, p=p, k=k, kk=kk: e.transpose(out=p[:, k * 128:(k + 1) * 128], in_=src[:, kk * 128:(kk + 1) * 128],
                                                           identity=ident[:]), [srcb, identb], [pb])
            if dst_bf_ap is not None:
                S.act(lambda e, p=p, half=half: e.copy(out=dst_bf_ap(half), in_=p[:].rearrange("p (a b) -> p a b", a=4)), [pb], [dst_bfb, pb])
            if dst_f is not None:
                S.dve(lambda e, p=p, half=half: e.tensor_copy(out=dst_f[:, half * 4:(half + 1) * 4, :],
                                                              in_=p[:].rearrange("p (a b) -> p a b", a=4)), [pb], [dst_fb])

    def swiglu_pass(ntl, wgd, wgdb, wud, wudb, wdd, wddb, nfc, ne, use_gates):
        ntok = ntl * 128
        blocks = [(s, min(512, ntok - s)) for s in range(0, ntok, 512)]
        for ex in range(ne):
            for c0 in range(0, nfc, 4):
                gc = min(4, nfc - c0)
                slot = cnt["g"] % 2
                cnt["g"] += 1
                wg_t, wg_b = wgs[slot]
                wu_t, wu_b = wus[slot]
                wd_t, wd_b = wds[slot]
                h_t, h_b = hT[0]
                if ne > 1:
                    sg_, su_, sd_ = wgd[ex], wud[ex], wdd[ex]
                else:
                    sg_, su_, sd_ = wgd, wud, wdd
                S.dma(lambda e, wg_t=wg_t, sg_=sg_, c0=c0, gc=gc: e.dma_start(
                    out=wg_t[:, :, 0:gc * 128], in_=sg_[:, c0 * 128:(c0 + gc) * 128].rearrange("(k p) f -> p k f", p=128)),
                    [wgdb], [wg_b], q="pool")
                S.dma(lambda e, wu_t=wu_t, su_=su_, c0=c0, gc=gc: e.dma_start(
                    out=wu_t[:, :, 0:gc * 128], in_=su_[:, c0 * 128:(c0 + gc) * 128].rearrange("(k p) f -> p k f", p=128)),
                    [wudb], [wu_b], q="pool")
                S.dma(lambda e, wd_t=wd_t, sd_=sd_, c0=c0, gc=gc: e.dma_start(
                    out=wd_t[:, 0:gc, :], in_=sd_[c0 * 128:(c0 + gc) * 128, :].rearrange("(c p) d -> p c d", p=128)),
                    [wddb], [wd_b], q="pool")
                for (s0, bw) in blocks:
                    for c in range(gc):
                        ab = cnt["ab"] % 2
                        cnt["ab"] += 1
                        pa, pab = psA[ab]
                        pb_, pbb = psB[ab]
                        sgx, sgb = sgt[ab]
                        for k in range(8):
                            S.pe(lambda e, pa=pa, k=k, c=c, s0=s0, bw=bw, wg_t=wg_t: e.matmul(
                                pa[:, 0:bw], lhsT=wg_t[:, k, c * 128:(c + 1) * 128], rhs=xT[:, k, s0:s0 + bw],
                                start=(k == 0), stop=(k == 7)), [wg_b, xTb], [pab])
                        for k in range(8):
                            S.pe(lambda e, pb_=pb_, k=k, c=c, s0=s0, bw=bw, wu_t=wu_t: e.matmul(
                                pb_[:, 0:bw], lhsT=wu_t[:, k, c * 128:(c + 1) * 128], rhs=xT[:, k, s0:s0 + bw],
                                start=(k == 0), stop=(k == 7)), [wu_b, xTb], [pbb])
                        S.act(lambda e, pa=pa, sgx=sgx, bw=bw: e.activation(out=sgx[:, 0:bw], in_=pa[:, 0:bw], func=AF.Silu),
                              [pab], [sgb])
                        S.dve(lambda e, sgx=sgx, pb_=pb_, h_t=h_t, c=c, s0=s0, bw=bw: e.tensor_tensor(
                            out=h_t[:, c, s0:s0 + bw], in0=sgx[:, 0:bw], in1=pb_[:, 0:bw], op=ALU.mult), [sgb, pbb], [h_b])
                for t in range(ntl):
                    for half in range(2):
                        p, pb2 = psD[cnt["d"] % 4]
                        cnt["d"] += 1
                        for c in range(gc):
                            S.pe(lambda e, p=p, c=c, t=t, half=half, h_t=h_t, wd_t=wd_t, gc=gc: e.matmul(
                                p[:], lhsT=h_t[:, c, t * 128:(t + 1) * 128], rhs=wd_t[:, c, half * 512:(half + 1) * 512],
                                start=(c == 0), stop=(c == gc - 1)), [h_b, wd_b], [pb2])
                        ya = yacc[t]
                        if use_gates:
                            S.dve(lambda e, p=p, ya=ya, t=t, ex=ex, half=half: e.scalar_tensor_tensor(
                                out=ya[:, half * 512:(half + 1) * 512], in0=p[:], scalar=gates[:, t, ex:ex + 1],
                                in1=ya[:, half * 512:(half + 1) * 512], op0=ALU.mult, op1=ALU.add),
                                [pb2, gatesb, yaccb[t]], [yaccb[t]])
                        else:
                            S.dve(lambda e, p=p, ya=ya, half=half: e.tensor_tensor(
                                out=ya[:, half * 512:(half + 1) * 512], in0=p[:], in1=ya[:, half * 512:(half + 1) * 512],
                                op=ALU.add), [pb2, yaccb[t]], [yaccb[t]])

    partsA = [(s, min(NP, NT - s)) for s in range(0, NT, NP)]
    for (t0, ntl) in partsA:
        for tl in range(ntl):
            ti = t0 + tl
            sl = cnt["t"] % NSL
            cnt["t"] += 1
            (om, omb), (xt_, xtb_), (xn, xnb) = ta[sl], tb[sl], tc_[sl]
            oT, oTb = tTb[sl]
            if dyn:
                for k in range(4):
                    S.dma(lambda e, om=om, ti=ti, k=k: e.dma_start(out=om[:, k * 256:(k + 1) * 256], in_=mxs[k, ti * 128:(ti + 1) * 128, :]), [omixb], [omb])
            else:
                S.dma(lambda e, om=om, ti=ti: e.dma_start(out=om[:], in_=omix[ti * 128:(ti + 1) * 128, :]), [omixb], [omb])
            S.dma(lambda e, xt_=xt_, ti=ti: e.dma_start(out=xt_[:], in_=xin[ti * 128:(ti + 1) * 128, :]), [xinb], [xtb_])
            transpose_to(om, omb, dst_bfb=oTb, dst_bf_ap=lambda half, oT=oT: oT[:, half * 4:(half + 1) * 4, :])
            for half in range(2):
                p, pb2 = psD[cnt["d"] % 4]
                cnt["d"] += 1
                for k in range(8):
                    S.pe(lambda e, p=p, k=k, half=half, oT=oT: e.matmul(p[:], lhsT=oT[:, k, :], rhs=wout[:, k, half * 512:(half + 1) * 512],
                                                                        start=(k == 0), stop=(k == 7)), [oTb, woutsb], [pb2])
                S.dve(lambda e, p=p, half=half, xt_=xt_: e.scalar_tensor_tensor(
                    out=xt_[:, half * 512:(half + 1) * 512], in0=xt_[:, half * 512:(half + 1) * 512], scalar=ALPHA, in1=p[:],
                    op0=ALU.mult, op1=ALU.add), [pb2, xtb_], [xtb_])
            ln_tile(C, xt_[:], xtb_, xn[:], xnb, lnp[0][0], lnp[0][1], lnp[1][0], lnp[1][1], lntmp[sl])
            S.act(lambda e, xn=xn, tl=tl: e.mul(out=yacc[tl][:], in_=xn[:], mul=ALPHA), [xnb], [yaccb[tl]])
            transpose_to(xn, xnb, dst_bfb=xTb, dst_bf_ap=lambda half, tl=tl: xT[:, half * 4:(half + 1) * 4, tl * 128:(tl + 1) * 128])
        if stop == "A1":
            S.emit(stack)
            return
        swiglu_pass(ntl, wg_d, wgb, wu_d, wub, wd_d, wdb, DFF // 128, 1, False)
        if stop == "A2":
            S.emit(stack)
            return
        for tl in range(ntl):
            ti = t0 + tl
            sl = cnt["t"] % NSL
            cnt["t"] += 1
            xn, xnb = tc_[sl]
            ln_tile(C, yacc[tl][:], yaccb[tl], xn[:], xnb, lnp[2][0], lnp[2][1], lnp[3][0], lnp[3][1], lntmp[sl])
            S.dma(lambda e, xn=xn, ti=ti: e.dma_start(out=x2s[ti * 128:(ti + 1) * 128, :], in_=xn[:]), [xnb], [x2sb])

    if stop == "A":
        S.emit(stack)
        return
    partsB = [(s, min(NP, NT - s)) for s in range(1, NT, NP)]
    load_lnp([4, 5, 6, 7, 8])
    pmT = [C.sb([128, 8, 128], F32, "pmT%d" % i) for i in range(NSL)]
    for (t0, ntl) in partsB:
        for tl in range(ntl):
            ti = t0 + tl
            sl = cnt["t"] % NSL
            cnt["t"] += 1
            (xc, xcb), (xp, xpb), (xn, xnb) = ta[sl], tb[sl], tc_[sl]
            pm, pmb = pmT[sl]
            xf, xfb = tTf[sl]
            S.dma(lambda e, xc=xc, ti=ti: e.dma_start(out=xc[:], in_=x2s[ti * 128:(ti + 1) * 128, :]), [x2sb], [xcb])
            S.dma(lambda e, xp=xp, ti=ti: e.dma_start(out=xp[:], in_=x2s[(ti - 1) * 128:ti * 128, :]), [x2sb], [xpb])
            first = (ti == 1)
            kp, kc = (0, 1) if first else (2, 3)
            for half in range(2):
                p, pb2 = psD[cnt["d"] % 4]
                cnt["d"] += 1
                for k in range(4):
                    kk = half * 4 + k
                    gi = kk // 2
                    S.pe(lambda e, p=p, k=k, kk=kk, gi=gi, xp=xp, kp=kp: e.matmul(
                        p[:, k * 128:(k + 1) * 128], lhsT=xp[:, kk * 128:(kk + 1) * 128], rhs=apool[:, kp * 4 + gi, :],
                        start=True, stop=False), [xpb, apoolb], [pb2])
                    S.pe(lambda e, p=p, k=k, kk=kk, gi=gi, xc=xc, kc=kc: e.matmul(
                        p[:, k * 128:(k + 1) * 128], lhsT=xc[:, kk * 128:(kk + 1) * 128], rhs=apool[:, kc * 4 + gi, :],
                        start=False, stop=True), [xcb, apoolb], [pb2])
                S.act(lambda e, p=p, half=half, pm=pm: e.copy(out=pm[:, half * 4:(half + 1) * 4, :],
                                                             in_=p[:].rearrange("p (a b) -> p a b", a=4)), [pb2], [pmb])
            for half in range(2):
                p, pb2 = psD[cnt["d"] % 4]
                cnt["d"] += 1
                for g2 in range(2):
                    gi = half * 2 + g2
                    for k in range(2):
                        S.pe(lambda e, p=p, g2=g2, gi=gi, k=k, pm=pm: e.matmul(
                            p[:, g2 * 256:(g2 + 1) * 256], lhsT=pm[:, gi * 2 + k, :], rhs=poolw[:, gi * 2 + k, :],
                            start=(k == 0), stop=(k == 1)), [pmb, poolwb], [pb2])
                S.dve(lambda e, p=p, half=half, xp=xp: e.tensor_tensor(
                    out=xp[:, half * 512:(half + 1) * 512], in0=p[:], in1=lnp[4][0][:, half * 512:(half + 1) * 512], op=ALU.mult),
                    [pb2, lnp[4][1]], [xpb])
            S.dve(lambda e, xc=xc, xp=xp: e.scalar_tensor_tensor(out=xc[:], in0=xc[:], scalar=ALPHA, in1=xp[:],
                                                                 op0=ALU.mult, op1=ALU.add), [xcb, xpb], [xcb])
            ln_tile(C, xc[:], xcb, xn[:], xnb, lnp[5][0], lnp[5][1], lnp[6][0], lnp[6][1], lntmp[sl])
            S.act(lambda e, xn=xn, tl=tl: e.mul(out=yacc[tl][:], in_=xn[:], mul=ALPHA), [xnb], [yaccb[tl]])
            transpose_to(xn, xnb, dst_bfb=xTb, dst_bf_ap=lambda half, tl=tl: xT[:, half * 4:(half + 1) * 4, tl * 128:(tl + 1) * 128],
                         dst_f=xf, dst_fb=xfb)
            p, pb2 = psD[cnt["d"] % 4]
            cnt["d"] += 1
            for k in range(8):
                S.pe(lambda e, p=p, k=k, xf=xf: e.matmul(p[:, 0:NEXP], lhsT=xf[:, k, :], rhs=rw[:, k, :], start=(k == 0), stop=(k == 7)),
                     [xfb, rwsb], [pb2])
            q = sm[sl]
            lg, lgb = q["lg"]
            mx, mxb = q["mx"]
            ee, eeb = q["e"]
            mm, mmb = q["m"]
            dd, ddb = q["d"]
            nb, nbb = q["nb"]
            S.dve(lambda e, p=p, lg=lg: e.tensor_copy(out=lg[:], in_=p[:, 0:NEXP]), [pb2], [lgb])
            S.dve(lambda e, lg=lg, mx=mx: e.max(out=mx[:], in_=lg[:]), [lgb], [mxb])
            S.dve(lambda e, nb=nb, mx=mx: e.tensor_scalar(out=nb[:], in0=mx[:, 0:1], scalar1=-1.0, scalar2=None, op0=ALU.mult), [mxb], [nbb])
            S.act(lambda e, ee=ee, lg=lg, nb=nb: e.activation(out=ee[:], in_=lg[:], func=AF.Exp, bias=nb[:, 0:1], scale=1.0), [lgb, nbb], [eeb])
            S.act(lambda e, dd=dd, mx=mx, nb=nb: e.activation(out=dd[:], in_=mx[:, 1:2], func=AF.Exp, bias=nb[:, 0:1], scale=1.0), [mxb, nbb], [ddb])
            S.dve(lambda e, dd=dd: e.tensor_scalar(out=dd[:], in0=dd[:], scalar1=1.0, scalar2=None, op0=ALU.add), [ddb], [ddb])
            S.dve(lambda e, dd=dd: e.reciprocal(out=dd[:], in_=dd[:]), [ddb], [ddb])
            S.dve(lambda e, mm=mm, lg=lg, mx=mx: e.tensor_scalar(out=mm[:], in0=lg[:], scalar1=mx[:, 1:2], scalar2=None, op0=ALU.is_ge), [lgb, mxb], [mmb])
            S.dve(lambda e, mm=mm, ee=ee: e.tensor_tensor(out=mm[:], in0=mm[:], in1=ee[:], op=ALU.mult), [mmb, eeb], [mmb])
            S.dve(lambda e, mm=mm, dd=dd, tl=tl: e.tensor_scalar(out=gates[:, tl, :], in0=mm[:], scalar1=dd[:, 0:1], scalar2=None, op0=ALU.mult),
                  [mmb, ddb], [gatesb])
        if stop == "B1":
            S.emit(stack)
            return
        swiglu_pass(ntl, eg_d, egb, eu_d, eub, ed_d, edb, DFE // 128, NEXP, True)
        for tl in range(ntl):
            ti = t0 + tl
            sl = cnt["t"] % NSL
            cnt["t"] += 1
            xn, xnb = tc_[sl]
            ln_tile(C, yacc[tl][:], yaccb[tl], xn[:], xnb, lnp[7][0], lnp[7][1], lnp[8][0], lnp[8][1], lntmp[sl])
            S.dma(lambda e, xn=xn, ti=ti: e.dma_start(out=out_d[(ti - 1) * 128:ti * 128, :], in_=xn[:]), [xnb], [outb])
    S.emit(stack)


def pool_consts():
    A = np.zeros((4, 4, 128, 128), np.float32)
    for wi, w in enumerate((2, 4, 8, 16)):
        for t in range(128):
            for j in range(w):
                tp = t - j
                if tp >= 0:
                    A[3, wi, tp, t] += 1.0 / w
                else:
                    A[2, wi, 128 + tp, t] += 1.0 / w
            A[3, wi, t, t] -= 1.0
            c = min(t + 1, w)
            for j in range(c):
                A[1, wi, t - j, t] += 1.0 / c
            A[1, wi, t, t] -= 1.0
    return A


SEQ = 8192
GD = 128
CH = 64


class PsumRing:
    def __init__(self, C, n=8):
        self.t = [C.ps([128, 512], F32, "bank%d" % i) for i in range(n)]
        self.i = 0

    def __call__(self):
        r = self.t[self.i % len(self.t)]
        self.i += 1
        return r


def load_xT_block(C, PS, xb_d, xbb, t0, ident, identb, xtile, xT, xTb):
    S = C.S
    for j in range(4):
        xt_, xtb_ = xtile[j % len(xtile)]
        S.dma(lambda e, xt_=xt_, j=j: e.dma_start(out=xt_[:], in_=xb_d[t0 + j * 128:t0 + (j + 1) * 128, :]), [xbb], [xtb_])
        for half in range(2):
            p, pb = PS()
            for k in range(4):
                kk = half * 4 + k
                S.pe(lambda e, p=p, k=k, kk=kk, xt_=xt_: e.transpose(out=p[:, k * 128:(k + 1) * 128], in_=xt_[:, kk * 128:(kk + 1) * 128],
                                                                     identity=ident[:]), [xtb_, identb], [pb])
            S.act(lambda e, p=p, half=half, j=j: e.copy(out=xT[:, half * 4:(half + 1) * 4, j * 128:(j + 1) * 128],
                                                        in_=p[:].rearrange("p (a b) -> p a b", a=4)), [pb], [xTb])


def build_gdn(nc, stack, nblk=SEQ // 512, shared=None, pfx="", io=None, ocol=0, NH=2):
    C = Ctx(nc, stack, shared, pfx, io)
    S = C.S
    PS = PsumRing(C)
    NC3 = 3 * NH
    xb_d, xbb = C.din("xb", [SEQ, D])
    wqkv_d, wqkvb = C.din("wqkv", [D, NC3 * 128])
    wz_d, wzb = C.din("wz", [D, NH * 128])
    wbg_d, wbgb = C.din("wbg", [D, 2 * NH])
    convw_d, convwb = C.din("convw", [NC3 * 128, 4])
    hp_d, hpb = C.din("hparm", [1, 2 * NH])
    nw_d, nwb = C.din("normw", [1, 128])
    cst_d, cstb = C.din("gcst", [6, 128, 128])
    ob_d, obb = C.dout("o_b", [SEQ, NH * 128])

    cst, cstsb = C.sb([128, 6, 128], F32, "cst")
    S.dma(lambda e: e.dma_start(out=cst[:], in_=cst_d.rearrange("a p t -> p a t")), [cstb], [cstsb])
    ident = cst[:, 0, :]
    identb = cstsb
    tri = cst[0:64, 1, 0:64]
    ones = cst[:, 2, :]
    mneg = cst[0:64, 3, 0:64]
    negst = cst[0:64, 4, 0:64]
    id64 = cst[0:64, 0, 0:64]
    wqkv, wqkvsb = C.sb([128, 8, NC3 * 128], BF16, "wqkv")
    S.dma(lambda e: e.dma_start(out=wqkv[:], in_=wqkv_d.rearrange("(k p) n -> p k n", p=128)), [wqkvb], [wqkvsb], q="pool")
    wz, wzsb = C.sb([128, 8, NH * 128], BF16, "wz")
    S.dma(lambda e: e.dma_start(out=wz[:], in_=wz_d.rearrange("(k p) n -> p k n", p=128)), [wzb], [wzsb], q="pool")
    wbg, wbgsb = C.sb([128, 8, 2 * NH], BF16, "wbg")
    S.dma(lambda e: e.dma_start(out=wbg[:], in_=wbg_d.rearrange("(k p) n -> p k n", p=128)), [wbgb], [wbgsb], q="pool")
    convw, convwsb = C.sb([128, NC3, 4], F32, "convw")
    S.dma(lambda e: e.dma_start(out=convw[:], in_=convw_d.rearrange("(c p) j -> p c j", p=128)), [convwb], [convwsb])
    hp, hpsb = C.sb([64, 2 * NH], F32, "hp")
    S.dma(lambda e: e.dma_start(out=hp[:], in_=hp_d.to_broadcast([64, 2 * NH])), [hpb], [hpsb])
    nw, nwsb = C.sb([64, 128], F32, "nw")
    S.dma(lambda e: e.dma_start(out=nw[:], in_=nw_d.to_broadcast([64, 128])), [nwb], [nwsb])
    nalog, nalogb = C.sb([64, NH], F32, "nalog")
    S.act(lambda e: e.activation(out=nalog[:], in_=hp[:, 0:NH], func=AF.Exp), [hpsb], [nalogb])
    S.dve(lambda e: e.tensor_scalar(out=nalog[:], in0=nalog[:], scalar1=-1.0, scalar2=None, op0=ALU.mult), [nalogb], [nalogb])

    xtile = [C.sb([128, D], F32, "xtile%d" % i) for i in range(2)]
    xT, xTb = C.sb([128, 8, 512], BF16, "xT")
    cin, cinb = C.sb([128, NC3, 515], F32, "cin")
    S.pool(lambda e: e.memset(cin[:].rearrange("p a b -> p (a b)"), 0.0), [], [cinb])
    cacc = [C.sb([128, 512], F32, "cacc%d" % i) for i in range(2)]
    qkv = [C.sb([128, 512], F32, "qkv%d" % i) for i in range(NC3)]
    sq = [C.sb([128, 512], F32, "sq%d" % i) for i in range(2)]
    rn = [C.sb([128, 512], F32, "rn%d" % i) for i in range(2)]
    zs, zsb = C.sb([64, 8, NH * 128], F32, "zs")
    bg, bgb = C.sb([64, 8, 2 * NH], F32, "bg")
    beta, betab = C.sb([64, 8, NH], F32, "beta")
    gg, ggb = C.sb([64, 8, NH], F32, "gg")
    gc, gcb = C.sb([64, 8, NH], F32, "gc")
    ngc, ngcb = C.sb([64, 8, NH], F32, "ngc")
    egc, egcb = C.sb([64, 8, NH], F32, "egc")
    negc, negcb = C.sb([64, 8, NH], F32, "negc")
    eglg, eglgb = C.sb([64, 8, NH], F32, "eglg")
    egl, eglb = C.sb([128, 8, NH], F32, "egl")
    St = [C.sb([128, 128], F32, "state%d" % h) for h in range(NH)]
    for h in range(NH):
        S.pool(lambda e, h=h: e.memset(St[h][0][:], 0.0), [], [St[h][1]])
    NCH = 2 * NH

    def ring(shape, name, n):
        return [C.sb(shape, F32, "%s%d" % (name, i)) for i in range(n)]
    NT_ = 2 * NH
    ktok = ring([64, 128], "ktok", NT_)
    dgc = ring([64, 64], "dgc", NT_)
    DT = ring([64, 64], "DT", NT_)
    X = ring([64, 64], "X", NT_)
    XT = ring([64, 64], "XT", NT_)
    G1s = ring([64, 64], "G1s", NT_)
    G2s = ring([64, 64], "G2s", NT_)
    vtok = ring([64, 128], "vtok", NCH)
    kpp = ring([64, 128], "kpp", NCH)
    Y = ring([64, 64], "Y", NCH)
    IT = ring([64, 64], "IT", NCH)
    NS_ = NH
    Rr = ring([64, 128], "R", NS_)
    vn = ring([64, 128], "vn", NS_)
    ivt = ring([64, 128], "ivt", NS_)
    ot = ring([64, 128], "ot", NS_)
    ss = ring([64, 1], "ss", NS_)
    osq = ring([64, 128], "osq", NS_)
    ofin = ring([64, NH * 128], "ofin", 2)
    tctr = {"i": 0}

    def drive(gens):
        gens = list(gens)
        while gens:
            for g in list(gens):
                try:
                    next(g)
                except StopIteration:
                    gens.remove(g)

    for blk in range(nblk):
        t0 = blk * 512
        load_xT_block(C, PS, xb_d, xbb, t0, ident, identb, xtile, xT, xTb)
        for c in range(NC3):
            p, pb = PS()
            for k in range(8):
                S.pe(lambda e, p=p, k=k, c=c: e.matmul(p[:], lhsT=wqkv[:, k, c * 128:(c + 1) * 128], rhs=xT[:, k, :],
                                                       start=(k == 0), stop=(k == 7)), [wqkvsb, xTb], [pb])
            S.act(lambda e, p=p, c=c: e.copy(out=cin[:, c, 3:515], in_=p[:]), [pb], [cinb])
        for c in range(NC3):
            ca, cab = cacc[c % 2]
            eng = S.dve
            eng(lambda e, ca=ca, c=c: e.tensor_scalar(out=ca[:], in0=cin[:, c, 0:512], scalar1=convw[:, c, 0:1], scalar2=None, op0=ALU.mult),
                [cinb, convwsb], [cab])
            for j in range(1, 4):
                eng(lambda e, ca=ca, c=c, j=j: e.scalar_tensor_tensor(out=ca[:], in0=cin[:, c, j:j + 512], scalar=convw[:, c, j:j + 1], in1=ca[:],
                                                                       op0=ALU.mult, op1=ALU.add), [cinb, convwsb, cab], [cab])
            S.act(lambda e, ca=ca, c=c: e.activation(out=qkv[c][0][:], in_=ca[:], func=AF.Silu), [cab], [qkv[c][1]])
        S.pool(lambda e: e.tensor_copy(out=cin[:, :, 0:3], in_=cin[:, :, 512:515]), [cinb], [cinb])
        for c in range(2 * NH):
            sq_, sqb = sq[c % 2]
            rn_, rnb = rn[c % 2]
            S.pool(lambda e, sq_=sq_, c=c: e.tensor_tensor(out=sq_[:], in0=qkv[c][0][:], in1=qkv[c][0][:], op=ALU.mult), [qkv[c][1]], [sqb])
            p, pb = PS()
            S.pe(lambda e, p=p, sq_=sq_: e.matmul(p[:], lhsT=ones, rhs=sq_[:], start=True, stop=True), [sqb, cstsb], [pb])
            S.act(lambda e, p=p, rn_=rn_: e.activation(out=rn_[:], in_=p[:], func=AF.Ln, bias=1e-6, scale=1.0), [pb], [rnb])
            S.act(lambda e, rn_=rn_: e.activation(out=rn_[:], in_=rn_[:], func=AF.Exp, scale=-0.5), [rnb], [rnb])
            sc = float(GD ** -0.5) if c < NH else 1.0
            S.dve(lambda e, rn_=rn_, c=c, sc=sc: e.scalar_tensor_tensor(out=qkv[c][0][:], in0=qkv[c][0][:], scalar=sc, in1=rn_[:],
                                                                        op0=ALU.mult, op1=ALU.mult), [qkv[c][1], rnb], [qkv[c][1]])
        for cc in range(8):
            p, pb = PS()
            for k in range(8):
                S.pe(lambda e, p=p, k=k, cc=cc: e.matmul(p[0:64, 0:NH * 128], lhsT=xT[:, k, cc * 64:(cc + 1) * 64], rhs=wz[:, k, :],
                                                         start=(k == 0), stop=(k == 7)), [xTb, wzsb], [pb])
            S.act(lambda e, p=p, cc=cc: e.activation(out=zs[:, cc, :], in_=p[0:64, 0:NH * 128], func=AF.Silu), [pb], [zsb])
            p2, pb2 = PS()
            for k in range(8):
                S.pe(lambda e, p2=p2, k=k, cc=cc: e.matmul(p2[0:64, 0:2 * NH], lhsT=xT[:, k, cc * 64:(cc + 1) * 64], rhs=wbg[:, k, :],
                                                           start=(k == 0), stop=(k == 7)), [xTb, wbgsb], [pb2])
            S.dve(lambda e, p2=p2, cc=cc: e.tensor_copy(out=bg[:, cc, :], in_=p2[0:64, 0:2 * NH]), [pb2], [bgb])
        S.act(lambda e: e.activation(out=beta[:], in_=bg[:, :, 0:NH], func=AF.Sigmoid), [bgb], [betab])
        for h in range(NH):
            S.act(lambda e, h=h: e.activation(out=gg[:, :, h], in_=bg[:, :, NH + h], func=AF.Exp, bias=hp[:, NH + h:NH + h + 1], scale=1.0),
                  [bgb, hpsb], [ggb])
        S.act(lambda e: e.activation(out=gg[:], in_=gg[:], func=AF.Ln, bias=1.0, scale=1.0), [ggb], [ggb])
        for h in range(NH):
            S.dve(lambda e, h=h: e.tensor_scalar(out=gg[:, :, h], in0=gg[:, :, h], scalar1=nalog[:, h:h + 1], scalar2=None, op0=ALU.mult),
                  [ggb, nalogb], [ggb])
        ggf = gg[:].rearrange("p a b -> p (a b)")
        NG = 8 * NH
        p, pb = PS()
        S.pe(lambda e, p=p: e.matmul(p[0:64, 0:NG], lhsT=tri, rhs=ggf, start=True, stop=True), [ggb, cstsb], [pb])
        S.dve(lambda e, p=p: e.tensor_copy(out=gc[:].rearrange("p a b -> p (a b)"), in_=p[0:64, 0:NG]), [pb], [gcb])
        S.dve(lambda e: e.tensor_scalar(out=ngc[:], in0=gc[:], scalar1=-1.0, scalar2=None, op0=ALU.mult), [gcb], [ngcb])
        S.act(lambda e: e.activation(out=egc[:], in_=gc[:], func=AF.Exp), [gcb], [egcb])
        S.dve(lambda e: e.tensor_scalar(out=negc[:], in0=egc[:], scalar1=-1.0, scalar2=None, op0=ALU.mult), [egcb], [negcb])
        p, pb = PS()
        S.pe(lambda e, p=p: e.matmul(p[:, 0:NG], lhsT=ones[0:64, :], rhs=ggf, start=True, stop=True), [ggb, cstsb], [pb])
        S.act(lambda e, p=p: e.activation(out=egl[:].rearrange("p a b -> p (a b)"), in_=p[:, 0:NG], func=AF.Exp), [pb], [eglb, pb])
        S.dve(lambda e, p=p: e.tensor_tensor(out=eglg[:].rearrange("p a b -> p (a b)"), in0=p[0:64, 0:NG],
                                             in1=gc[:].rearrange("p a b -> p (a b)"), op=ALU.subtract), [pb, gcb], [eglgb])
        S.act(lambda e: e.activation(out=eglg[:], in_=eglg[:], func=AF.Exp), [eglgb], [eglgb])

        def pre(cc, h):
            csl = slice(cc * 64, (cc + 1) * 64)
            ch = (cc % 2) * NH + h
            r = tctr["i"]
            tctr["i"] += 1
            qT, qTb = qkv[h]
            kT, kTb = qkv[NH + h]
            vT, vTb = qkv[2 * NH + h]
            kt, ktb = ktok[r % NT_]
            dg, dgb = dgc[r % NT_]
            dt_, dtb = DT[r % NT_]
            x_, xb_ = X[r % NT_]
            xt_, xtb_ = XT[r % NT_]
            g1, g1b = G1s[r % NT_]
            g2, g2b = G2s[r % NT_]
            vt, vtb = vtok[ch]
            kp_, kpb = kpp[ch]
            y_, yb_ = Y[ch]
            it_, itb = IT[ch]
            bcol = beta[:, cc, h:h + 1]
            p, pb = PS()
            S.pe(lambda e: e.transpose(out=p[0:64, 0:128], in_=kT[:, csl], identity=ident), [kTb, cstsb], [pb])
            S.dve(lambda e: e.tensor_copy(out=kt[:], in_=p[0:64, 0:128]), [pb], [ktb])
            p2, pb2 = PS()
            S.pe(lambda e: e.transpose(out=p2[0:64, 0:128], in_=vT[:, csl], identity=ident), [vTb, cstsb], [pb2])
            S.act(lambda e: e.copy(out=vt[:], in_=p2[0:64, 0:128]), [pb2], [vtb])
            S.pool(lambda e: e.tensor_scalar(out=dg[:], in0=id64, scalar1=gc[:, cc, h:h + 1], scalar2=None, op0=ALU.mult), [gcb, cstsb], [dgb])
            yield
            p3, pb3 = PS()
            S.pe(lambda e: e.matmul(p3[0:64, 0:64], lhsT=ones[0:64, 0:64], rhs=dg[:], start=True, stop=False), [dgb, cstsb], [pb3])
            S.pe(lambda e: e.matmul(p3[0:64, 0:64], lhsT=id64, rhs=mneg, start=False, stop=True), [cstsb], [pb3])
            S.act(lambda e: e.activation(out=dt_[:], in_=p3[0:64, 0:64], func=AF.Exp, bias=ngc[:, cc, h:h + 1], scale=1.0), [pb3, ngcb], [dtb])
            p4, pb4 = PS()
            S.pe(lambda e: e.matmul(p4[0:64, 0:64], lhsT=kT[:, csl], rhs=kT[:, csl], start=True, stop=True), [kTb], [pb4])
            S.dve(lambda e: e.tensor_scalar(out=g1[:], in0=p4[0:64, 0:64], scalar1=bcol, scalar2=None, op0=ALU.mult), [pb4, betab], [g1b])
            p5, pb5 = PS()
            S.pe(lambda e: e.matmul(p5[0:64, 0:64], lhsT=kT[:, csl], rhs=qT[:, csl], start=True, stop=True), [kTb, qTb], [pb5])
            S.dve(lambda e: e.tensor_copy(out=g2[:], in_=p5[0:64, 0:64]), [pb5], [g2b])
            S.pool(lambda e: e.tensor_scalar(out=kp_[:], in0=kt[:], scalar1=eglg[:, cc, h:h + 1], scalar2=None, op0=ALU.mult), [ktb, eglgb], [kpb])
            yield
            S.dve(lambda e: e.tensor_tensor(out=x_[:], in0=g1[:], in1=dt_[:], op=ALU.mult), [g1b, dtb], [xb_])
            S.dve(lambda e: e.tensor_tensor(out=x_[:], in0=x_[:], in1=negst, op=ALU.mult), [xb_, cstsb], [xb_])
            S.pool(lambda e: e.tensor_tensor(out=it_[:], in0=g2[:], in1=dt_[:], op=ALU.mult), [g2b, dtb], [itb])
            yield
            p6, pb6 = PS()
            S.pe(lambda e: e.transpose(out=p6[0:64, 0:64], in_=x_[:], identity=id64), [xb_, cstsb], [pb6])
            S.act(lambda e: e.copy(out=xt_[:], in_=p6[0:64, 0:64]), [pb6], [xtb_])
            S.pool(lambda e: e.tensor_tensor(out=y_[:], in0=x_[:], in1=id64, op=ALU.add), [xb_, cstsb], [yb_])
            yield
            for m in range(5):
                pt, ptb = PS()
                S.pe(lambda e, pt=pt: e.matmul(pt[0:64, 0:64], lhsT=x_[:], rhs=xt_[:], start=True, stop=True), [xb_, xtb_], [ptb])
                if m < 4:
                    pa, pab = PS()
                    S.pe(lambda e, pa=pa: e.matmul(pa[0:64, 0:64], lhsT=xt_[:], rhs=x_[:], start=True, stop=True), [xb_, xtb_], [pab])
                    S.dve(lambda e, pa=pa: e.tensor_copy(out=x_[:], in_=pa[0:64, 0:64]), [pab], [xb_])
                S.act(lambda e, pt=pt: e.copy(out=xt_[:], in_=pt[0:64, 0:64]), [ptb], [xtb_])
                yield
                py, pyb = PS()
                S.pe(lambda e, py=py: e.matmul(py[0:64, 0:64], lhsT=xt_[:], rhs=y_[:], start=True, stop=True), [xtb_, yb_], [pyb])
                S.dve(lambda e, py=py: e.tensor_tensor(out=y_[:], in0=py[0:64, 0:64], in1=y_[:], op=ALU.add), [pyb, yb_], [yb_])
                yield

        def scan(cc, h, of, ofb):
            csl = slice(cc * 64, (cc + 1) * 64)
            ch = (cc % 2) * NH + h
            qT, qTb = qkv[h]
            kT, kTb = qkv[NH + h]
            s_, sb_ = St[h]
            vt, vtb = vtok[ch]
            kp_, kpb = kpp[ch]
            y_, yb_ = Y[ch]
            it_, itb = IT[ch]
            R_, Rb = Rr[h]
            vn_, vnb = vn[h]
            qs_, qsb = ivt[h]
            o_, ob_ = ot[h]
            ss_, ssb = ss[h]
            oq_, oqb = osq[h]
            bcol = beta[:, cc, h:h + 1]
            p, pb = PS()
            S.pe(lambda e: e.matmul(p[0:64, 0:128], lhsT=kT[:, csl], rhs=s_[:], start=True, stop=True), [kTb, sb_], [pb])
            S.dve(lambda e: e.scalar_tensor_tensor(out=R_[:], in0=p[0:64, 0:128], scalar=negc[:, cc, h:h + 1], in1=vt[:], op0=ALU.mult, op1=ALU.add),
                  [pb, negcb, vtb], [Rb])
            pq, pqb = PS()
            S.pe(lambda e: e.matmul(pq[0:64, 0:128], lhsT=qT[:, csl], rhs=s_[:], start=True, stop=True), [qTb, sb_], [pqb])
            S.dve(lambda e: e.tensor_scalar(out=qs_[:], in0=pq[0:64, 0:128], scalar1=egc[:, cc, h:h + 1], scalar2=None, op0=ALU.mult), [pqb, egcb], [qsb])
            yield
            p2, pb2 = PS()
            S.pe(lambda e: e.matmul(p2[0:64, 0:128], lhsT=y_[:], rhs=R_[:], start=True, stop=True), [yb_, Rb], [pb2])
            S.dve(lambda e: e.tensor_scalar(out=vn_[:], in0=p2[0:64, 0:128], scalar1=bcol, scalar2=None, op0=ALU.mult), [pb2, betab], [vnb])
            yield
            p3, pb3 = PS()
            S.pe(lambda e: e.matmul(p3[:, 0:128], lhsT=kp_[:], rhs=vn_[:], start=True, stop=True), [kpb, vnb], [pb3])
            S.dve(lambda e: e.scalar_tensor_tensor(out=s_[:], in0=s_[:], scalar=egl[:, cc, h:h + 1], in1=p3[:, 0:128], op0=ALU.mult, op1=ALU.add),
                  [pb3, eglb, sb_], [sb_])
            p4, pb4 = PS()
            S.pe(lambda e: e.matmul(p4[0:64, 0:128], lhsT=it_[:], rhs=vn_[:], start=True, stop=True), [itb, vnb], [pb4])
            S.dve(lambda e: e.tensor_tensor(out=o_[:], in0=p4[0:64, 0:128], in1=qs_[:], op=ALU.add), [pb4, qsb], [ob_])
            yield
            S.pool(lambda e: e.tensor_tensor(out=oq_[:], in0=o_[:], in1=o_[:], op=ALU.mult), [ob_], [oqb])
            yield
            S.dve(lambda e: e.reduce_sum(out=ss_[:], in_=oq_[:], axis=AX.X), [oqb], [ssb])
            yield
            S.act(lambda e: e.activation(out=ss_[:], in_=ss_[:], func=AF.Ln, bias=1e-6, scale=1.0 / GD), [ssb], [ssb])
            S.act(lambda e: e.activation(out=ss_[:], in_=ss_[:], func=AF.Exp, scale=-0.5), [ssb], [ssb])
            yield
            S.dve(lambda e: e.scalar_tensor_tensor(out=o_[:], in0=o_[:], scalar=ss_[:, 0:1], in1=nw[:], op0=ALU.mult, op1=ALU.mult),
                  [ob_, ssb, nwsb], [ob_])
            yield
            S.pool(lambda e: e.tensor_tensor(out=of[:, h * 128:(h + 1) * 128], in0=o_[:], in1=zs[:, cc, h * 128:(h + 1) * 128], op=ALU.mult),
                   [ob_, zsb], [ofb])

        for cg in range(4):
            drive([pre(cc, h) for cc in (2 * cg, 2 * cg + 1) for h in range(NH)])
            for cc in (2 * cg, 2 * cg + 1):
                of, ofb = ofin[cc % 2]
                drive([scan(cc, h, of, ofb) for h in range(NH)])
                S.dma(lambda e, of=of, cc=cc, t0=t0: e.dma_start(out=ob_d[t0 + cc * 64:t0 + (cc + 1) * 64, ocol:ocol + NH * 128], in_=of[:]), [ofb], [obb])
    S.emit(stack)


def gdn_consts():
    c = np.zeros((6, 128, 128), np.float32)
    c[0] = np.eye(128)
    k = np.arange(128)
    c[1] = (k[:, None] <= k[None, :]).astype(np.float32)
    c[2] = 1.0
    c[3] = np.where(k[None, :] < k[:, None], -30000.0, 0.0)
    c[4] = np.where(k[None, :] > k[:, None], -1.0, 0.0)
    return c


def gdn_inputs(inp, b, half, heads=None):
    if heads is None:
        heads = [2 * half, 2 * half + 1]
    w_in = inp["ev_w_in"][0]
    o_gq = 512 + 6 * 128 + 24
    o_gk, o_gv, o_gz = o_gq + 512, o_gq + 1024, o_gq + 1536
    o_gb = o_gq + 2048
    o_ga = o_gb + 4
    hc = lambda off: np.concatenate([w_in[:, off + h * 128: off + (h + 1) * 128] for h in heads], 1)
    wqkv = np.concatenate([hc(o_gq), hc(o_gk), hc(o_gv)], 1)
    wz = hc(o_gz)
    wbg = np.concatenate([w_in[:, [o_gb + h for h in heads]], w_in[:, [o_ga + h for h in heads]]], 1)
    cw = inp["ev_conv_w"][0]
    cc = lambda off: np.concatenate([cw[:, off + h * 128: off + (h + 1) * 128] for h in heads], 1)
    convw = np.concatenate([cc(0), cc(512), cc(1024)], 1).T
    hparm = np.concatenate([inp["ev_a_log"][0][heads], inp["ev_dt_bias"][0][heads]])[None, :]
    return {"xb": np.ascontiguousarray(inp["x"][b]), "wqkv": np.ascontiguousarray(wqkv), "wz": np.ascontiguousarray(wz),
            "wbg": np.ascontiguousarray(wbg), "convw": np.ascontiguousarray(convw), "hparm": np.ascontiguousarray(hparm),
            "normw": np.ascontiguousarray(inp["ev_gdn_norm"][0][None, :]), "gcst": gdn_consts()}


def build_nsa(nc, stack, nqblk=SEQ // 512, shared=None, pfx="", io=None, ocol=0):
    C = Ctx(nc, stack, shared, pfx, io)
    S = C.S
    PS = PsumRing(C, 4)
    ACC = [C.ps([128, 512], F32, "acc%d" % i) for i in range(4)]
    xb_d, xbb = C.din("xb", [SEQ, D])
    wq_d, wqb = C.din("wq", [D, 1024])
    wk_d, wkb = C.din("wk", [D, 384])
    wv_d, wvb = C.din("wv", [D, 128])
    wgt_d, wgtb = C.din("wgt", [D, 12])
    cwk_d, cwkb = C.din("cmpwk", [2, 64, 32, 64])
    cwv_d, cwvb = C.din("cmpwv", [128, 32, 64])
    pek_d, pekb = C.din("cmppek", [64, 32])
    pev_d, pevb = C.din("cmppev", [128, 32])
    rope_d, ropeb = C.din("rope", [2, 128, SEQ])
    ropek_d, ropekb = C.din("ropek", [2, 64, 512])
    E_d, Eb = C.din("Eexp", [128, 32, 128])
    mk_d, mkb = C.din("masks", [8, 128, 512])
    cmk_d, cmkb = C.din("cmask", [17, 128, 128])
    ov_d, ovb = C.din("overlap", [512, 128])
    W_d, Wb = C.din("forceW", [128, 256])
    id_d, idb = C.din("ident", [128, 128])
    oa_d, oab = C.dout("o_a", [SEQ, 256])

    ident, identb = C.sb([128, 128], F32, "ident")
    S.dma(lambda e: e.dma_start(out=ident[:], in_=id_d), [idb], [identb])
    if "zero_rows" in C.io:
        for (zdst, zsrc) in C.io["zero_rows"]:
            S.dma(lambda e, zdst=zdst, zsrc=zsrc: e.dma_start(out=zdst, in_=zsrc), [], [oab])
    wq, wqsb = C.sb([128, 8, 1024], BF16, "wq")
    S.dma(lambda e: e.dma_start(out=wq[:], in_=wq_d.rearrange("(k p) n -> p k n", p=128)), [wqb], [wqsb], q="pool")
    wk, wksb = C.sb([128, 8, 384], BF16, "wk")
    S.dma(lambda e: e.dma_start(out=wk[:], in_=wk_d.rearrange("(k p) n -> p k n", p=128)), [wkb], [wksb], q="pool")
    wv, wvsb = C.sb([128, 8, 128], BF16, "wv")
    S.dma(lambda e: e.dma_start(out=wv[:], in_=wv_d.rearrange("(k p) n -> p k n", p=128)), [wvb], [wvsb], q="pool")
    wgt, wgtsb = C.sb([128, 8, 12], BF16, "wgt")
    S.dma(lambda e: e.dma_start(out=wgt[:], in_=wgt_d.rearrange("(k p) n -> p k n", p=128)), [wgtb], [wgtsb], q="pool")
    cwk, cwksb = C.sb([64, 2, 32, 64], BF16, "cwk")
    S.dma(lambda e: e.dma_start(out=cwk[:], in_=cwk_d.rearrange("a d l e -> d a l e")), [cwkb], [cwksb], q="pool")
    cwv, cwvsb = C.sb([128, 32, 64], BF16, "cwv")
    S.dma(lambda e: e.dma_start(out=cwv[:], in_=cwv_d), [cwvb], [cwvsb], q="pool")
    pek, peksb = C.sb([64, 32], BF16, "pek")
    S.dma(lambda e: e.dma_start(out=pek[:], in_=pek_d), [pekb], [peksb], q="pool")
    pev, pevsb = C.sb([128, 32], BF16, "pev")
    S.dma(lambda e: e.dma_start(out=pev[:], in_=pev_d), [pevb], [pevsb], q="pool")
    ropek, ropeksb = C.sb([64, 2, 512], F32, "ropek")
    S.dma(lambda e: e.dma_start(out=ropek[:], in_=ropek_d.rearrange("a d n -> d a n")), [ropekb], [ropeksb])
    Ex, Exb = C.sb([128, 32, 128], BF16, "Ex")
    S.dma(lambda e: e.dma_start(out=Ex[:], in_=E_d), [Eb], [Exb], q="pool")
    mk, mksb = C.sb([128, 8, 512], BF16, "mk")
    S.dma(lambda e: e.dma_start(out=mk[:], in_=mk_d.rearrange("a p q -> p a q")), [mkb], [mksb], q="pool")
    cmk, cmksb = C.sb([128, 17, 128], BF16, "cmk")
    S.dma(lambda e: e.dma_start(out=cmk[:], in_=cmk_d.rearrange("a p q -> p a q")), [cmkb], [cmksb], q="pool")
    Wt, Wsb = C.sb([128, 256], F32, "Wt")
    S.dma(lambda e: e.dma_start(out=Wt[:], in_=W_d), [Wb], [Wsb])

    kT2, kT2b = C.sb([128, SEQ], BF16, "kT2")
    kvcin, kvcinb = C.sb([128, SEQ], BF16, "kvcin")
    vslc, vslcb = C.sb([128, 64, 65], BF16, "vslc")
    vwin, vwinb = C.sb([128, 64, 65], BF16, "vwin")
    S.pool(lambda e: e.memset(vslc[:].rearrange("p a b -> p (a b)"), 1.0), [], [vslcb])
    S.pool(lambda e: e.memset(vwin[:].rearrange("p a b -> p (a b)"), 1.0), [], [vwinb])
    kcT, kcTb = C.sb([64, 512], F32, "kcT")
    vca, vcab = C.sb([128, 4, 193], F32, "vca")
    S.pool(lambda e: e.memset(vca[:].rearrange("p a b -> p (a b)"), 0.0), [], [vcab])
    S.pool(lambda e: e.memset(vca[:, :, 64:65], 1.0), [vcab], [vcab])
    S.dma(lambda e: e.dma_start(out=vca[:, :, 65:193], in_=ov_d.rearrange("(c p) m -> p c m", p=128)), [ovb, vcab], [vcab])

    xtile = [C.sb([128, D], F32, "xtile%d" % i) for i in range(2)]
    xT, xTb = C.sb([128, 8, 512], BF16, "xT")
    rp, rpb = C.sb([128, 2, 512], F32, "rp")
    t1, t1b = C.sb([128, 512], F32, "t1")
    t2, t2b = C.sb([128, 512], F32, "t2")

    def rope_from(pa, pab, pbk, pbkb, dst, dstb):
        S.dve(lambda e: e.tensor_tensor(out=t1[:], in0=pa, in1=rp[:, 0, :], op=ALU.mult), [pab, rpb], [t1b])
        S.dve(lambda e: e.tensor_tensor(out=t2[:], in0=pbk, in1=rp[:, 1, :], op=ALU.mult), [pbkb, rpb], [t2b])
        S.pool(lambda e: e.tensor_tensor(out=dst, in0=t1[:], in1=t2[:], op=ALU.add), [t1b, t2b], [dstb])

    for blk in range(SEQ // 512):
        t0 = blk * 512
        load_xT_block(C, PS, xb_d, xbb, t0, ident, identb, xtile, xT, xTb)
        S.dma(lambda e, t0=t0: e.dma_start(out=rp[:], in_=rope_d[:, :, t0:t0 + 512].rearrange("a d n -> d a n")), [ropeb], [rpb])
        pk = []
        for c in range(3):
            p, pb = ACC[c]
            for k in range(8):
                S.pe(lambda e, p=p, k=k, c=c: e.matmul(p[:], lhsT=wk[:, k, c * 128:(c + 1) * 128], rhs=xT[:, k, :],
                                                       start=(k == 0), stop=(k == 7)), [wksb, xTb], [pb])
            pk.append((p, pb))
        S.act(lambda e, t0=t0: e.copy(out=kvcin[:, t0:t0 + 512], in_=pk[0][0][:]), [pk[0][1]], [kvcinb])
        rope_from(pk[1][0][:], pk[1][1], pk[2][0][:], pk[2][1], kT2[:, t0:t0 + 512], kT2b)
        for j in range(4):
            p, pb = PS()
            for k in range(8):
                S.pe(lambda e, p=p, k=k, j=j: e.matmul(p[:, 0:128], lhsT=xT[:, k, j * 128:(j + 1) * 128], rhs=wv[:, k, :],
                                                       start=(k == 0), stop=(k == 7)), [xTb, wvsb], [pb])
            tix = blk * 4 + j
            S.act(lambda e, p=p, tix=tix: e.copy(out=vslc[:, tix, 0:64], in_=p[:, 0:64]), [pb], [vslcb, pb])
            S.dve(lambda e, p=p, tix=tix: e.tensor_copy(out=vwin[:, tix, 0:64], in_=p[:, 64:128]), [pb], [vwinb])
    kb_, kbb = C.sb([128, 4], F32, "kbias")
    for a in range(2):
        p, pb = PS()
        for l in range(32):
            S.pe(lambda e, p=p, l=l, a=a: e.matmul(p[0:64, 0:1], lhsT=cwk[:, a, l, :], rhs=pek[:, l:l + 1],
                                                   start=(l == 0), stop=(l == 31)), [cwksb, peksb], [pb])
        S.dve(lambda e, p=p, a=a: e.tensor_copy(out=kb_[0:64, a:a + 1], in_=p[0:64, 0:1]), [pb], [kbb])
    p, pb = PS()
    for l in range(32):
        S.pe(lambda e, p=p, l=l: e.matmul(p[0:64, 0:1], lhsT=cwv[64:128, l, :], rhs=pev[64:128, l:l + 1],
                                          start=(l == 0), stop=(l == 31)), [cwvsb, pevsb], [pb])
    S.dve(lambda e, p=p: e.tensor_copy(out=kb_[0:64, 2:3], in_=p[0:64, 0:1]), [pb], [kbb])
    kc0, kc0b = C.sb([64, 512], F32, "kc0")
    kc1, kc1b = C.sb([64, 512], F32, "kc1")
    S.pool(lambda e: e.memset(kc0[:], 0.0), [], [kc0b])
    S.pool(lambda e: e.memset(kc1[:], 0.0), [], [kc1b])
    for a, (dst, dstb) in enumerate([(kc0, kc0b), (kc1, kc1b)]):
        p, pb = PS()
        for l in range(32):
            S.pe(lambda e, p=p, l=l, a=a: e.matmul(p[0:64, 0:511], lhsT=cwk[:, a, l, :], rhs=kvcin[0:64, l:l + 16 * 510 + 1:16],
                                                   start=(l == 0), stop=(l == 31)), [cwksb, kvcinb], [pb])
        S.dve(lambda e, p=p, dst=dst, a=a: e.tensor_scalar(out=dst[:, 0:511], in0=p[0:64, 0:511], scalar1=kb_[0:64, a:a + 1], scalar2=None, op0=ALU.add),
              [pb, kbb], [dstb])
    S.dve(lambda e: e.tensor_tensor(out=kc0[:], in0=kc0[:], in1=ropek[:, 0, :], op=ALU.mult), [kc0b, ropeksb], [kc0b])
    S.dve(lambda e: e.tensor_tensor(out=kc1[:], in0=kc1[:], in1=ropek[:, 1, :], op=ALU.mult), [kc1b, ropeksb], [kc1b])
    S.dve(lambda e: e.tensor_tensor(out=kcT[:], in0=kc0[:], in1=kc1[:], op=ALU.add), [kc0b, kc1b], [kcTb])
    vbT, vbTb = C.sb([64, 128], F32, "vbT")
    S.pool(lambda e: e.memset(vbT[:], 0.0), [], [vbTb])
    S.dve(lambda e: e.tensor_scalar(out=vbT[:], in0=vbT[:], scalar1=kb_[0:64, 2:3], scalar2=None, op0=ALU.add), [vbTb, kbb], [vbTb])
    vbr, vbrb = C.sb([128, 64], F32, "vbr")
    p, pb = PS()
    S.pe(lambda e, p=p: e.transpose(out=p[:, 0:64], in_=vbT[:], identity=ident[0:64, 0:64]), [vbTb, identb], [pb])
    S.dve(lambda e, p=p: e.tensor_copy(out=vbr[:], in_=p[:, 0:64]), [pb], [vbrb])
    for c in range(4):
        nn = 128 if c < 3 else 127
        p, pb = PS()
        for l in range(32):
            s0 = l + 16 * 128 * c
            S.pe(lambda e, p=p, l=l, s0=s0, nn=nn: e.matmul(p[0:nn, 0:64], lhsT=kvcin[64:128, s0:s0 + 16 * (nn - 1) + 1:16], rhs=cwv[64:128, l, :],
                                                            start=(l == 0), stop=(l == 31)), [cwvsb, kvcinb], [pb])
        S.dve(lambda e, p=p, c=c, nn=nn: e.tensor_tensor(out=vca[0:nn, c, 0:64], in0=p[0:nn, 0:64], in1=vbr[0:nn, :], op=ALU.add), [pb, vbrb, vcab], [vcab])

    qT = [C.sb([128, 512], F32, "qT%d" % h) for h in range(4)]
    qTh = [C.sb([128, 512], BF16, "qTh%d" % h) for h in range(4)]
    gts, gtsb = C.sb([128, 4, 12], F32, "gts")
    PTc = [C.sb([128, 512], F32, "PTc%d" % i) for i in range(4)]
    rz = [C.sb([128, 1], F32, "rz%d" % i) for i in range(4)]
    oc, ocb = C.sb([128, 4, 4, 64], F32, "oc")
    imps = [C.sb([128, 128], F32, "imp%d" % i) for i in range(2)]
    sc2, sc2b = C.sb([128, 128], F32, "sc2")
    m8a, m8ab = C.sb([128, 8], F32, "m8a")
    m8b, m8bb = C.sb([128, 8], F32, "m8b")
    sel, selb = C.sb([128, 128], F32, "sel")
    selT, selTb = C.sb([128, 512], BF16, "selT")
    maskT = [C.sb([128, 512], BF16, "maskT%d" % i) for i in range(3)]
    PT = [C.sb([128, 512], BF16, "PT%d" % i) for i in range(6)]
    osT = [C.sb([65, 512], F32, "osT%d" % i) for i in range(2)]
    fs = [C.sb([128, 1], F32, "fs%d" % i) for i in range(2)]
    oa, oab_ = C.sb([128, 4, 256], F32, "oa")
    ctr = {"pt": 0, "m": 0, "o": 0}
    SCALE = 0.125

    for qblk in range(nqblk):
        t0 = qblk * 512
        load_xT_block(C, PS, xb_d, xbb, t0, ident, identb, xtile, xT, xTb)
        S.dma(lambda e, t0=t0: e.dma_start(out=rp[:], in_=rope_d[:, :, t0:t0 + 512].rearrange("a d n -> d a n")), [ropeb], [rpb])
        for h in range(4):
            pa, pab = PS()
            pb_, pbb = PS()
            for k in range(8):
                S.pe(lambda e, pa=pa, k=k, h=h: e.matmul(pa[:], lhsT=wq[:, k, h * 256:h * 256 + 128], rhs=xT[:, k, :],
                                                         start=(k == 0), stop=(k == 7)), [wqsb, xTb], [pab])
            for k in range(8):
                S.pe(lambda e, pb_=pb_, k=k, h=h: e.matmul(pb_[:], lhsT=wq[:, k, h * 256 + 128:h * 256 + 256], rhs=xT[:, k, :],
                                                           start=(k == 0), stop=(k == 7)), [wqsb, xTb], [pbb])
            rope_from(pa[:], pab, pb_[:], pbb, qT[h][0][:], qT[h][1])
            S.act(lambda e, h=h: e.copy(out=qTh[h][0][:], in_=qT[h][0][:]), [qT[h][1]], [qTh[h][1]])
        for j in range(4):
            p, pb = PS()
            for k in range(8):
                S.pe(lambda e, p=p, k=k, j=j: e.matmul(p[:, 0:12], lhsT=xT[:, k, j * 128:(j + 1) * 128], rhs=wgt[:, k, :],
                                                       start=(k == 0), stop=(k == 7)), [xTb, wgtsb], [pb])
            S.act(lambda e, p=p, j=j: e.activation(out=gts[:, j, :], in_=p[:, 0:12], func=AF.Sigmoid), [pb], [gtsb])
        csteps = [(j, h) for j in range(4) for h in range(4)]
        cinfo = {}

        def chunks_of(qb):
            out = []
            for c in range(4):
                delta = 128 * c - 8 * qb
                if delta >= 7:
                    continue
                out.append((c, None if delta <= -129 else (delta + 128) // 8))
            return out

        def cstageA(j, h):
            qb = qblk * 4 + j
            chunks = chunks_of(qb)
            i = ctr["pt"]
            ctr["pt"] += 1
            ptc, ptcb = PTc[i % len(PTc)]
            rz_, rzb = rz[i % len(rz)]
            p, pb = PS()
            for (c, mi) in chunks:
                S.pe(lambda e, p=p, c=c, h=h, j=j: e.matmul(p[:, c * 128:(c + 1) * 128], lhsT=kcT[:, c * 128:(c + 1) * 128],
                                                            rhs=qT[h][0][0:64, j * 128:(j + 1) * 128], start=True, stop=True),
                     [kcTb, qT[h][1]], [pb])
            nc_ = len(chunks) * 128
            S.act(lambda e, p=p, ptc=ptc, nc_=nc_: e.activation(out=ptc[:, 0:nc_], in_=p[:, 0:nc_], func=AF.Exp, scale=SCALE), [pb], [ptcb])
            for (c, mi) in chunks:
                if mi is not None:
                    S.dve(lambda e, ptc=ptc, c=c, mi=mi: e.tensor_tensor(out=ptc[:, c * 128:(c + 1) * 128], in0=ptc[:, c * 128:(c + 1) * 128],
                                                                         in1=cmk[:, mi, :], op=ALU.mult), [ptcb, cmksb], [ptcb])
            cinfo[(j, h)] = (chunks, ptc, ptcb, rz_, rzb)

        def cstageB(j, h):
            qb = qblk * 4 + j
            chunks, ptc, ptcb, rz_, rzb = cinfo[(j, h)]
            po, pob = PS()
            for ci, (c, mi) in enumerate(chunks):
                S.pe(lambda e, po=po, ptc=ptc, c=c, ci=ci, n=len(chunks): e.matmul(po[:, 0:193], lhsT=ptc[:, c * 128:(c + 1) * 128], rhs=vca[:, c, :],
                                                                                 start=(ci == 0), stop=(ci == n - 1)), [ptcb, vcab], [pob])
            S.dve(lambda e, po=po, rz_=rz_: e.tensor_scalar(out=rz_[:], in0=po[:, 64:65], scalar1=1e-30, scalar2=None, op0=ALU.max), [pob], [rzb])
            S.dve(lambda e, rz_=rz_: e.reciprocal(out=rz_[:], in_=rz_[:]), [rzb], [rzb])
            S.dve(lambda e, po=po, rz_=rz_, j=j, h=h: e.tensor_scalar(out=oc[:, j, h, :], in0=po[:, 0:64], scalar1=rz_[:, 0:1], scalar2=None,
                                                                    op0=ALU.mult), [pob, rzb], [ocb])
            im_, imb_ = imps[j % 2]
            if h == 0:
                S.dve(lambda e, po=po, rz_=rz_, im_=im_: e.tensor_scalar(out=im_[:], in0=po[:, 65:193], scalar1=rz_[:, 0:1], scalar2=None, op0=ALU.mult),
                      [pob, rzb], [imb_])
            else:
                S.dve(lambda e, po=po, rz_=rz_, im_=im_: e.scalar_tensor_tensor(out=im_[:], in0=po[:, 65:193], scalar=rz_[:, 0:1], in1=im_[:],
                                                                                op0=ALU.mult, op1=ALU.add), [pob, rzb, imb_], [imb_])
            if h == 3:
                S.dve(lambda e, qb=qb, im_=im_: e.tensor_tensor(out=im_[:], in0=im_[:], in1=Wt[:, 128 - 2 * qb:256 - 2 * qb], op=ALU.max), [imb_, Wsb], [imb_])
                S.pool(lambda e, im_=im_: e.memset(im_[:, 0:1], 1e6), [imb_], [imb_])
                S.dve(lambda e, im_=im_: e.max(out=m8a[:], in_=im_[:]), [imb_], [m8ab])
                S.dve(lambda e, im_=im_: e.match_replace(out=sc2[:], in_to_replace=m8a[:], in_values=im_[:], imm_value=-2.0), [m8ab, imb_], [sc2b])
                S.dve(lambda e: e.max(out=m8b[:], in_=sc2[:]), [sc2b], [m8bb])
                S.dve(lambda e, im_=im_: e.tensor_scalar(out=sel[:], in0=im_[:], scalar1=m8b[:, 7:8], scalar2=None, op0=ALU.is_ge), [imb_, m8bb], [selb])
                pst, pstb = PS()
                S.pe(lambda e, pst=pst: e.transpose(out=pst[:, 0:128], in_=sel[:], identity=ident[:]), [selb, identb], [pstb])
                S.act(lambda e, pst=pst, j=j: e.copy(out=selT[:, j * 128:(j + 1) * 128], in_=pst[:, 0:128]), [pstb], [selTb])

        LC = 2
        for i in range(len(csteps) + LC):
            if i < len(csteps):
                cstageA(*csteps[i])
            if i >= LC:
                cstageB(*csteps[i - LC])

        for br in range(2):
            if br == 0:
                kcs = list(range(0, 4 * qblk + 4))
                VV, VVb = vslc, vslcb
                r0 = 0
            else:
                kcs = [kc for kc in range(4 * qblk - 4, 4 * qblk + 4) if kc >= 0]
                VV, VVb = vwin, vwinb
                r0 = 64
            pend = []
            LS = 3

            def flush_one():
                (h, kc, pt_, ptb_, ki, n) = pend.pop(0)
                S.pe(lambda e, h=h, kc=kc, pt_=pt_, VV=VV, ki=ki, n=n: e.matmul(ACC[h][0][0:65, :], lhsT=VV[:, kc, :], rhs=pt_[:],
                                                                              start=(ki == 0), stop=(ki == n - 1)), [VVb, ptb_], [ACC[h][1]])
            for ki, kc in enumerate(kcs):
                dk = kc - 4 * qblk
                if br == 0:
                    mt, mtb = maskT[ctr["m"] % len(maskT)]
                    ctr["m"] += 1
                    pm, pmb = PS()
                    base = 0 if (2 * kc) < 64 else 64
                    v = kc % 32
                    S.pe(lambda e, pm=pm, base=base, v=v: e.matmul(pm[:], lhsT=Ex[base:base + 64, v, :], rhs=selT[base:base + 64, :], start=True, stop=True),
                         [Exb, selTb], [pmb])
                    if dk >= 0:
                        S.dve(lambda e, pm=pm, mt=mt, dk=dk: e.tensor_tensor(out=mt[:], in0=pm[:], in1=mk[:, 4 + dk, :], op=ALU.mult), [pmb, mksb], [mtb])
                    else:
                        S.dve(lambda e, pm=pm, mt=mt: e.tensor_copy(out=mt[:], in_=pm[:]), [pmb], [mtb])
                    mask_ap, mask_b = mt[:], mtb
                else:
                    mask_ap, mask_b = mk[:, dk + 4, :], mksb
                for h in range(4):
                    pt_, ptb_ = PT[ctr["pt"] % len(PT)]
                    ctr["pt"] += 1
                    ps_, psb_ = PS()
                    S.pe(lambda e, ps_=ps_, kc=kc, h=h, r0=r0: e.matmul(ps_[:], lhsT=kT2[r0:r0 + 64, kc * 128:(kc + 1) * 128], rhs=qTh[h][0][r0:r0 + 64, :],
                                                                       start=True, stop=True), [kT2b, qTh[h][1]], [psb_])
                    S.act(lambda e, ps_=ps_, pt_=pt_: e.activation(out=pt_[:], in_=ps_[:], func=AF.Exp, scale=SCALE), [psb_], [ptb_])
                    S.dve(lambda e, pt_=pt_, mask_ap=mask_ap: e.tensor_tensor(out=pt_[:], in0=pt_[:], in1=mask_ap, op=ALU.mult), [ptb_, mask_b], [ptb_])
                    pend.append((h, kc, pt_, ptb_, ki, len(kcs)))
                    if len(pend) > LS:
                        flush_one()
            while pend:
                flush_one()
            for h in range(4):
                ot_, otb_ = osT[h % 2]
                S.act(lambda e, h=h, ot_=ot_: e.copy(out=ot_[:], in_=ACC[h][0][0:65, :]), [ACC[h][1]], [otb_])
                for j in range(4):
                    i = ctr["o"]
                    ctr["o"] += 1
                    fs_, fsb = fs[i % 2]
                    p, pb = PS()
                    S.pe(lambda e, p=p, ot_=ot_, j=j: e.transpose(out=p[:, 0:65], in_=ot_[:, j * 128:(j + 1) * 128], identity=ident[0:65, 0:65]),
                         [otb_, identb], [pb])
                    S.dve(lambda e, p=p, fs_=fs_: e.tensor_scalar(out=fs_[:], in0=p[:, 64:65], scalar1=1e-30, scalar2=None, op0=ALU.max), [pb], [fsb])
                    S.dve(lambda e, fs_=fs_: e.reciprocal(out=fs_[:], in_=fs_[:]), [fsb], [fsb])
                    gi = h * 3 + 1 + br
                    S.dve(lambda e, fs_=fs_, j=j, gi=gi: e.tensor_tensor(out=fs_[:], in0=fs_[:], in1=gts[:, j, gi:gi + 1], op=ALU.mult), [fsb, gtsb], [fsb])
                    dsl = oa[:, j, h * 64:(h + 1) * 64]
                    if br == 0:
                        S.pool(lambda e, j=j, h=h, dsl=dsl: e.tensor_scalar(out=dsl, in0=oc[:, j, h, :], scalar1=gts[:, j, h * 3:h * 3 + 1], scalar2=None,
                                                                           op0=ALU.mult), [ocb, gtsb, oab_], [oab_])
                    S.dve(lambda e, p=p, fs_=fs_, dsl=dsl: e.scalar_tensor_tensor(out=dsl, in0=p[:, 0:64], scalar=fs_[:, 0:1], in1=dsl,
                                                                                 op0=ALU.mult, op1=ALU.add), [pb, fsb, oab_], [oab_])
        for j in range(4):
            S.dma(lambda e, j=j, t0=t0: e.dma_start(out=oa_d[t0 + j * 128:t0 + (j + 1) * 128, ocol:ocol + 256], in_=oa[:, j, :]), [oab_], [oab])
    S.emit(stack)


def nsa_consts():
    c = {}
    half = 32
    inv = np.power(10000.0, -np.arange(half, dtype=np.float32) / half).astype(np.float32)
    pos = np.arange(SEQ, dtype=np.float32)
    ang = pos[None, :] * inv[:, None]
    cos, sin = np.cos(ang).astype(np.float32), np.sin(ang).astype(np.float32)
    cosC = np.concatenate([cos, cos], 0)
    sinS = np.concatenate([-sin, sin], 0)
    c["rope"] = np.stack([np.concatenate([cosC, cosC], 0), np.concatenate([sinS, sinS], 0)]).astype(np.float32)
    posk = (np.arange(512, dtype=np.float32) * 16 + 15.5)
    angk = posk[None, :] * inv[:, None]
    ck, sk = np.cos(angk).astype(np.float32), np.sin(angk).astype(np.float32)
    c["ropek"] = np.stack([np.concatenate([ck, ck], 0), np.concatenate([-sk, sk], 0)]).astype(np.float32)
    p = np.arange(128)
    E = np.zeros((128, 32, 128), np.float32)
    for v in range(32):
        for k in range(128):
            r = 2 * v + k // 64
            if r < 64:
                E[r, v, k] = 1.0
                E[64 + r, v, k] = 1.0
    c["Eexp"] = E
    q = np.arange(512)
    mk = np.zeros((8, 128, 512), np.float32)
    for i in range(8):
        kp = 128 * (i - 4) + p[:, None]
        rel = q[None, :] - kp
        mk[i] = ((rel >= 0) & (rel < 512)).astype(np.float32)
    c["masks"] = mk
    ql = np.arange(128)
    cm = np.zeros((17, 128, 128), np.float32)
    for mi in range(17):
        delta = mi * 8 - 128
        npr = p[:, None] + delta
        cm[mi] = (16 * npr + 31 <= ql[None, :]).astype(np.float32)
    c["cmask"] = cm
    n = np.arange(512)
    m = np.arange(128)
    ov = ((16 * n[:, None] <= 64 * m[None, :] + 63) & (16 * n[:, None] + 31 >= 64 * m[None, :])).astype(np.float32)
    ov[511] = 0.0
    c["overlap"] = ov
    W = np.full((128, 256), -1.0, np.float32)
    for qq in range(128):
        rels = (0, -1) if qq < 64 else (0, 1)
        for r in rels:
            W[qq, 128 + r] = 1e6
    c["forceW"] = W
    c["ident"] = np.eye(128, dtype=np.float32)
    return c


def nsa_inputs(inp, b, hkv, consts):
    w_in = inp["ev_w_in"][0]
    sw = lambda a: np.concatenate([a[..., 32:], a[..., :32]], -1)
    cols = []
    for g in range(4):
        h = hkv * 4 + g
        qh = w_in[:, h * 64:(h + 1) * 64]
        cols += [qh, qh, sw(qh), sw(qh)]
    wq = np.concatenate(cols, 1)
    kv = lambda i: w_in[:, 512 + i * 128 + hkv * 64: 512 + i * 128 + (hkv + 1) * 64]
    wk = np.concatenate([kv(0), kv(1), kv(2), kv(4), sw(kv(2)), sw(kv(4))], 1)
    wv = np.concatenate([kv(3), kv(5)], 1)
    og = 512 + 6 * 128
    wgt = w_in[:, og + hkv * 12: og + (hkv + 1) * 12]
    wkc = inp["ev_cmp_w_k"][0]
    wvc = inp["ev_cmp_w_v"][0]
    cmpwk = np.stack([wkc.transpose(1, 0, 2), sw(wkc).transpose(1, 0, 2)])
    cmpwv = np.concatenate([np.zeros((64, 32, 64), np.float32), wvc.transpose(1, 0, 2)], 0)
    pek = inp["ev_cmp_pe_k"][0].T
    pev = np.concatenate([np.zeros((64, 32), np.float32), inp["ev_cmp_pe_v"][0].T], 0)
    d = {"xb": np.ascontiguousarray(inp["x"][b]), "wq": np.ascontiguousarray(wq), "wk": np.ascontiguousarray(wk),
         "wv": np.ascontiguousarray(wv), "wgt": np.ascontiguousarray(wgt), "cmpwk": np.ascontiguousarray(cmpwk),
         "cmpwv": np.ascontiguousarray(cmpwv), "cmppek": np.ascontiguousarray(pek), "cmppev": np.ascontiguousarray(pev)}
    d.update(consts)
    return d


NSA_SHARED = {"rope": [2, 128, SEQ], "ropek": [2, 64, 512], "Eexp": [128, 32, 128], "masks": [8, 128, 512],
              "cmask": [17, 128, 128], "overlap": [512, 128], "forceW": [128, 256], "ident": [128, 128]}
I32 = mybir.dt.int32


def build_fused(nc, stack):
    from contextlib import ExitStack
    shared = {"stack": stack}
    xpad = nc.dram_tensor("xpad", [SEQ + 128, D], F32, kind="ExternalInput").ap()
    nonce = nc.dram_tensor("nonce", [1, 16], I32, kind="ExternalInput").ap()
    mixh = nc.dram_tensor("mixh", [2, SEQ + 128, 256], F32, kind="Internal").ap()
    shmix = nc.dram_tensor("shmix", [2, 2, SEQ + 128, 256], F32, kind="Internal", addr_space="Shared").ap()
    flag = nc.dram_tensor("shflag", [2, 16], I32, kind="Internal", addr_space="Shared").ap()
    io = {"xb": xpad[128:SEQ + 128, :], "gcst": nc.dram_tensor("gcst", [6, 128, 128], F32, kind="ExternalInput").ap()}
    for k, shp in NSA_SHARED.items():
        io[k] = nc.dram_tensor(k, shp, F32, kind="ExternalInput").ap()
    with ExitStack() as st:
        d = dict(io)
        d["o_a"] = mixh[0, 128:SEQ + 128, :]
        d["zero_rows"] = [(mixh[0, 0:128, :], xpad[0:128, 0:256]), (mixh[1, 0:128, :], xpad[0:128, 0:256])]
        build_nsa(nc, st, shared=shared, pfx="n_", io=d, ocol=0)
    with ExitStack() as st:
        d = {"xb": io["xb"], "gcst": io["gcst"], "o_b": mixh[1, 128:SEQ + 128, :]}
        build_gdn(nc, st, shared=shared, pfx="g_", io=d, ocol=0, NH=2)
    with ExitStack() as st:
        C = Ctx(nc, st, shared, "x_")
        S = C.S
        mb, sb_, fb, nb = Buf("mixh"), Buf("shmix"), Buf("flag"), Buf("nonce")
        S.dma(lambda e: e.dma_start(out=shmix[bass.ds(nc.partition_id() % 2, 1), :, :, :], in_=mixh), [mb], [sb_])
        S.dma(lambda e: e.dma_start(out=flag[bass.ds(nc.partition_id() % 2, 1), :], in_=nonce), [nb, sb_], [fb])

        def poll(e):
            with e.register("pf") as f, e.register("pn") as n, e.register("pd") as dd:
                e.reg_load(n, nonce[0:1, 0:1])
                other = flag[bass.ds(1 - nc.partition_id() % 2, 1), 0:1]
                e.reg_load(f, other)
                e.reg_sub(dd, f, n)
                with e.While(dd):
                    e.reg_load(f, other)
                    e.reg_sub(dd, f, n)
            return e.nop()
        S.add("sp", poll, [fb], [fb, sb_])
        S.emit(st)
    with ExitStack() as st:
        d = {"xpad": xpad, "shmix": shmix, "ident": io["ident"]}
        build_tail(nc, st, shared=shared, pfx="t_", io=d, dyn=True)


_PROGS = {}


def _prog(name, builder):
    if name not in _PROGS:
        from contextlib import ExitStack
        nc = bass.Bass("TRN2", target_bir_lowering=False)
        with ExitStack() as st:
            builder(nc, st)
        _PROGS[name] = nc
    return _PROGS[name]


def fused_inputs(inp, c, cs, A, lnp, nonce):
    b, r = c // 2, c % 2
    d = {"xpad": np.concatenate([np.zeros((128, D), np.float32), inp["x"][b]]), "gcst": gdn_consts(),
         "nonce": np.full((1, 16), nonce, np.int32)}
    d.update(cs)
    for k, v in nsa_inputs(inp, b, r, {}).items():
        if k != "xb":
            d["n_" + k] = v
    for k, v in gdn_inputs(inp, b, r).items():
        if k not in ("xb", "gcst"):
            d["g_" + k] = v
    Ac = A
    if r == 1:
        Ac = A.copy()
        Ac[0] = A[2]
        Ac[1] = A[3]
    t = {"w_out": inp["ev_w_out"][0], "lnp": lnp, "ffn_wg": inp["ev_ffn_wg"][0], "ffn_wu": inp["ev_ffn_wu"][0],
         "ffn_wd": inp["ev_ffn_wd"][0], "pool_w": inp["od_pool_w"][0], "router_w": inp["od_router_w"][0],
         "exp_wg": inp["od_exp_wg"][0], "exp_wu": inp["od_exp_wu"][0], "exp_wd": inp["od_exp_wd"][0], "apool": Ac}
    for k, v in t.items():
        d["t_" + k] = v
    return d


_CALLS = [0]


def kernel(**inp):
    inp = {k: np.asarray(v) for k, v in inp.items()}
    B = inp["x"].shape[0]
    cores = list(range(NCORES))
    cs = nsa_consts()
    A = pool_consts()
    lnp = np.stack([inp[k][0] for k in ["ev_ln1_g", "ev_ln1_b", "ev_ln2_g", "ev_ln2_b", "od_pool_scale",
                                        "od_ln1_g", "od_ln1_b", "od_ln2_g", "od_ln2_b"]])
    _CALLS[0] += 1
    nonce = (int.from_bytes(os.urandom(3), "little") << 4) + (_CALLS[0] % 16) + 1
    ims = [fused_inputs(inp, c, cs, A, lnp, nonce) for c in cores]
    res = run_bass_kernel_spmd(_prog("fused", build_fused), ims, core_ids=cores)
    out = np.stack([np.concatenate([res.results[2 * b]["t_out"], res.results[2 * b + 1]["t_out"]]) for b in range(B)])
    return out.astype(np.float32)
```

```python
import os
import numpy as np
import concourse.bass as bass
import concourse.mybir as mybir
from concourse.bass_utils import run_bass_kernel_spmd

F32 = mybir.dt.float32
BF16 = mybir.dt.bfloat16
AF = mybir.ActivationFunctionType
ALU = mybir.AluOpType
AX = mybir.AxisListType

NCORES = 8
D = 1024
ALPHA = float((2 * 2) ** 0.25)
LN_EPS = 1e-5


class Buf:
    __slots__ = ("name", "lw", "rs")

    def __init__(self, name):
        self.name = name
        self.lw = None
        self.rs = []


class Sched:
    NDMA = 24

    def __init__(self, nc, shared=None):
        self.nc = nc
        self.ops = []
        self.shared = shared

    def add(self, eng, fn, reads=(), writes=(), dma=False):
        i = len(self.ops)
        deps = set()
        for b in reads:
            if b.lw is not None:
                deps.add(b.lw)
        for b in writes:
            if b.lw is not None:
                deps.add(b.lw)
            deps.update(b.rs)
        for b in reads:
            b.rs.append(i)
        for b in writes:
            b.lw = i
            b.rs = []
        self.ops.append([eng, fn, deps, dma])
        return i

    def pe(self, fn, reads=(), writes=()):
        return self.add("pe", fn, reads, writes)

    def act(self, fn, reads=(), writes=()):
        return self.add("act", fn, reads, writes)

    def dve(self, fn, reads=(), writes=()):
        return self.add("dve", fn, reads, writes)

    def pool(self, fn, reads=(), writes=()):
        return self.add("pool", fn, reads, writes)

    def dma(self, fn, reads=(), writes=(), q="sp"):
        return self.add(q, fn, reads, writes, dma=True)

    def emit(self, stack):
        nc = self.nc
        ops = self.ops
        n = len(ops)
        engs = ["pe", "act", "dve", "pool", "sp"]
        has_dep = [False] * n
        red = []
        pos = [0] * n
        _c = {}
        for i, op in enumerate(ops):
            _c[op[0]] = _c.get(op[0], 0) + 1
            pos[i] = _c[op[0]]
        for i, (eng, fn, deps, dma) in enumerate(ops):
            latest = {}
            dl = []
            for j in deps:
                if ops[j][3]:
                    dl.append(j)
                else:
                    e = ops[j][0]
                    if e not in latest or latest[e] < j:
                        latest[e] = j
            for e, j in latest.items():
                if e == eng and not dma:
                    if eng == "pe":
                        continue
                    if eng in ("act", "dve") and pos[i] - pos[j] >= 2:
                        continue
                dl.append(j)
            red.append(dl)
            for j in dl:
                has_dep[j] = True
        last = {}
        for i, op in enumerate(ops):
            last[op[0]] = i
        for e, i in last.items():
            has_dep[i] = True
        sh = self.shared
        if sh is None:
            sh = {}
        if "sem_eng" not in sh:
            stack = sh.get("stack", stack)
            sh["sem_eng"] = {e: stack.enter_context(nc.semaphore("s_" + e)) for e in engs}
            sh["sem_dma"] = [stack.enter_context(nc.semaphore("s_dma%d" % k)) for k in range(self.NDMA)]
            sh["cnt"] = {e: 0 for e in engs}
            sh["kd"] = 0
            sh["dma_final"] = {}
        sem_eng, sem_dma = sh["sem_eng"], sh["sem_dma"]
        sig = [None] * n
        prevw = [None] * n
        cnt = dict(sh["cnt"])
        start_cnt = dict(sh["cnt"])
        kd = sh["kd"]
        for i, (eng, fn, deps, dma) in enumerate(ops):
            if dma:
                s = kd % self.NDMA
                g = kd // self.NDMA + 1
                sig[i] = (("d", s), 16 * g)
                if g > 1:
                    prevw[i] = (("d", s), 16 * (g - 1))
                kd += 1
            elif has_dep[i]:
                cnt[eng] += 1
                sig[i] = (("e", eng), cnt[eng])
        dma_final = dict(sh["dma_final"])
        start_dma = dict(sh["dma_final"])
        for i in range(n):
            if ops[i][3]:
                dma_final[sig[i][0]] = sig[i][1]
        sh["cnt"] = dict(cnt)
        sh["kd"] = kd
        sh["dma_final"] = dict(dma_final)

        def semh(key):
            return sem_dma[key[1]] if key[0] == "d" else sem_eng[key[1]]

        per_eng = {e: [] for e in engs}
        for i, op in enumerate(ops):
            per_eng[op[0]].append(i)

        def run_engine(ename, e):
            waited = {("e", en): v for en, v in start_cnt.items()}
            waited.update(start_dma)
            for i in per_eng[ename]:
                _, fn, _, dma = ops[i]
                need = {}
                for j in red[i]:
                    k, v = sig[j]
                    if need.get(k, 0) < v:
                        need[k] = v
                if prevw[i] is not None:
                    k, v = prevw[i]
                    if need.get(k, 0) < v:
                        need[k] = v
                for k, v in need.items():
                    if waited.get(k, 0) < v:
                        e.wait_ge(semh(k), v)
                        waited[k] = v
                ins = fn(e)
                if sig[i] is not None:
                    k, v = sig[i]
                    ins.then_inc(semh(k), 16 if dma else 1)
            for k, v in dma_final.items():
                if waited.get(k, 0) < v:
                    e.wait_ge(semh(k), v)
            for en in engs:
                if en != ename and cnt[en] > waited.get(("e", en), 0):
                    e.wait_ge(sem_eng[en], cnt[en])

        with nc.Block() as block:
            @block.tensor
            def _(e):
                run_engine("pe", e)

            @block.scalar
            def _(e):
                run_engine("act", e)

            @block.vector
            def _(e):
                run_engine("dve", e)

            @block.gpsimd
            def _(e):
                run_engine("pool", e)

            @block.sync
            def _(e):
                run_engine("sp", e)


class Ctx:
    def __init__(self, nc, stack, shared=None, pfx="", io=None):
        self.nc = nc
        self.stack = stack
        self.S = Sched(nc, shared)
        self.n = 0
        self.pfx = pfx
        self.io = io or {}

    def sb(self, shape, dt, name=None):
        self.n += 1
        name = "sb_" + self.pfx + (name or ("t%d" % self.n))
        t = self.stack.enter_context(self.nc.sbuf_tensor(name, list(shape), dt))
        return t, Buf(name)

    def ps(self, shape, dt, name=None):
        self.n += 1
        name = "ps_" + self.pfx + (name or ("p%d" % self.n))
        t = self.stack.enter_context(self.nc.psum_tensor(name, list(shape), dt))
        return t, Buf(name)

    def din(self, name, shape, dt=F32):
        if name in self.io:
            return self.io[name], Buf(name)
        return self.nc.dram_tensor(self.pfx + name, list(shape), dt, kind="ExternalInput").ap(), Buf(name)

    def dout(self, name, shape, dt=F32):
        if name in self.io:
            return self.io[name], Buf(name)
        return self.nc.dram_tensor(self.pfx + name, list(shape), dt, kind="ExternalOutput").ap(), Buf(name)

    def dscr(self, name, shape, dt=F32):
        return self.nc.dram_tensor(name, list(shape), dt, kind="Internal").ap(), Buf(name)


NT_TAIL = 33
DFF = 2816
DFE = 3584
NEXP = 8


def ln_tile(C, src, srcb, dst, dstb, gt, gtb, bt, btb, tmp):
    S = C.S
    st6, st6b, mv, mvb, rstd, rstdb = tmp
    for h in range(2):
        S.dve(lambda e, h=h: e.bn_stats(out=st6[:, h, :], in_=src[:, h * 512:(h + 1) * 512]), [srcb], [st6b])
    S.dve(lambda e: e.bn_aggr(out=mv[:], in_=st6[:].rearrange("p a b -> p (a b)")), [st6b], [mvb])
    S.act(lambda e: e.activation(out=rstd[:], in_=mv[:, 1:2], func=AF.Ln, bias=LN_EPS, scale=1.0), [mvb], [rstdb])
    S.act(lambda e: e.activation(out=rstd[:], in_=rstd[:], func=AF.Exp, scale=-0.5), [rstdb], [rstdb])
    S.dve(lambda e: e.tensor_scalar(out=dst, in0=src, scalar1=mv[:, 0:1], scalar2=rstd[:, 0:1],
                                    op0=ALU.subtract, op1=ALU.mult), [srcb, mvb, rstdb], [dstb])
    S.pool(lambda e: e.tensor_tensor(out=dst, in0=dst, in1=gt[:], op=ALU.mult), [dstb, gtb], [dstb])
    S.pool(lambda e: e.tensor_tensor(out=dst, in0=dst, in1=bt[:], op=ALU.add), [dstb, btb], [dstb])


def build_tail(nc, stack, stop=None, shared=None, pfx="", io=None, dyn=False):
    C = Ctx(nc, stack, shared, pfx, io)
    S = C.S
    NT = NT_TAIL
    if dyn:
        xpad, xpadb = C.din("xpad", [SEQ + 128, D])
        shmix, shmixb = C.din("shmix", [2, 2, SEQ + 128, 256])
        xin, xinb = C.dscr(pfx + "xts", [NT * 128, D])
        mxs, omixb = C.dscr(pfx + "mxs", [4, NT * 128, 256])
        omix = None

        def dyn_rows(ap):
            return ap[bass.ds((nc.partition_id() % 2) * 4096, NT * 128), :]
        S.dma(lambda e: e.dma_start(out=xin, in_=dyn_rows(xpad)), [xpadb], [xinb], q="act")
        for k in range(4):
            S.dma(lambda e, k=k: e.dma_start(out=mxs[k], in_=dyn_rows(shmix[k % 2, k // 2])), [shmixb], [omixb], q=("act" if k == 0 else "pool"))
    else:
        xin, xinb = C.din("xin", [NT * 128, D])
        omix, omixb = C.din("omix", [NT * 128, D])
    wout_d, woutb = C.din("w_out", [D, D])
    lnp_d, lnpb = C.din("lnp", [9, D])
    wg_d, wgb = C.din("ffn_wg", [D, DFF])
    wu_d, wub = C.din("ffn_wu", [D, DFF])
    wd_d, wdb = C.din("ffn_wd", [DFF, D])
    pw_d, pwb = C.din("pool_w", [4, 256, 256])
    rw_d, rwb = C.din("router_w", [D, NEXP])
    eg_d, egb = C.din("exp_wg", [NEXP, D, DFE])
    eu_d, eub = C.din("exp_wu", [NEXP, D, DFE])
    ed_d, edb = C.din("exp_wd", [NEXP, DFE, D])
    ap_d, apb = C.din("apool", [4, 4, 128, 128])
    id_d, idb = C.din("ident", [128, 128])
    x2s, x2sb = C.dout("x2s", [NT * 128, D])
    out_d, outb = C.dout("out", [(NT - 1) * 128, D])

    ident, identb = C.sb([128, 128], F32, "ident")
    S.dma(lambda e: e.dma_start(out=ident[:], in_=id_d), [idb], [identb])
    lnpt = [C.sb([128, D], F32, "lnp%d" % i) for i in range(5)]
    lnp = {}

    def load_lnp(rows):
        for j, i in enumerate(rows):
            t, b = lnpt[j]
            S.dma(lambda e, t=t, i=i: e.dma_start(out=t[:], in_=lnp_d[i:i + 1, :].to_broadcast([128, D])), [lnpb], [b])
            lnp[i] = (t, b)
    load_lnp([0, 1, 2, 3])
    wout, woutsb = C.sb([128, 8, D], BF16, "wout")
    S.dma(lambda e: e.dma_start(out=wout[:], in_=wout_d.rearrange("(k p) n -> p k n", p=128)), [woutb], [woutsb], q="pool")
    apool, apoolb = C.sb([128, 16, 128], F32, "apool")
    S.dma(lambda e: e.dma_start(out=apool[:], in_=ap_d.rearrange("a w p t -> p (a w) t")), [apb], [apoolb])
    poolw, poolwb = C.sb([128, 8, 256], F32, "poolw")
    S.dma(lambda e: e.dma_start(out=poolw[:], in_=pw_d.rearrange("g (k p) d -> p (g k) d", p=128)), [pwb], [poolwb])
    rw, rwsb = C.sb([128, 8, NEXP], F32, "rw")
    S.dma(lambda e: e.dma_start(out=rw[:], in_=rw_d.rearrange("(k p) n -> p k n", p=128)), [rwb], [rwsb])

    NP = 11
    yacc, yaccb = [], []
    for t in range(NP):
        a, b = C.sb([128, D], F32, "yacc%d" % t)
        yacc.append(a)
        yaccb.append(b)
    xT, xTb = C.sb([128, 8, NP * 128], BF16, "xT")
    gates, gatesb = C.sb([128, NP, NEXP], F32, "gates")
    hT = [C.sb([128, 4, NP * 128], BF16, "hT%d" % i) for i in range(1)]
    wgs = [C.sb([128, 8, 512], BF16, "wgs%d" % i) for i in range(2)]
    wus = [C.sb([128, 8, 512], BF16, "wus%d" % i) for i in range(2)]
    wds = [C.sb([128, 4, D], BF16, "wds%d" % i) for i in range(2)]
    sgt = [C.sb([128, 512], BF16, "sg%d" % i) for i in range(2)]
    NSL = 1
    ta = [C.sb([128, D], F32, "ta%d" % i) for i in range(NSL)]
    tb = [C.sb([128, D], F32, "tb%d" % i) for i in range(NSL)]
    tc_ = [C.sb([128, D], F32, "tc%d" % i) for i in range(NSL)]
    tTb = [C.sb([128, 8, 128], BF16, "tTb%d" % i) for i in range(NSL)]
    tTf = [C.sb([128, 8, 128], F32, "tTf%d" % i) for i in range(NSL)]
    lntmp = []
    for i in range(NSL):
        a = C.sb([128, 2, 6], F32)
        b = C.sb([128, 2], F32)
        c = C.sb([128, 1], F32)
        lntmp.append((a[0], a[1], b[0], b[1], c[0], c[1]))
    sm = [dict(mx=C.sb([128, 8], F32), e=C.sb([128, 8], F32), m=C.sb([128, 8], F32), d=C.sb([128, 1], F32),
               nb=C.sb([128, 1], F32), lg=C.sb([128, 8], F32)) for i in range(NSL)]
    psA = [C.ps([128, 512], F32, "psA%d" % i) for i in range(2)]
    psB = [C.ps([128, 512], F32, "psB%d" % i) for i in range(2)]
    psD = [C.ps([128, 512], F32, "psD%d" % i) for i in range(4)]
    cnt = {"t": 0, "g": 0, "ab": 0, "d": 0}

    def transpose_to(src, srcb, dst_bf=None, dst_bfb=None, dst_f=None, dst_fb=None, dst_bf_ap=None):
        for half in range(2):
            p, pb = psD[cnt["d"] % 4]
            cnt["d"] += 1
            for k in range(4):
                kk = half * 4 + k
                S.pe(lambda e, p=p, k=k, kk=kk: e.transpose(out=p[:, k * 128:(k + 1) * 128], in_=src[:, kk * 128:(kk + 1) * 128],
                                                           identity=ident[:]), [srcb, identb], [pb])
            if dst_bf_ap is not None:
                S.act(lambda e, p=p, half=half: e.copy(out=dst_bf_ap(half), in_=p[:].rearrange("p (a b) -> p a b", a=4)), [pb], [dst_bfb, pb])
            if dst_f is not None:
                S.dve(lambda e, p=p, half=half: e.tensor_copy(out=dst_f[:, half * 4:(half + 1) * 4, :],
                                                              in_=p[:].rearrange("p (a b) -> p a b", a=4)), [pb], [dst_fb])

    def swiglu_pass(ntl, wgd, wgdb, wud, wudb, wdd, wddb, nfc, ne, use_gates):
        ntok = ntl * 128
        blocks = [(s, min(512, ntok - s)) for s in range(0, ntok, 512)]
        for ex in range(ne):
            for c0 in range(0, nfc, 4):
                gc = min(4, nfc - c0)
                slot = cnt["g"] % 2
                cnt["g"] += 1
                wg_t, wg_b = wgs[slot]
                wu_t, wu_b = wus[slot]
                wd_t, wd_b = wds[slot]
                h_t, h_b = hT[0]
                if ne > 1:
                    sg_, su_, sd_ = wgd[ex], wud[ex], wdd[ex]
                else:
                    sg_, su_, sd_ = wgd, wud, wdd
                S.dma(lambda e, wg_t=wg_t, sg_=sg_, c0=c0, gc=gc: e.dma_start(
                    out=wg_t[:, :, 0:gc * 128], in_=sg_[:, c0 * 128:(c0 + gc) * 128].rearrange("(k p) f -> p k f", p=128)),
                    [wgdb], [wg_b], q="pool")
                S.dma(lambda e, wu_t=wu_t, su_=su_, c0=c0, gc=gc: e.dma_start(
                    out=wu_t[:, :, 0:gc * 128], in_=su_[:, c0 * 128:(c0 + gc) * 128].rearrange("(k p) f -> p k f", p=128)),
                    [wudb], [wu_b], q="pool")
                S.dma(lambda e, wd_t=wd_t, sd_=sd_, c0=c0, gc=gc: e.dma_start(
                    out=wd_t[:, 0:gc, :], in_=sd_[c0 * 128:(c0 + gc) * 128, :].rearrange("(c p) d -> p c d", p=128)),
                    [wddb], [wd_b], q="pool")
                for (s0, bw) in blocks:
                    for c in range(gc):
                        ab = cnt["ab"] % 2
                        cnt["ab"] += 1
                        pa, pab = psA[ab]
                        pb_, pbb = psB[ab]
                        sgx, sgb = sgt[ab]
                        for k in range(8):
                            S.pe(lambda e, pa=pa, k=k, c=c, s0=s0, bw=bw, wg_t=wg_t: e.matmul(
                                pa[:, 0:bw], lhsT=wg_t[:, k, c * 128:(c + 1) * 128], rhs=xT[:, k, s0:s0 + bw],
                                start=(k == 0), stop=(k == 7)), [wg_b, xTb], [pab])
                        for k in range(8):
                            S.pe(lambda e, pb_=pb_, k=k, c=c, s0=s0, bw=bw, wu_t=wu_t: e.matmul(
                                pb_[:, 0:bw], lhsT=wu_t[:, k, c * 128:(c + 1) * 128], rhs=xT[:, k, s0:s0 + bw],
                                start=(k == 0), stop=(k == 7)), [wu_b, xTb], [pbb])
                        S.act(lambda e, pa=pa, sgx=sgx, bw=bw: e.activation(out=sgx[:, 0:bw], in_=pa[:, 0:bw], func=AF.Silu),
                              [pab], [sgb])
                        S.dve(lambda e, sgx=sgx, pb_=pb_, h_t=h_t, c=c, s0=s0, bw=bw: e.tensor_tensor(
                            out=h_t[:, c, s0:s0 + bw], in0=sgx[:, 0:bw], in1=pb_[:, 0:bw], op=ALU.mult), [sgb, pbb], [h_b])
                for t in range(ntl):
                    for half in range(2):
                        p, pb2 = psD[cnt["d"] % 4]
                        cnt["d"] += 1
                        for c in range(gc):
                            S.pe(lambda e, p=p, c=c, t=t, half=half, h_t=h_t, wd_t=wd_t, gc=gc: e.matmul(
                                p[:], lhsT=h_t[:, c, t * 128:(t + 1) * 128], rhs=wd_t[:, c, half * 512:(half + 1) * 512],
                                start=(c == 0), stop=(c == gc - 1)), [h_b, wd_b], [pb2])
                        ya = yacc[t]
                        if use_gates:
                            S.dve(lambda e, p=p, ya=ya, t=t, ex=ex, half=half: e.scalar_tensor_tensor(
                                out=ya[:, half * 512:(half + 1) * 512], in0=p[:], scalar=gates[:, t, ex:ex + 1],
                                in1=ya[:, half * 512:(half + 1) * 512], op0=ALU.mult, op1=ALU.add),
                                [pb2, gatesb, yaccb[t]], [yaccb[t]])
                        else:
                            S.dve(lambda e, p=p, ya=ya, half=half: e.tensor_tensor(
                                out=ya[:, half * 512:(half + 1) * 512], in0=p[:], in1=ya[:, half * 512:(half + 1) * 512],
                                op=ALU.add), [pb2, yaccb[t]], [yaccb[t]])

    partsA = [(s, min(NP, NT - s)) for s in range(0, NT, NP)]
    for (t0, ntl) in partsA:
        for tl in range(ntl):
            ti = t0 + tl
            sl = cnt["t"] % NSL
            cnt["t"] += 1
            (om, omb), (xt_, xtb_), (xn, xnb) = ta[sl], tb[sl], tc_[sl]
            oT, oTb = tTb[sl]
            if dyn:
                for k in range(4):
                    S.dma(lambda e, om=om, ti=ti, k=k: e.dma_start(out=om[:, k * 256:(k + 1) * 256], in_=mxs[k, ti * 128:(ti + 1) * 128, :]), [omixb], [omb])
            else:
                S.dma(lambda e, om=om, ti=ti: e.dma_start(out=om[:], in_=omix[ti * 128:(ti + 1) * 128, :]), [omixb], [omb])
            S.dma(lambda e, xt_=xt_, ti=ti: e.dma_start(out=xt_[:], in_=xin[ti * 128:(ti + 1) * 128, :]), [xinb], [xtb_])
            transpose_to(om, omb, dst_bfb=oTb, dst_bf_ap=lambda half, oT=oT: oT[:, half * 4:(half + 1) * 4, :])
            for half in range(2):
                p, pb2 = psD[cnt["d"] % 4]
                cnt["d"] += 1
                for k in range(8):
                    S.pe(lambda e, p=p, k=k, half=half, oT=oT: e.matmul(p[:], lhsT=oT[:, k, :], rhs=wout[:, k, half * 512:(half + 1) * 512],
                                                                        start=(k == 0), stop=(k == 7)), [oTb, woutsb], [pb2])
                S.dve(lambda e, p=p, half=half, xt_=xt_: e.scalar_tensor_tensor(
                    out=xt_[:, half * 512:(half + 1) * 512], in0=xt_[:, half * 512:(half + 1) * 512], scalar=ALPHA, in1=p[:],
                    op0=ALU.mult, op1=ALU.add), [pb2, xtb_], [xtb_])
            ln_tile(C, xt_[:], xtb_, xn[:], xnb, lnp[0][0], lnp[0][1], lnp[1][0], lnp[1][1], lntmp[sl])
            S.act(lambda e, xn=xn, tl=tl: e.mul(out=yacc[tl][:], in_=xn[:], mul=ALPHA), [xnb], [yaccb[tl]])
            transpose_to(xn, xnb, dst_bfb=xTb, dst_bf_ap=lambda half, tl=tl: xT[:, half * 4:(half + 1) * 4, tl * 128:(tl + 1) * 128])
        if stop == "A1":
            S.emit(stack)
            return
        swiglu_pass(ntl, wg_d, wgb, wu_d, wub, wd_d, wdb, DFF // 128, 1, False)
        if stop == "A2":
            S.emit(stack)
            return
        for tl in range(ntl):
            ti = t0 + tl
            sl = cnt["t"] % NSL
            cnt["t"] += 1
            xn, xnb = tc_[sl]
            ln_tile(C, yacc[tl][:], yaccb[tl], xn[:], xnb, lnp[2][0], lnp[2][1], lnp[3][0], lnp[3][1], lntmp[sl])
            S.dma(lambda e, xn=xn, ti=ti: e.dma_start(out=x2s[ti * 128:(ti + 1) * 128, :], in_=xn[:]), [xnb], [x2sb])

    if stop == "A":
        S.emit(stack)
        return
    partsB = [(s, min(NP, NT - s)) for s in range(1, NT, NP)]
    load_lnp([4, 5, 6, 7, 8])
    pmT = [C.sb([128, 8, 128], F32, "pmT%d" % i) for i in range(NSL)]
    for (t0, ntl) in partsB:
        for tl in range(ntl):
            ti = t0 + tl
            sl = cnt["t"] % NSL
            cnt["t"] += 1
            (xc, xcb), (xp, xpb), (xn, xnb) = ta[sl], tb[sl], tc_[sl]
            pm, pmb = pmT[sl]
            xf, xfb = tTf[sl]
            S.dma(lambda e, xc=xc, ti=ti: e.dma_start(out=xc[:], in_=x2s[ti * 128:(ti + 1) * 128, :]), [x2sb], [xcb])
            S.dma(lambda e, xp=xp, ti=ti: e.dma_start(out=xp[:], in_=x2s[(ti - 1) * 128:ti * 128, :]), [x2sb], [xpb])
            first = (ti == 1)
            kp, kc = (0, 1) if first else (2, 3)
            for half in range(2):
                p, pb2 = psD[cnt["d"] % 4]
                cnt["d"] += 1
                for k in range(4):
                    kk = half * 4 + k
                    gi = kk // 2
                    S.pe(lambda e, p=p, k=k, kk=kk, gi=gi, xp=xp, kp=kp: e.matmul(
                        p[:, k * 128:(k + 1) * 128], lhsT=xp[:, kk * 128:(kk + 1) * 128], rhs=apool[:, kp * 4 + gi, :],
                        start=True, stop=False), [xpb, apoolb], [pb2])
                    S.pe(lambda e, p=p, k=k, kk=kk, gi=gi, xc=xc, kc=kc: e.matmul(
                        p[:, k * 128:(k + 1) * 128], lhsT=xc[:, kk * 128:(kk + 1) * 128], rhs=apool[:, kc * 4 + gi, :],
                        start=False, stop=True), [xcb, apoolb], [pb2])
                S.act(lambda e, p=p, half=half, pm=pm: e.copy(out=pm[:, half * 4:(half + 1) * 4, :],
                                                             in_=p[:].rearrange("p (a b) -> p a b", a=4)), [pb2], [pmb])
            for half in range(2):
                p, pb2 = psD[cnt["d"] % 4]
                cnt["d"] += 1
                for g2 in range(2):
                    gi = half * 2 + g2
                    for k in range(2):
                        S.pe(lambda e, p=p, g2=g2, gi=gi, k=k, pm=pm: e.matmul(
                            p[:, g2 * 256:(g2 + 1) * 256], lhsT=pm[:, gi * 2 + k, :], rhs=poolw[:, gi * 2 + k, :],
                            start=(k == 0), stop=(k == 1)), [pmb, poolwb], [pb2])
                S.dve(lambda e, p=p, half=half, xp=xp: e.tensor_tensor(
                    out=xp[:, half * 512:(half + 1) * 512], in0=p[:], in1=lnp[4][0][:, half * 512:(half + 1) * 512], op=ALU.mult),
                    [pb2, lnp[4][1]], [xpb])
            S.dve(lambda e, xc=xc, xp=xp: e.scalar_tensor_tensor(out=xc[:], in0=xc[:], scalar=ALPHA, in1=xp[:],
                                                                 op0=ALU.mult, op1=ALU.add), [xcb, xpb], [xcb])
            ln_tile(C, xc[:], xcb, xn[:], xnb, lnp[5][0], lnp[5][1], lnp[6][0], lnp[6][1], lntmp[sl])
            S.act(lambda e, xn=xn, tl=tl: e.mul(out=yacc[tl][:], in_=xn[:], mul=ALPHA), [xnb], [yaccb[tl]])
            transpose_to(xn, xnb, dst_bfb=xTb, dst_bf_ap=lambda half, tl=tl: xT[:, half * 4:(half + 1) * 4, tl * 128:(tl + 1) * 128],
                         dst_f=xf, dst_fb=xfb)
            p, pb2 = psD[cnt["d"] % 4]
            cnt["d"] += 1
            for k in range(8):
                S.pe(lambda e, p=p, k=k, xf=xf: e.matmul(p[:, 0:NEXP], lhsT=xf[:, k, :], rhs=rw[:, k, :], start=(k == 0), stop=(k == 7)),
                     [xfb, rwsb], [pb2])
            q = sm[sl]
            lg, lgb = q["lg"]
            mx, mxb = q["mx"]
            ee, eeb = q["e"]
            mm, mmb = q["m"]
            dd, ddb = q["d"]
            nb, nbb = q["nb"]
            S.dve(lambda e, p=p, lg=lg: e.tensor_copy(out=lg[:], in_=p[:, 0:NEXP]), [pb2], [lgb])
            S.dve(lambda e, lg=lg, mx=mx: e.max(out=mx[:], in_=lg[:]), [lgb], [mxb])
            S.dve(lambda e, nb=nb, mx=mx: e.tensor_scalar(out=nb[:], in0=mx[:, 0:1], scalar1=-1.0, scalar2=None, op0=ALU.mult), [mxb], [nbb])
            S.act(lambda e, ee=ee, lg=lg, nb=nb: e.activation(out=ee[:], in_=lg[:], func=AF.Exp, bias=nb[:, 0:1], scale=1.0), [lgb, nbb], [eeb])
            S.act(lambda e, dd=dd, mx=mx, nb=nb: e.activation(out=dd[:], in_=mx[:, 1:2], func=AF.Exp, bias=nb[:, 0:1], scale=1.0), [mxb, nbb], [ddb])
            S.dve(lambda e, dd=dd: e.tensor_scalar(out=dd[:], in0=dd[:], scalar1=1.0, scalar2=None, op0=ALU.add), [ddb], [ddb])
            S.dve(lambda e, dd=dd: e.reciprocal(out=dd[:], in_=dd[:]), [ddb], [ddb])
            S.dve(lambda e, mm=mm, lg=lg, mx=mx: e.tensor_scalar(out=mm[:], in0=lg[:], scalar1=mx[:, 1:2], scalar2=None, op0=ALU.is_ge), [lgb, mxb], [mmb])
            S.dve(lambda e, mm=mm, ee=ee: e.tensor_tensor(out=mm[:], in0=mm[:], in1=ee[:], op=ALU.mult), [mmb, eeb], [mmb])
            S.dve(lambda e, mm=mm, dd=dd, tl=tl: e.tensor_scalar(out=gates[:, tl, :], in0=mm[:], scalar1=dd[:, 0:1], scalar2=None, op0=ALU.mult),
                  [mmb, ddb], [gatesb])
        if stop == "B1":
            S.emit(stack)
            return
        swiglu_pass(ntl, eg_d, egb, eu_d, eub, ed_d, edb, DFE // 128, NEXP, True)
        for tl in range(ntl):
            ti = t0 + tl
            sl = cnt["t"] % NSL
            cnt["t"] += 1
            xn, xnb = tc_[sl]
            ln_tile(C, yacc[tl][:], yaccb[tl], xn[:], xnb, lnp[7][0], lnp[7][1], lnp[8][0], lnp[8][1], lntmp[sl])
            S.dma(lambda e, xn=xn, ti=ti: e.dma_start(out=out_d[(ti - 1) * 128:ti * 128, :], in_=xn[:]), [xnb], [outb])
    S.emit(stack)


def pool_consts():
    A = np.zeros((4, 4, 128, 128), np.float32)
    for wi, w in enumerate((2, 4, 8, 16)):
        for t in range(128):
            for j in range(w):
                tp = t - j
                if tp >= 0:
                    A[3, wi, tp, t] += 1.0 / w
                else:
                    A[2, wi, 128 + tp, t] += 1.0 / w
            A[3, wi, t, t] -= 1.0
            c = min(t + 1, w)
            for j in range(c):
                A[1, wi, t - j, t] += 1.0 / c
            A[1, wi, t, t] -= 1.0
    return A


SEQ = 8192
GD = 128
CH = 64


class PsumRing:
    def __init__(self, C, n=8):
        self.t = [C.ps([128, 512], F32, "bank%d" % i) for i in range(n)]
        self.i = 0

    def __call__(self):
        r = self.t[self.i % len(self.t)]
        self.i += 1
        return r


def load_xT_block(C, PS, xb_d, xbb, t0, ident, identb, xtile, xT, xTb):
    S = C.S
    for j in range(4):
        xt_, xtb_ = xtile[j % len(xtile)]
        S.dma(lambda e, xt_=xt_, j=j: e.dma_start(out=xt_[:], in_=xb_d[t0 + j * 128:t0 + (j + 1) * 128, :]), [xbb], [xtb_])
        for half in range(2):
            p, pb = PS()
            for k in range(4):
                kk = half * 4 + k
                S.pe(lambda e, p=p, k=k, kk=kk, xt_=xt_: e.transpose(out=p[:, k * 128:(k + 1) * 128], in_=xt_[:, kk * 128:(kk + 1) * 128],
                                                                     identity=ident[:]), [xtb_, identb], [pb])
            S.act(lambda e, p=p, half=half, j=j: e.copy(out=xT[:, half * 4:(half + 1) * 4, j * 128:(j + 1) * 128],
                                                        in_=p[:].rearrange("p (a b) -> p a b", a=4)), [pb], [xTb])


def build_gdn(nc, stack, nblk=SEQ // 512, shared=None, pfx="", io=None, ocol=0, NH=2):
    C = Ctx(nc, stack, shared, pfx, io)
    S = C.S
    PS = PsumRing(C)
    NC3 = 3 * NH
    xb_d, xbb = C.din("xb", [SEQ, D])
    wqkv_d, wqkvb = C.din("wqkv", [D, NC3 * 128])
    wz_d, wzb = C.din("wz", [D, NH * 128])
    wbg_d, wbgb = C.din("wbg", [D, 2 * NH])
    convw_d, convwb = C.din("convw", [NC3 * 128, 4])
    hp_d, hpb = C.din("hparm", [1, 2 * NH])
    nw_d, nwb = C.din("normw", [1, 128])
    cst_d, cstb = C.din("gcst", [6, 128, 128])
    ob_d, obb = C.dout("o_b", [SEQ, NH * 128])

    cst, cstsb = C.sb([128, 6, 128], F32, "cst")
    S.dma(lambda e: e.dma_start(out=cst[:], in_=cst_d.rearrange("a p t -> p a t")), [cstb], [cstsb])
    ident = cst[:, 0, :]
    identb = cstsb
    tri = cst[0:64, 1, 0:64]
    ones = cst[:, 2, :]
    mneg = cst[0:64, 3, 0:64]
    negst = cst[0:64, 4, 0:64]
    id64 = cst[0:64, 0, 0:64]
    wqkv, wqkvsb = C.sb([128, 8, NC3 * 128], BF16, "wqkv")
    S.dma(lambda e: e.dma_start(out=wqkv[:], in_=wqkv_d.rearrange("(k p) n -> p k n", p=128)), [wqkvb], [wqkvsb], q="pool")
    wz, wzsb = C.sb([128, 8, NH * 128], BF16, "wz")
    S.dma(lambda e: e.dma_start(out=wz[:], in_=wz_d.rearrange("(k p) n -> p k n", p=128)), [wzb], [wzsb], q="pool")
    wbg, wbgsb = C.sb([128, 8, 2 * NH], BF16, "wbg")
    S.dma(lambda e: e.dma_start(out=wbg[:], in_=wbg_d.rearrange("(k p) n -> p k n", p=128)), [wbgb], [wbgsb], q="pool")
    convw, convwsb = C.sb([128, NC3, 4], F32, "convw")
    S.dma(lambda e: e.dma_start(out=convw[:], in_=convw_d.rearrange("(c p) j -> p c j", p=128)), [convwb], [convwsb])
    hp, hpsb = C.sb([64, 2 * NH], F32, "hp")
    S.dma(lambda e: e.dma_start(out=hp[:], in_=hp_d.to_broadcast([64, 2 * NH])), [hpb], [hpsb])
    nw, nwsb = C.sb([64, 128], F32, "nw")
    S.dma(lambda e: e.dma_start(out=nw[:], in_=nw_d.to_broadcast([64, 128])), [nwb], [nwsb])
    nalog, nalogb = C.sb([64, NH], F32, "nalog")
    S.act(lambda e: e.activation(out=nalog[:], in_=hp[:, 0:NH], func=AF.Exp), [hpsb], [nalogb])
    S.dve(lambda e: e.tensor_scalar(out=nalog[:], in0=nalog[:], scalar1=-1.0, scalar2=None, op0=ALU.mult), [nalogb], [nalogb])

    xtile = [C.sb([128, D], F32, "xtile%d" % i) for i in range(2)]
    xT, xTb = C.sb([128, 8, 512], BF16, "xT")
    cin, cinb = C.sb([128, NC3, 515], F32, "cin")
    S.pool(lambda e: e.memset(cin[:].rearrange("p a b -> p (a b)"), 0.0), [], [cinb])
    cacc = [C.sb([128, 512], F32, "cacc%d" % i) for i in range(2)]
    qkv = [C.sb([128, 512], F32, "qkv%d" % i) for i in range(NC3)]
    sq = [C.sb([128, 512], F32, "sq%d" % i) for i in range(2)]
    rn = [C.sb([128, 512], F32, "rn%d" % i) for i in range(2)]
    zs, zsb = C.sb([64, 8, NH * 128], F32, "zs")
    bg, bgb = C.sb([64, 8, 2 * NH], F32, "bg")
    beta, betab = C.sb([64, 8, NH], F32, "beta")
    gg, ggb = C.sb([64, 8, NH], F32, "gg")
    gc, gcb = C.sb([64, 8, NH], F32, "gc")
    ngc, ngcb = C.sb([64, 8, NH], F32, "ngc")
    egc, egcb = C.sb([64, 8, NH], F32, "egc")
    negc, negcb = C.sb([64, 8, NH], F32, "negc")
    eglg, eglgb = C.sb([64, 8, NH], F32, "eglg")
    egl, eglb = C.sb([128, 8, NH], F32, "egl")
    St = [C.sb([128, 128], F32, "state%d" % h) for h in range(NH)]
    for h in range(NH):
        S.pool(lambda e, h=h: e.memset(St[h][0][:], 0.0), [], [St[h][1]])
    CG = 2
    NCH = 2 * CG * NH

    def ring(shape, name, n):
        return [C.sb(shape, F32, "%s%d" % (name, i)) for i in range(n)]
    NT_ = CG * NH
    ktok = ring([64, 128], "ktok", NT_)
    dgc = ring([64, 64], "dgc", NT_)
    DT = ring([64, 64], "DT", NT_)
    X = ring([64, 64], "X", NT_)
    XT = ring([64, 64], "XT", NT_)
    G1s = ring([64, 64], "G1s", NT_)
    G2s = ring([64, 64], "G2s", NT_)
    vtok = ring([64, 128], "vtok", NCH)
    kpp = ring([64, 128], "kpp", NCH)
    Y = ring([64, 64], "Y", NCH)
    IT = ring([64, 64], "IT", NCH)
    NS_ = NH
    Rr = ring([64, 128], "R", NS_)
    vn = ring([64, 128], "vn", NS_)
    ivt = ring([64, 128], "ivt", NS_)
    ot = ring([64, 128], "ot", NS_)
    ss = ring([64, 1], "ss", NS_)
    osq = ring([64, 128], "osq", NS_)
    ofin = ring([64, NH * 128], "ofin", 2)
    tctr = {"i": 0}

    def drive(gens):
        gens = list(gens)
        while gens:
            for g in list(gens):
                try:
                    next(g)
                except StopIteration:
                    gens.remove(g)

    for blk in range(nblk):
        t0 = blk * 512
        load_xT_block(C, PS, xb_d, xbb, t0, ident, identb, xtile, xT, xTb)
        for c in range(NC3):
            p, pb = PS()
            for k in range(8):
                S.pe(lambda e, p=p, k=k, c=c: e.matmul(p[:], lhsT=wqkv[:, k, c * 128:(c + 1) * 128], rhs=xT[:, k, :],
                                                       start=(k == 0), stop=(k == 7)), [wqkvsb, xTb], [pb])
            S.act(lambda e, p=p, c=c: e.copy(out=cin[:, c, 3:515], in_=p[:]), [pb], [cinb])
        for c in range(NC3):
            ca, cab = cacc[c % 2]
            eng = S.dve
            eng(lambda e, ca=ca, c=c: e.tensor_scalar(out=ca[:], in0=cin[:, c, 0:512], scalar1=convw[:, c, 0:1], scalar2=None, op0=ALU.mult),
                [cinb, convwsb], [cab])
            for j in range(1, 4):
                eng(lambda e, ca=ca, c=c, j=j: e.scalar_tensor_tensor(out=ca[:], in0=cin[:, c, j:j + 512], scalar=convw[:, c, j:j + 1], in1=ca[:],
                                                                       op0=ALU.mult, op1=ALU.add), [cinb, convwsb, cab], [cab])
            S.act(lambda e, ca=ca, c=c: e.activation(out=qkv[c][0][:], in_=ca[:], func=AF.Silu), [cab], [qkv[c][1]])
        S.pool(lambda e: e.tensor_copy(out=cin[:, :, 0:3], in_=cin[:, :, 512:515]), [cinb], [cinb])
        for c in range(2 * NH):
            sq_, sqb = sq[c % 2]
            rn_, rnb = rn[c % 2]
            S.pool(lambda e, sq_=sq_, c=c: e.tensor_tensor(out=sq_[:], in0=qkv[c][0][:], in1=qkv[c][0][:], op=ALU.mult), [qkv[c][1]], [sqb])
            p, pb = PS()
            S.pe(lambda e, p=p, sq_=sq_: e.matmul(p[:], lhsT=ones, rhs=sq_[:], start=True, stop=True), [sqb, cstsb], [pb])
            S.act(lambda e, p=p, rn_=rn_: e.activation(out=rn_[:], in_=p[:], func=AF.Ln, bias=1e-6, scale=1.0), [pb], [rnb])
            S.act(lambda e, rn_=rn_: e.activation(out=rn_[:], in_=rn_[:], func=AF.Exp, scale=-0.5), [rnb], [rnb])
            sc = float(GD ** -0.5) if c < NH else 1.0
            S.dve(lambda e, rn_=rn_, c=c, sc=sc: e.scalar_tensor_tensor(out=qkv[c][0][:], in0=qkv[c][0][:], scalar=sc, in1=rn_[:],
                                                                        op0=ALU.mult, op1=ALU.mult), [qkv[c][1], rnb], [qkv[c][1]])
        for cc in range(8):
            p, pb = PS()
            for k in range(8):
                S.pe(lambda e, p=p, k=k, cc=cc: e.matmul(p[0:64, 0:NH * 128], lhsT=xT[:, k, cc * 64:(cc + 1) * 64], rhs=wz[:, k, :],
                                                         start=(k == 0), stop=(k == 7)), [xTb, wzsb], [pb])
            S.act(lambda e, p=p, cc=cc: e.activation(out=zs[:, cc, :], in_=p[0:64, 0:NH * 128], func=AF.Silu), [pb], [zsb])
            p2, pb2 = PS()
            for k in range(8):
                S.pe(lambda e, p2=p2, k=k, cc=cc: e.matmul(p2[0:64, 0:2 * NH], lhsT=xT[:, k, cc * 64:(cc + 1) * 64], rhs=wbg[:, k, :],
                                                           start=(k == 0), stop=(k == 7)), [xTb, wbgsb], [pb2])
            S.dve(lambda e, p2=p2, cc=cc: e.tensor_copy(out=bg[:, cc, :], in_=p2[0:64, 0:2 * NH]), [pb2], [bgb])
        S.act(lambda e: e.activation(out=beta[:], in_=bg[:, :, 0:NH], func=AF.Sigmoid), [bgb], [betab])
        for h in range(NH):
            S.act(lambda e, h=h: e.activation(out=gg[:, :, h], in_=bg[:, :, NH + h], func=AF.Exp, bias=hp[:, NH + h:NH + h + 1], scale=1.0),
                  [bgb, hpsb], [ggb])
        S.act(lambda e: e.activation(out=gg[:], in_=gg[:], func=AF.Ln, bias=1.0, scale=1.0), [ggb], [ggb])
        for h in range(NH):
            S.dve(lambda e, h=h: e.tensor_scalar(out=gg[:, :, h], in0=gg[:, :, h], scalar1=nalog[:, h:h + 1], scalar2=None, op0=ALU.mult),
                  [ggb, nalogb], [ggb])
        ggf = gg[:].rearrange("p a b -> p (a b)")
        NG = 8 * NH
        p, pb = PS()
        S.pe(lambda e, p=p: e.matmul(p[0:64, 0:NG], lhsT=tri, rhs=ggf, start=True, stop=True), [ggb, cstsb], [pb])
        S.dve(lambda e, p=p: e.tensor_copy(out=gc[:].rearrange("p a b -> p (a b)"), in_=p[0:64, 0:NG]), [pb], [gcb])
        S.dve(lambda e: e.tensor_scalar(out=ngc[:], in0=gc[:], scalar1=-1.0, scalar2=None, op0=ALU.mult), [gcb], [ngcb])
        S.act(lambda e: e.activation(out=egc[:], in_=gc[:], func=AF.Exp), [gcb], [egcb])
        S.dve(lambda e: e.tensor_scalar(out=negc[:], in0=egc[:], scalar1=-1.0, scalar2=None, op0=ALU.mult), [egcb], [negcb])
        p, pb = PS()
        S.pe(lambda e, p=p: e.matmul(p[:, 0:NG], lhsT=ones[0:64, :], rhs=ggf, start=True, stop=True), [ggb, cstsb], [pb])
        S.act(lambda e, p=p: e.activation(out=egl[:].rearrange("p a b -> p (a b)"), in_=p[:, 0:NG], func=AF.Exp), [pb], [eglb, pb])
        S.dve(lambda e, p=p: e.tensor_tensor(out=eglg[:].rearrange("p a b -> p (a b)"), in0=p[0:64, 0:NG],
                                             in1=gc[:].rearrange("p a b -> p (a b)"), op=ALU.subtract), [pb, gcb], [eglgb])
        S.act(lambda e: e.activation(out=eglg[:], in_=eglg[:], func=AF.Exp), [eglgb], [eglgb])

        def pre(cc, h):
            csl = slice(cc * 64, (cc + 1) * 64)
            ch = (cc % (2 * CG)) * NH + h
            r = tctr["i"]
            tctr["i"] += 1
            qT, qTb = qkv[h]
            kT, kTb = qkv[NH + h]
            vT, vTb = qkv[2 * NH + h]
            kt, ktb = ktok[r % NT_]
            dg, dgb = dgc[r % NT_]
            dt_, dtb = DT[r % NT_]
            x_, xb_ = X[r % NT_]
            xt_, xtb_ = XT[r % NT_]
            g1, g1b = G1s[r % NT_]
            g2, g2b = G2s[r % NT_]
            vt, vtb = vtok[ch]
            kp_, kpb = kpp[ch]
            y_, yb_ = Y[ch]
            it_, itb = IT[ch]
            bcol = beta[:, cc, h:h + 1]
            p, pb = PS()
            S.pe(lambda e: e.transpose(out=p[0:64, 0:128], in_=kT[:, csl], identity=ident), [kTb, cstsb], [pb])
            S.dve(lambda e: e.tensor_copy(out=kt[:], in_=p[0:64, 0:128]), [pb], [ktb])
            p2, pb2 = PS()
            S.pe(lambda e: e.transpose(out=p2[0:64, 0:128], in_=vT[:, csl], identity=ident), [vTb, cstsb], [pb2])
            S.act(lambda e: e.copy(out=vt[:], in_=p2[0:64, 0:128]), [pb2], [vtb])
            S.pool(lambda e: e.tensor_scalar(out=dg[:], in0=id64, scalar1=gc[:, cc, h:h + 1], scalar2=None, op0=ALU.mult), [gcb, cstsb], [dgb])
            yield
            p3, pb3 = PS()
            S.pe(lambda e: e.matmul(p3[0:64, 0:64], lhsT=ones[0:64, 0:64], rhs=dg[:], start=True, stop=False), [dgb, cstsb], [pb3])
            S.pe(lambda e: e.matmul(p3[0:64, 0:64], lhsT=id64, rhs=mneg, start=False, stop=True), [cstsb], [pb3])
            S.act(lambda e: e.activation(out=dt_[:], in_=p3[0:64, 0:64], func=AF.Exp, bias=ngc[:, cc, h:h + 1], scale=1.0), [pb3, ngcb], [dtb])
            p4, pb4 = PS()
            S.pe(lambda e: e.matmul(p4[0:64, 0:64], lhsT=kT[:, csl], rhs=kT[:, csl], start=True, stop=True), [kTb], [pb4])
            S.dve(lambda e: e.tensor_scalar(out=g1[:], in0=p4[0:64, 0:64], scalar1=bcol, scalar2=None, op0=ALU.mult), [pb4, betab], [g1b])
            p5, pb5 = PS()
            S.pe(lambda e: e.matmul(p5[0:64, 0:64], lhsT=kT[:, csl], rhs=qT[:, csl], start=True, stop=True), [kTb, qTb], [pb5])
            S.dve(lambda e: e.tensor_copy(out=g2[:], in_=p5[0:64, 0:64]), [pb5], [g2b])
            S.pool(lambda e: e.tensor_scalar(out=kp_[:], in0=kt[:], scalar1=eglg[:, cc, h:h + 1], scalar2=None, op0=ALU.mult), [ktb, eglgb], [kpb])
            yield
            S.dve(lambda e: e.tensor_tensor(out=x_[:], in0=g1[:], in1=dt_[:], op=ALU.mult), [g1b, dtb], [xb_])
            S.dve(lambda e: e.tensor_tensor(out=x_[:], in0=x_[:], in1=negst, op=ALU.mult), [xb_, cstsb], [xb_])
            S.pool(lambda e: e.tensor_tensor(out=it_[:], in0=g2[:], in1=dt_[:], op=ALU.mult), [g2b, dtb], [itb])
            yield
            p6, pb6 = PS()
            S.pe(lambda e: e.transpose(out=p6[0:64, 0:64], in_=x_[:], identity=id64), [xb_, cstsb], [pb6])
            S.act(lambda e: e.copy(out=xt_[:], in_=p6[0:64, 0:64]), [pb6], [xtb_])
            S.pool(lambda e: e.tensor_tensor(out=y_[:], in0=x_[:], in1=id64, op=ALU.add), [xb_, cstsb], [yb_])
            yield
            for m in range(5):
                pt, ptb = PS()
                S.pe(lambda e, pt=pt: e.matmul(pt[0:64, 0:64], lhsT=x_[:], rhs=xt_[:], start=True, stop=True), [xb_, xtb_], [ptb])
                if m < 4:
                    pa, pab = PS()
                    S.pe(lambda e, pa=pa: e.matmul(pa[0:64, 0:64], lhsT=xt_[:], rhs=x_[:], start=True, stop=True), [xb_, xtb_], [pab])
                    S.dve(lambda e, pa=pa: e.tensor_copy(out=x_[:], in_=pa[0:64, 0:64]), [pab], [xb_])
                S.act(lambda e, pt=pt: e.copy(out=xt_[:], in_=pt[0:64, 0:64]), [ptb], [xtb_])
                yield
                py, pyb = PS()
                S.pe(lambda e, py=py: e.matmul(py[0:64, 0:64], lhsT=xt_[:], rhs=y_[:], start=True, stop=True), [xtb_, yb_], [pyb])
                S.dve(lambda e, py=py: e.tensor_tensor(out=y_[:], in0=py[0:64, 0:64], in1=y_[:], op=ALU.add), [pyb, yb_], [yb_])
                yield

        def scan(cc, h, of, ofb):
            csl = slice(cc * 64, (cc + 1) * 64)
            ch = (cc % (2 * CG)) * NH + h
            qT, qTb = qkv[h]
            kT, kTb = qkv[NH + h]
            s_, sb_ = St[h]
            vt, vtb = vtok[ch]
            kp_, kpb = kpp[ch]
            y_, yb_ = Y[ch]
            it_, itb = IT[ch]
            R_, Rb = Rr[h]
            vn_, vnb = vn[h]
            qs_, qsb = ivt[h]
            o_, ob_ = ot[h]
            ss_, ssb = ss[h]
            oq_, oqb = osq[h]
            bcol = beta[:, cc, h:h + 1]
            p, pb = PS()
            S.pe(lambda e: e.matmul(p[0:64, 0:128], lhsT=kT[:, csl], rhs=s_[:], start=True, stop=True), [kTb, sb_], [pb])
            S.dve(lambda e: e.scalar_tensor_tensor(out=R_[:], in0=p[0:64, 0:128], scalar=negc[:, cc, h:h + 1], in1=vt[:], op0=ALU.mult, op1=ALU.add),
                  [pb, negcb, vtb], [Rb])
            pq, pqb = PS()
            S.pe(lambda e: e.matmul(pq[0:64, 0:128], lhsT=qT[:, csl], rhs=s_[:], start=True, stop=True), [qTb, sb_], [pqb])
            S.dve(lambda e: e.tensor_scalar(out=qs_[:], in0=pq[0:64, 0:128], scalar1=egc[:, cc, h:h + 1], scalar2=None, op0=ALU.mult), [pqb, egcb], [qsb])
            yield
            p2, pb2 = PS()
            S.pe(lambda e: e.matmul(p2[0:64, 0:128], lhsT=y_[:], rhs=R_[:], start=True, stop=True), [yb_, Rb], [pb2])
            S.dve(lambda e: e.tensor_scalar(out=vn_[:], in0=p2[0:64, 0:128], scalar1=bcol, scalar2=None, op0=ALU.mult), [pb2, betab], [vnb])
            yield
            p3, pb3 = PS()
            S.pe(lambda e: e.matmul(p3[:, 0:128], lhsT=kp_[:], rhs=vn_[:], start=True, stop=True), [kpb, vnb], [pb3])
            S.dve(lambda e: e.scalar_tensor_tensor(out=s_[:], in0=s_[:], scalar=egl[:, cc, h:h + 1], in1=p3[:, 0:128], op0=ALU.mult, op1=ALU.add),
                  [pb3, eglb, sb_], [sb_])
            p4, pb4 = PS()
            S.pe(lambda e: e.matmul(p4[0:64, 0:128], lhsT=it_[:], rhs=vn_[:], start=True, stop=True), [itb, vnb], [pb4])
            S.dve(lambda e: e.tensor_tensor(out=o_[:], in0=p4[0:64, 0:128], in1=qs_[:], op=ALU.add), [pb4, qsb], [ob_])
            yield
            S.pool(lambda e: e.tensor_tensor(out=oq_[:], in0=o_[:], in1=o_[:], op=ALU.mult), [ob_], [oqb])
            yield
            S.dve(lambda e: e.reduce_sum(out=ss_[:], in_=oq_[:], axis=AX.X), [oqb], [ssb])
            yield
            S.act(lambda e: e.activation(out=ss_[:], in_=ss_[:], func=AF.Ln, bias=1e-6, scale=1.0 / GD), [ssb], [ssb])
            S.act(lambda e: e.activation(out=ss_[:], in_=ss_[:], func=AF.Exp, scale=-0.5), [ssb], [ssb])
            yield
            S.dve(lambda e: e.scalar_tensor_tensor(out=o_[:], in0=o_[:], scalar=ss_[:, 0:1], in1=nw[:], op0=ALU.mult, op1=ALU.mult),
                  [ob_, ssb, nwsb], [ob_])
            yield
            S.pool(lambda e: e.tensor_tensor(out=of[:, h * 128:(h + 1) * 128], in0=o_[:], in1=zs[:, cc, h * 128:(h + 1) * 128], op=ALU.mult),
                   [ob_, zsb], [ofb])

        def scan_seq(ccs, t0=t0):
            for cc in ccs:
                of, ofb = ofin[cc % 2]
                gens = [scan(cc, h, of, ofb) for h in range(NH)]
                while gens:
                    for g in list(gens):
                        try:
                            next(g)
                        except StopIteration:
                            gens.remove(g)
                    yield
                S.dma(lambda e, of=of, cc=cc, t0=t0: e.dma_start(out=ob_d[t0 + cc * 64:t0 + (cc + 1) * 64, ocol:ocol + NH * 128], in_=of[:]), [ofb], [obb])

        NGRP = 8 // CG
        grp = lambda g: list(range(g * CG, (g + 1) * CG))
        drive([pre(cc, h) for cc in grp(0) for h in range(NH)])
        for g in range(NGRP):
            gens = [scan_seq(grp(g))]
            if g + 1 < NGRP:
                gens += [pre(cc, h) for cc in grp(g + 1) for h in range(NH)]
            drive(gens)
    S.emit(stack)


def gdn_consts():
    c = np.zeros((6, 128, 128), np.float32)
    c[0] = np.eye(128)
    k = np.arange(128)
    c[1] = (k[:, None] <= k[None, :]).astype(np.float32)
    c[2] = 1.0
    c[3] = np.where(k[None, :] < k[:, None], -30000.0, 0.0)
    c[4] = np.where(k[None, :] > k[:, None], -1.0, 0.0)
    return c


def gdn_inputs(inp, b, half, heads=None):
    if heads is None:
        heads = [2 * half, 2 * half + 1]
    w_in = inp["ev_w_in"][0]
    o_gq = 512 + 6 * 128 + 24
    o_gk, o_gv, o_gz = o_gq + 512, o_gq + 1024, o_gq + 1536
    o_gb = o_gq + 2048
    o_ga = o_gb + 4
    hc = lambda off: np.concatenate([w_in[:, off + h * 128: off + (h + 1) * 128] for h in heads], 1)
    wqkv = np.concatenate([hc(o_gq), hc(o_gk), hc(o_gv)], 1)
    wz = hc(o_gz)
    wbg = np.concatenate([w_in[:, [o_gb + h for h in heads]], w_in[:, [o_ga + h for h in heads]]], 1)
    cw = inp["ev_conv_w"][0]
    cc = lambda off: np.concatenate([cw[:, off + h * 128: off + (h + 1) * 128] for h in heads], 1)
    convw = np.concatenate([cc(0), cc(512), cc(1024)], 1).T
    hparm = np.concatenate([inp["ev_a_log"][0][heads], inp["ev_dt_bias"][0][heads]])[None, :]
    return {"xb": np.ascontiguousarray(inp["x"][b]), "wqkv": np.ascontiguousarray(wqkv), "wz": np.ascontiguousarray(wz),
            "wbg": np.ascontiguousarray(wbg), "convw": np.ascontiguousarray(convw), "hparm": np.ascontiguousarray(hparm),
            "normw": np.ascontiguousarray(inp["ev_gdn_norm"][0][None, :]), "gcst": gdn_consts()}


def build_nsa(nc, stack, nqblk=SEQ // 512, shared=None, pfx="", io=None, ocol=0):
    C = Ctx(nc, stack, shared, pfx, io)
    S = C.S
    PS = PsumRing(C, 4)
    ACC = [C.ps([128, 512], F32, "acc%d" % i) for i in range(4)]
    xb_d, xbb = C.din("xb", [SEQ, D])
    wq_d, wqb = C.din("wq", [D, 1024])
    wk_d, wkb = C.din("wk", [D, 384])
    wv_d, wvb = C.din("wv", [D, 128])
    wgt_d, wgtb = C.din("wgt", [D, 12])
    cwk_d, cwkb = C.din("cmpwk", [2, 64, 32, 64])
    cwv_d, cwvb = C.din("cmpwv", [128, 32, 64])
    pek_d, pekb = C.din("cmppek", [64, 32])
    pev_d, pevb = C.din("cmppev", [128, 32])
    rope_d, ropeb = C.din("rope", [2, 128, SEQ])
    ropek_d, ropekb = C.din("ropek", [2, 64, 512])
    E_d, Eb = C.din("Eexp", [128, 32, 128])
    mk_d, mkb = C.din("masks", [8, 128, 512])
    cmk_d, cmkb = C.din("cmask", [17, 128, 128])
    ov_d, ovb = C.din("overlap", [512, 128])
    W_d, Wb = C.din("forceW", [128, 256])
    id_d, idb = C.din("ident", [128, 128])
    oa_d, oab = C.dout("o_a", [SEQ, 256])

    ident, identb = C.sb([128, 128], F32, "ident")
    S.dma(lambda e: e.dma_start(out=ident[:], in_=id_d), [idb], [identb])
    if "zero_rows" in C.io:
        for (zdst, zsrc) in C.io["zero_rows"]:
            S.dma(lambda e, zdst=zdst, zsrc=zsrc: e.dma_start(out=zdst, in_=zsrc), [], [oab])
    wq, wqsb = C.sb([128, 8, 1024], BF16, "wq")
    S.dma(lambda e: e.dma_start(out=wq[:], in_=wq_d.rearrange("(k p) n -> p k n", p=128)), [wqb], [wqsb], q="pool")
    wk, wksb = C.sb([128, 8, 384], BF16, "wk")
    S.dma(lambda e: e.dma_start(out=wk[:], in_=wk_d.rearrange("(k p) n -> p k n", p=128)), [wkb], [wksb], q="pool")
    wv, wvsb = C.sb([128, 8, 128], BF16, "wv")
    S.dma(lambda e: e.dma_start(out=wv[:], in_=wv_d.rearrange("(k p) n -> p k n", p=128)), [wvb], [wvsb], q="pool")
    wgt, wgtsb = C.sb([128, 8, 12], BF16, "wgt")
    S.dma(lambda e: e.dma_start(out=wgt[:], in_=wgt_d.rearrange("(k p) n -> p k n", p=128)), [wgtb], [wgtsb], q="pool")
    cwk, cwksb = C.sb([64, 2, 32, 64], BF16, "cwk")
    S.dma(lambda e: e.dma_start(out=cwk[:], in_=cwk_d.rearrange("a d l e -> d a l e")), [cwkb], [cwksb], q="pool")
    cwv, cwvsb = C.sb([128, 32, 64], BF16, "cwv")
    S.dma(lambda e: e.dma_start(out=cwv[:], in_=cwv_d), [cwvb], [cwvsb], q="pool")
    pek, peksb = C.sb([64, 32], BF16, "pek")
    S.dma(lambda e: e.dma_start(out=pek[:], in_=pek_d), [pekb], [peksb], q="pool")
    pev, pevsb = C.sb([128, 32], BF16, "pev")
    S.dma(lambda e: e.dma_start(out=pev[:], in_=pev_d), [pevb], [pevsb], q="pool")
    ropek, ropeksb = C.sb([64, 2, 512], F32, "ropek")
    S.dma(lambda e: e.dma_start(out=ropek[:], in_=ropek_d.rearrange("a d n -> d a n")), [ropekb], [ropeksb])
    Ex, Exb = C.sb([128, 32, 128], BF16, "Ex")
    S.dma(lambda e: e.dma_start(out=Ex[:], in_=E_d), [Eb], [Exb], q="pool")
    mk, mksb = C.sb([128, 8, 512], BF16, "mk")
    S.dma(lambda e: e.dma_start(out=mk[:], in_=mk_d.rearrange("a p q -> p a q")), [mkb], [mksb], q="pool")
    cmk, cmksb = C.sb([128, 17, 128], BF16, "cmk")
    S.dma(lambda e: e.dma_start(out=cmk[:], in_=cmk_d.rearrange("a p q -> p a q")), [cmkb], [cmksb], q="pool")
    Wt, Wsb = C.sb([128, 256], F32, "Wt")
    S.dma(lambda e: e.dma_start(out=Wt[:], in_=W_d), [Wb], [Wsb])

    kT2, kT2b = C.sb([128, SEQ], BF16, "kT2")
    kvcin, kvcinb = C.sb([128, SEQ], BF16, "kvcin")
    vslc, vslcb = C.sb([128, 64, 65], BF16, "vslc")
    vwin, vwinb = C.sb([128, 64, 65], BF16, "vwin")
    S.pool(lambda e: e.memset(vslc[:].rearrange("p a b -> p (a b)"), 1.0), [], [vslcb])
    S.pool(lambda e: e.memset(vwin[:].rearrange("p a b -> p (a b)"), 1.0), [], [vwinb])
    kcT, kcTb = C.sb([64, 512], F32, "kcT")
    vca, vcab = C.sb([128, 4, 193], F32, "vca")
    S.pool(lambda e: e.memset(vca[:].rearrange("p a b -> p (a b)"), 0.0), [], [vcab])
    S.pool(lambda e: e.memset(vca[:, :, 64:65], 1.0), [vcab], [vcab])
    S.dma(lambda e: e.dma_start(out=vca[:, :, 65:193], in_=ov_d.rearrange("(c p) m -> p c m", p=128)), [ovb, vcab], [vcab])

    xtile = [C.sb([128, D], F32, "xtile%d" % i) for i in range(2)]
    xT, xTb = C.sb([128, 8, 512], BF16, "xT")
    rp, rpb = C.sb([128, 2, 512], F32, "rp")
    t1, t1b = C.sb([128, 512], F32, "t1")
    t2, t2b = C.sb([128, 512], F32, "t2")

    def rope_from(pa, pab, pbk, pbkb, dst, dstb):
        S.dve(lambda e: e.tensor_tensor(out=t1[:], in0=pa, in1=rp[:, 0, :], op=ALU.mult), [pab, rpb], [t1b])
        S.dve(lambda e: e.tensor_tensor(out=t2[:], in0=pbk, in1=rp[:, 1, :], op=ALU.mult), [pbkb, rpb], [t2b])
        S.pool(lambda e: e.tensor_tensor(out=dst, in0=t1[:], in1=t2[:], op=ALU.add), [t1b, t2b], [dstb])

    for blk in range(SEQ // 512):
        t0 = blk * 512
        load_xT_block(C, PS, xb_d, xbb, t0, ident, identb, xtile, xT, xTb)
        S.dma(lambda e, t0=t0: e.dma_start(out=rp[:], in_=rope_d[:, :, t0:t0 + 512].rearrange("a d n -> d a n")), [ropeb], [rpb])
        pk = []
        for c in range(3):
            p, pb = ACC[c]
            for k in range(8):
                S.pe(lambda e, p=p, k=k, c=c: e.matmul(p[:], lhsT=wk[:, k, c * 128:(c + 1) * 128], rhs=xT[:, k, :],
                                                       start=(k == 0), stop=(k == 7)), [wksb, xTb], [pb])
            pk.append((p, pb))
        S.act(lambda e, t0=t0: e.copy(out=kvcin[:, t0:t0 + 512], in_=pk[0][0][:]), [pk[0][1]], [kvcinb])
        rope_from(pk[1][0][:], pk[1][1], pk[2][0][:], pk[2][1], kT2[:, t0:t0 + 512], kT2b)
        for j in range(4):
            p, pb = PS()
            for k in range(8):
                S.pe(lambda e, p=p, k=k, j=j: e.matmul(p[:, 0:128], lhsT=xT[:, k, j * 128:(j + 1) * 128], rhs=wv[:, k, :],
                                                       start=(k == 0), stop=(k == 7)), [xTb, wvsb], [pb])
            tix = blk * 4 + j
            S.act(lambda e, p=p, tix=tix: e.copy(out=vslc[:, tix, 0:64], in_=p[:, 0:64]), [pb], [vslcb, pb])
            S.dve(lambda e, p=p, tix=tix: e.tensor_copy(out=vwin[:, tix, 0:64], in_=p[:, 64:128]), [pb], [vwinb])
    kb_, kbb = C.sb([128, 4], F32, "kbias")
    for a in range(2):
        p, pb = PS()
        for l in range(32):
            S.pe(lambda e, p=p, l=l, a=a: e.matmul(p[0:64, 0:1], lhsT=cwk[:, a, l, :], rhs=pek[:, l:l + 1],
                                                   start=(l == 0), stop=(l == 31)), [cwksb, peksb], [pb])
        S.dve(lambda e, p=p, a=a: e.tensor_copy(out=kb_[0:64, a:a + 1], in_=p[0:64, 0:1]), [pb], [kbb])
    p, pb = PS()
    for l in range(32):
        S.pe(lambda e, p=p, l=l: e.matmul(p[0:64, 0:1], lhsT=cwv[64:128, l, :], rhs=pev[64:128, l:l + 1],
                                          start=(l == 0), stop=(l == 31)), [cwvsb, pevsb], [pb])
    S.dve(lambda e, p=p: e.tensor_copy(out=kb_[0:64, 2:3], in_=p[0:64, 0:1]), [pb], [kbb])
    kc0, kc0b = C.sb([64, 512], F32, "kc0")
    kc1, kc1b = C.sb([64, 512], F32, "kc1")
    S.pool(lambda e: e.memset(kc0[:], 0.0), [], [kc0b])
    S.pool(lambda e: e.memset(kc1[:], 0.0), [], [kc1b])
    for a, (dst, dstb) in enumerate([(kc0, kc0b), (kc1, kc1b)]):
        p, pb = PS()
        for l in range(32):
            S.pe(lambda e, p=p, l=l, a=a: e.matmul(p[0:64, 0:511], lhsT=cwk[:, a, l, :], rhs=kvcin[0:64, l:l + 16 * 510 + 1:16],
                                                   start=(l == 0), stop=(l == 31)), [cwksb, kvcinb], [pb])
        S.dve(lambda e, p=p, dst=dst, a=a: e.tensor_scalar(out=dst[:, 0:511], in0=p[0:64, 0:511], scalar1=kb_[0:64, a:a + 1], scalar2=None, op0=ALU.add),
              [pb, kbb], [dstb])
    S.dve(lambda e: e.tensor_tensor(out=kc0[:], in0=kc0[:], in1=ropek[:, 0, :], op=ALU.mult), [kc0b, ropeksb], [kc0b])
    S.dve(lambda e: e.tensor_tensor(out=kc1[:], in0=kc1[:], in1=ropek[:, 1, :], op=ALU.mult), [kc1b, ropeksb], [kc1b])
    S.dve(lambda e: e.tensor_tensor(out=kcT[:], in0=kc0[:], in1=kc1[:], op=ALU.add), [kc0b, kc1b], [kcTb])
    vbT, vbTb = C.sb([64, 128], F32, "vbT")
    S.pool(lambda e: e.memset(vbT[:], 0.0), [], [vbTb])
    S.dve(lambda e: e.tensor_scalar(out=vbT[:], in0=vbT[:], scalar1=kb_[0:64, 2:3], scalar2=None, op0=ALU.add), [vbTb, kbb], [vbTb])
    vbr, vbrb = C.sb([128, 64], F32, "vbr")
    p, pb = PS()
    S.pe(lambda e, p=p: e.transpose(out=p[:, 0:64], in_=vbT[:], identity=ident[0:64, 0:64]), [vbTb, identb], [pb])
    S.dve(lambda e, p=p: e.tensor_copy(out=vbr[:], in_=p[:, 0:64]), [pb], [vbrb])
    for c in range(4):
        nn = 128 if c < 3 else 127
        p, pb = PS()
        for l in range(32):
            s0 = l + 16 * 128 * c
            S.pe(lambda e, p=p, l=l, s0=s0, nn=nn: e.matmul(p[0:nn, 0:64], lhsT=kvcin[64:128, s0:s0 + 16 * (nn - 1) + 1:16], rhs=cwv[64:128, l, :],
                                                            start=(l == 0), stop=(l == 31)), [cwvsb, kvcinb], [pb])
        S.dve(lambda e, p=p, c=c, nn=nn: e.tensor_tensor(out=vca[0:nn, c, 0:64], in0=p[0:nn, 0:64], in1=vbr[0:nn, :], op=ALU.add), [pb, vbrb, vcab], [vcab])

    qT = [C.sb([128, 512], F32, "qT%d" % h) for h in range(4)]
    qTh = [C.sb([128, 512], BF16, "qTh%d" % h) for h in range(4)]
    gts, gtsb = C.sb([128, 4, 12], F32, "gts")
    PTc = [C.sb([128, 512], F32, "PTc%d" % i) for i in range(4)]
    rz = [C.sb([128, 1], F32, "rz%d" % i) for i in range(4)]
    oc, ocb = C.sb([128, 4, 4, 64], F32, "oc")
    imps = [C.sb([128, 128], F32, "imp%d" % i) for i in range(2)]
    sc2, sc2b = C.sb([128, 128], F32, "sc2")
    m8a, m8ab = C.sb([128, 8], F32, "m8a")
    m8b, m8bb = C.sb([128, 8], F32, "m8b")
    sel, selb = C.sb([128, 128], F32, "sel")
    selT, selTb = C.sb([128, 512], BF16, "selT")
    maskT = [C.sb([128, 512], BF16, "maskT%d" % i) for i in range(3)]
    PT = [C.sb([128, 512], BF16, "PT%d" % i) for i in range(6)]
    osT = [C.sb([65, 512], F32, "osT%d" % i) for i in range(2)]
    fs = [C.sb([128, 1], F32, "fs%d" % i) for i in range(2)]
    oa, oab_ = C.sb([128, 4, 256], F32, "oa")
    ctr = {"pt": 0, "m": 0, "o": 0}
    SCALE = 0.125

    for qblk in range(nqblk):
        t0 = qblk * 512
        load_xT_block(C, PS, xb_d, xbb, t0, ident, identb, xtile, xT, xTb)
        S.dma(lambda e, t0=t0: e.dma_start(out=rp[:], in_=rope_d[:, :, t0:t0 + 512].rearrange("a d n -> d a n")), [ropeb], [rpb])
        for h in range(4):
            pa, pab = PS()
            pb_, pbb = PS()
            for k in range(8):
                S.pe(lambda e, pa=pa, k=k, h=h: e.matmul(pa[:], lhsT=wq[:, k, h * 256:h * 256 + 128], rhs=xT[:, k, :],
                                                         start=(k == 0), stop=(k == 7)), [wqsb, xTb], [pab])
            for k in range(8):
                S.pe(lambda e, pb_=pb_, k=k, h=h: e.matmul(pb_[:], lhsT=wq[:, k, h * 256 + 128:h * 256 + 256], rhs=xT[:, k, :],
                                                           start=(k == 0), stop=(k == 7)), [wqsb, xTb], [pbb])
            rope_from(pa[:], pab, pb_[:], pbb, qT[h][0][:], qT[h][1])
            S.act(lambda e, h=h: e.copy(out=qTh[h][0][:], in_=qT[h][0][:]), [qT[h][1]], [qTh[h][1]])
        for j in range(4):
            p, pb = PS()
            for k in range(8):
                S.pe(lambda e, p=p, k=k, j=j: e.matmul(p[:, 0:12], lhsT=xT[:, k, j * 128:(j + 1) * 128], rhs=wgt[:, k, :],
                                                       start=(k == 0), stop=(k == 7)), [xTb, wgtsb], [pb])
            S.act(lambda e, p=p, j=j: e.activation(out=gts[:, j, :], in_=p[:, 0:12], func=AF.Sigmoid), [pb], [gtsb])
        csteps = [(j, h) for j in range(4) for h in range(4)]
        cinfo = {}

        def chunks_of(qb):
            out = []
            for c in range(4):
                delta = 128 * c - 8 * qb
                if delta >= 7:
                    continue
                out.append((c, None if delta <= -129 else (delta + 128) // 8))
            return out

        def cstageA(j, h):
            qb = qblk * 4 + j
            chunks = chunks_of(qb)
            i = ctr["pt"]
            ctr["pt"] += 1
            ptc, ptcb = PTc[i % len(PTc)]
            rz_, rzb = rz[i % len(rz)]
            p, pb = PS()
            for (c, mi) in chunks:
                S.pe(lambda e, p=p, c=c, h=h, j=j: e.matmul(p[:, c * 128:(c + 1) * 128], lhsT=kcT[:, c * 128:(c + 1) * 128],
                                                            rhs=qT[h][0][0:64, j * 128:(j + 1) * 128], start=True, stop=True),
                     [kcTb, qT[h][1]], [pb])
            nc_ = len(chunks) * 128
            S.act(lambda e, p=p, ptc=ptc, nc_=nc_: e.activation(out=ptc[:, 0:nc_], in_=p[:, 0:nc_], func=AF.Exp, scale=SCALE), [pb], [ptcb])
            for (c, mi) in chunks:
                if mi is not None:
                    S.dve(lambda e, ptc=ptc, c=c, mi=mi: e.tensor_tensor(out=ptc[:, c * 128:(c + 1) * 128], in0=ptc[:, c * 128:(c + 1) * 128],
                                                                         in1=cmk[:, mi, :], op=ALU.mult), [ptcb, cmksb], [ptcb])
            cinfo[(j, h)] = (chunks, ptc, ptcb, rz_, rzb)

        def cstageB(j, h):
            qb = qblk * 4 + j
            chunks, ptc, ptcb, rz_, rzb = cinfo[(j, h)]
            po, pob = PS()
            for ci, (c, mi) in enumerate(chunks):
                S.pe(lambda e, po=po, ptc=ptc, c=c, ci=ci, n=len(chunks): e.matmul(po[:, 0:193], lhsT=ptc[:, c * 128:(c + 1) * 128], rhs=vca[:, c, :],
                                                                                 start=(ci == 0), stop=(ci == n - 1)), [ptcb, vcab], [pob])
            S.dve(lambda e, po=po, rz_=rz_: e.tensor_scalar(out=rz_[:], in0=po[:, 64:65], scalar1=1e-30, scalar2=None, op0=ALU.max), [pob], [rzb])
            S.dve(lambda e, rz_=rz_: e.reciprocal(out=rz_[:], in_=rz_[:]), [rzb], [rzb])
            S.dve(lambda e, po=po, rz_=rz_, j=j, h=h: e.tensor_scalar(out=oc[:, j, h, :], in0=po[:, 0:64], scalar1=rz_[:, 0:1], scalar2=None,
                                                                    op0=ALU.mult), [pob, rzb], [ocb])
            im_, imb_ = imps[j % 2]
            if h == 0:
                S.dve(lambda e, po=po, rz_=rz_, im_=im_: e.tensor_scalar(out=im_[:], in0=po[:, 65:193], scalar1=rz_[:, 0:1], scalar2=None, op0=ALU.mult),
                      [pob, rzb], [imb_])
            else:
                S.dve(lambda e, po=po, rz_=rz_, im_=im_: e.scalar_tensor_tensor(out=im_[:], in0=po[:, 65:193], scalar=rz_[:, 0:1], in1=im_[:],
                                                                                op0=ALU.mult, op1=ALU.add), [pob, rzb, imb_], [imb_])
            if h == 3:
                S.dve(lambda e, qb=qb, im_=im_: e.tensor_tensor(out=im_[:], in0=im_[:], in1=Wt[:, 128 - 2 * qb:256 - 2 * qb], op=ALU.max), [imb_, Wsb], [imb_])
                S.pool(lambda e, im_=im_: e.memset(im_[:, 0:1], 1e6), [imb_], [imb_])
                S.dve(lambda e, im_=im_: e.max(out=m8a[:], in_=im_[:]), [imb_], [m8ab])
                S.dve(lambda e, im_=im_: e.match_replace(out=sc2[:], in_to_replace=m8a[:], in_values=im_[:], imm_value=-2.0), [m8ab, imb_], [sc2b])
                S.dve(lambda e: e.max(out=m8b[:], in_=sc2[:]), [sc2b], [m8bb])
                S.dve(lambda e, im_=im_: e.tensor_scalar(out=sel[:], in0=im_[:], scalar1=m8b[:, 7:8], scalar2=None, op0=ALU.is_ge), [imb_, m8bb], [selb])
                pst, pstb = PS()
                S.pe(lambda e, pst=pst: e.transpose(out=pst[:, 0:128], in_=sel[:], identity=ident[:]), [selb, identb], [pstb])
                S.act(lambda e, pst=pst, j=j: e.copy(out=selT[:, j * 128:(j + 1) * 128], in_=pst[:, 0:128]), [pstb], [selTb])

        LC = 2
        for i in range(len(csteps) + LC):
            if i < len(csteps):
                cstageA(*csteps[i])
            if i >= LC:
                cstageB(*csteps[i - LC])

        for br in range(2):
            if br == 0:
                kcs = list(range(0, 4 * qblk + 4))
                VV, VVb = vslc, vslcb
                r0 = 0
            else:
                kcs = [kc for kc in range(4 * qblk - 4, 4 * qblk + 4) if kc >= 0]
                VV, VVb = vwin, vwinb
                r0 = 64
            pend = []
            LS = 3

            def flush_one():
                (h, kc, pt_, ptb_, ki, n) = pend.pop(0)
                S.pe(lambda e, h=h, kc=kc, pt_=pt_, VV=VV, ki=ki, n=n: e.matmul(ACC[h][0][0:65, :], lhsT=VV[:, kc, :], rhs=pt_[:],
                                                                              start=(ki == 0), stop=(ki == n - 1)), [VVb, ptb_], [ACC[h][1]])
            for ki, kc in enumerate(kcs):
                dk = kc - 4 * qblk
                if br == 0:
                    mt, mtb = maskT[ctr["m"] % len(maskT)]
                    ctr["m"] += 1
                    pm, pmb = PS()
                    base = 0 if (2 * kc) < 64 else 64
                    v = kc % 32
                    S.pe(lambda e, pm=pm, base=base, v=v: e.matmul(pm[:], lhsT=Ex[base:base + 64, v, :], rhs=selT[base:base + 64, :], start=True, stop=True),
                         [Exb, selTb], [pmb])
                    if dk >= 0:
                        S.dve(lambda e, pm=pm, mt=mt, dk=dk: e.tensor_tensor(out=mt[:], in0=pm[:], in1=mk[:, 4 + dk, :], op=ALU.mult), [pmb, mksb], [mtb])
                    else:
                        S.dve(lambda e, pm=pm, mt=mt: e.tensor_copy(out=mt[:], in_=pm[:]), [pmb], [mtb])
                    mask_ap, mask_b = mt[:], mtb
                else:
                    mask_ap, mask_b = mk[:, dk + 4, :], mksb
                for h in range(4):
                    pt_, ptb_ = PT[ctr["pt"] % len(PT)]
                    ctr["pt"] += 1
                    ps_, psb_ = PS()
                    S.pe(lambda e, ps_=ps_, kc=kc, h=h, r0=r0: e.matmul(ps_[:], lhsT=kT2[r0:r0 + 64, kc * 128:(kc + 1) * 128], rhs=qTh[h][0][r0:r0 + 64, :],
                                                                       start=True, stop=True), [kT2b, qTh[h][1]], [psb_])
                    S.act(lambda e, ps_=ps_, pt_=pt_: e.activation(out=pt_[:], in_=ps_[:], func=AF.Exp, scale=SCALE), [psb_], [ptb_])
                    S.dve(lambda e, pt_=pt_, mask_ap=mask_ap: e.tensor_tensor(out=pt_[:], in0=pt_[:], in1=mask_ap, op=ALU.mult), [ptb_, mask_b], [ptb_])
                    pend.append((h, kc, pt_, ptb_, ki, len(kcs)))
                    if len(pend) > LS:
                        flush_one()
            while pend:
                flush_one()
            for h in range(4):
                ot_, otb_ = osT[h % 2]
                S.act(lambda e, h=h, ot_=ot_: e.copy(out=ot_[:], in_=ACC[h][0][0:65, :]), [ACC[h][1]], [otb_])
                for j in range(4):
                    i = ctr["o"]
                    ctr["o"] += 1
                    fs_, fsb = fs[i % 2]
                    p, pb = PS()
                    S.pe(lambda e, p=p, ot_=ot_, j=j: e.transpose(out=p[:, 0:65], in_=ot_[:, j * 128:(j + 1) * 128], identity=ident[0:65, 0:65]),
                         [otb_, identb], [pb])
                    S.dve(lambda e, p=p, fs_=fs_: e.tensor_scalar(out=fs_[:], in0=p[:, 64:65], scalar1=1e-30, scalar2=None, op0=ALU.max), [pb], [fsb])
                    S.dve(lambda e, fs_=fs_: e.reciprocal(out=fs_[:], in_=fs_[:]), [fsb], [fsb])
                    gi = h * 3 + 1 + br
                    S.dve(lambda e, fs_=fs_, j=j, gi=gi: e.tensor_tensor(out=fs_[:], in0=fs_[:], in1=gts[:, j, gi:gi + 1], op=ALU.mult), [fsb, gtsb], [fsb])
                    dsl = oa[:, j, h * 64:(h + 1) * 64]
                    if br == 0:
                        S.pool(lambda e, j=j, h=h, dsl=dsl: e.tensor_scalar(out=dsl, in0=oc[:, j, h, :], scalar1=gts[:, j, h * 3:h * 3 + 1], scalar2=None,
                                                                           op0=ALU.mult), [ocb, gtsb, oab_], [oab_])
                    S.dve(lambda e, p=p, fs_=fs_, dsl=dsl: e.scalar_tensor_tensor(out=dsl, in0=p[:, 0:64], scalar=fs_[:, 0:1], in1=dsl,
                                                                                 op0=ALU.mult, op1=ALU.add), [pb, fsb, oab_], [oab_])
        for j in range(4):
            S.dma(lambda e, j=j, t0=t0: e.dma_start(out=oa_d[t0 + j * 128:t0 + (j + 1) * 128, ocol:ocol + 256], in_=oa[:, j, :]), [oab_], [oab])
    S.emit(stack)


def nsa_consts():
    c = {}
    half = 32
    inv = np.power(10000.0, -np.arange(half, dtype=np.float32) / half).astype(np.float32)
    pos = np.arange(SEQ, dtype=np.float32)
    ang = pos[None, :] * inv[:, None]
    cos, sin = np.cos(ang).astype(np.float32), np.sin(ang).astype(np.float32)
    cosC = np.concatenate([cos, cos], 0)
    sinS = np.concatenate([-sin, sin], 0)
    c["rope"] = np.stack([np.concatenate([cosC, cosC], 0), np.concatenate([sinS, sinS], 0)]).astype(np.float32)
    posk = (np.arange(512, dtype=np.float32) * 16 + 15.5)
    angk = posk[None, :] * inv[:, None]
    ck, sk = np.cos(angk).astype(np.float32), np.sin(angk).astype(np.float32)
    c["ropek"] = np.stack([np.concatenate([ck, ck], 0), np.concatenate([-sk, sk], 0)]).astype(np.float32)
    p = np.arange(128)
    E = np.zeros((128, 32, 128), np.float32)
    for v in range(32):
        for k in range(128):
            r = 2 * v + k // 64
            if r < 64:
                E[r, v, k] = 1.0
                E[64 + r, v, k] = 1.0
    c["Eexp"] = E
    q = np.arange(512)
    mk = np.zeros((8, 128, 512), np.float32)
    for i in range(8):
        kp = 128 * (i - 4) + p[:, None]
        rel = q[None, :] - kp
        mk[i] = ((rel >= 0) & (rel < 512)).astype(np.float32)
    c["masks"] = mk
    ql = np.arange(128)
    cm = np.zeros((17, 128, 128), np.float32)
    for mi in range(17):
        delta = mi * 8 - 128
        npr = p[:, None] + delta
        cm[mi] = (16 * npr + 31 <= ql[None, :]).astype(np.float32)
    c["cmask"] = cm
    n = np.arange(512)
    m = np.arange(128)
    ov = ((16 * n[:, None] <= 64 * m[None, :] + 63) & (16 * n[:, None] + 31 >= 64 * m[None, :])).astype(np.float32)
    ov[511] = 0.0
    c["overlap"] = ov
    W = np.full((128, 256), -1.0, np.float32)
    for qq in range(128):
        rels = (0, -1) if qq < 64 else (0, 1)
        for r in rels:
            W[qq, 128 + r] = 1e6
    c["forceW"] = W
    c["ident"] = np.eye(128, dtype=np.float32)
    return c


def nsa_inputs(inp, b, hkv, consts):
    w_in = inp["ev_w_in"][0]
    sw = lambda a: np.concatenate([a[..., 32:], a[..., :32]], -1)
    cols = []
    for g in range(4):
        h = hkv * 4 + g
        qh = w_in[:, h * 64:(h + 1) * 64]
        cols += [qh, qh, sw(qh), sw(qh)]
    wq = np.concatenate(cols, 1)
    kv = lambda i: w_in[:, 512 + i * 128 + hkv * 64: 512 + i * 128 + (hkv + 1) * 64]
    wk = np.concatenate([kv(0), kv(1), kv(2), kv(4), sw(kv(2)), sw(kv(4))], 1)
    wv = np.concatenate([kv(3), kv(5)], 1)
    og = 512 + 6 * 128
    wgt = w_in[:, og + hkv * 12: og + (hkv + 1) * 12]
    wkc = inp["ev_cmp_w_k"][0]
    wvc = inp["ev_cmp_w_v"][0]
    cmpwk = np.stack([wkc.transpose(1, 0, 2), sw(wkc).transpose(1, 0, 2)])
    cmpwv = np.concatenate([np.zeros((64, 32, 64), np.float32), wvc.transpose(1, 0, 2)], 0)
    pek = inp["ev_cmp_pe_k"][0].T
    pev = np.concatenate([np.zeros((64, 32), np.float32), inp["ev_cmp_pe_v"][0].T], 0)
    d = {"xb": np.ascontiguousarray(inp["x"][b]), "wq": np.ascontiguousarray(wq), "wk": np.ascontiguousarray(wk),
         "wv": np.ascontiguousarray(wv), "wgt": np.ascontiguousarray(wgt), "cmpwk": np.ascontiguousarray(cmpwk),
         "cmpwv": np.ascontiguousarray(cmpwv), "cmppek": np.ascontiguousarray(pek), "cmppev": np.ascontiguousarray(pev)}
    d.update(consts)
    return d


NSA_SHARED = {"rope": [2, 128, SEQ], "ropek": [2, 64, 512], "Eexp": [128, 32, 128], "masks": [8, 128, 512],
              "cmask": [17, 128, 128], "overlap": [512, 128], "forceW": [128, 256], "ident": [128, 128]}
I32 = mybir.dt.int32


def build_fused(nc, stack):
    from contextlib import ExitStack
    shared = {"stack": stack}
    xpad = nc.dram_tensor("xpad", [SEQ + 128, D], F32, kind="ExternalInput").ap()
    nonce = nc.dram_tensor("nonce", [1, 16], I32, kind="ExternalInput").ap()
    mixh = nc.dram_tensor("mixh", [2, SEQ + 128, 256], F32, kind="Internal").ap()
    shmix = nc.dram_tensor("shmix", [2, 2, SEQ + 128, 256], F32, kind="Internal", addr_space="Shared").ap()
    flag = nc.dram_tensor("shflag", [2, 16], I32, kind="Internal", addr_space="Shared").ap()
    io = {"xb": xpad[128:SEQ + 128, :], "gcst": nc.dram_tensor("gcst", [6, 128, 128], F32, kind="ExternalInput").ap()}
    for k, shp in NSA_SHARED.items():
        io[k] = nc.dram_tensor(k, shp, F32, kind="ExternalInput").ap()
    with ExitStack() as st:
        d = dict(io)
        d["o_a"] = mixh[0, 128:SEQ + 128, :]
        d["zero_rows"] = [(mixh[0, 0:128, :], xpad[0:128, 0:256]), (mixh[1, 0:128, :], xpad[0:128, 0:256])]
        build_nsa(nc, st, shared=shared, pfx="n_", io=d, ocol=0)
    with ExitStack() as st:
        d = {"xb": io["xb"], "gcst": io["gcst"], "o_b": mixh[1, 128:SEQ + 128, :]}
        build_gdn(nc, st, shared=shared, pfx="g_", io=d, ocol=0, NH=2)
    with ExitStack() as st:
        C = Ctx(nc, st, shared, "x_")
        S = C.S
        mb, sb_, fb, nb = Buf("mixh"), Buf("shmix"), Buf("flag"), Buf("nonce")
        S.dma(lambda e: e.dma_start(out=shmix[bass.ds(nc.partition_id() % 2, 1), :, :, :], in_=mixh), [mb], [sb_])
        S.dma(lambda e: e.dma_start(out=flag[bass.ds(nc.partition_id() % 2, 1), :], in_=nonce), [nb, sb_], [fb])

        def poll(e):
            with e.register("pf") as f, e.register("pn") as n, e.register("pd") as dd:
                e.reg_load(n, nonce[0:1, 0:1])
                other = flag[bass.ds(1 - nc.partition_id() % 2, 1), 0:1]
                e.reg_load(f, other)
                e.reg_sub(dd, f, n)
                with e.While(dd):
                    e.reg_load(f, other)
                    e.reg_sub(dd, f, n)
            return e.nop()
        S.add("sp", poll, [fb], [fb, sb_])
        S.emit(st)
    with ExitStack() as st:
        d = {"xpad": xpad, "shmix": shmix, "ident": io["ident"]}
        build_tail(nc, st, shared=shared, pfx="t_", io=d, dyn=True)


_PROGS = {}


def _prog(name, builder):
    if name not in _PROGS:
        from contextlib import ExitStack
        nc = bass.Bass("TRN2", target_bir_lowering=False)
        with ExitStack() as st:
            builder(nc, st)
        _PROGS[name] = nc
    return _PROGS[name]


def fused_inputs(inp, c, cs, A, lnp, nonce):
    b, r = c // 2, c % 2
    d = {"xpad": np.concatenate([np.zeros((128, D), np.float32), inp["x"][b]]), "gcst": gdn_consts(),
         "nonce": np.full((1, 16), nonce, np.int32)}
    d.update(cs)
    for k, v in nsa_inputs(inp, b, r, {}).items():
        if k != "xb":
            d["n_" + k] = v
    for k, v in gdn_inputs(inp, b, r).items():
        if k not in ("xb", "gcst"):
            d["g_" + k] = v
    Ac = A
    if r == 1:
        Ac = A.copy()
        Ac[0] = A[2]
        Ac[1] = A[3]
    t = {"w_out": inp["ev_w_out"][0], "lnp": lnp, "ffn_wg": inp["ev_ffn_wg"][0], "ffn_wu": inp["ev_ffn_wu"][0],
         "ffn_wd": inp["ev_ffn_wd"][0], "pool_w": inp["od_pool_w"][0], "router_w": inp["od_router_w"][0],
         "exp_wg": inp["od_exp_wg"][0], "exp_wu": inp["od_exp_wu"][0], "exp_wd": inp["od_exp_wd"][0], "apool": Ac}
    for k, v in t.items():
        d["t_" + k] = v
    return d


_CALLS = [0]


def kernel(**inp):
    inp = {k: np.asarray(v) for k, v in inp.items()}
    B = inp["x"].shape[0]
    cores = list(range(NCORES))
    cs = nsa_consts()
    A = pool_consts()
    lnp = np.stack([inp[k][0] for k in ["ev_ln1_g", "ev_ln1_b", "ev_ln2_g", "ev_ln2_b", "od_pool_scale",
                                        "od_ln1_g", "od_ln1_b", "od_ln2_g", "od_ln2_b"]])
    _CALLS[0] += 1
    nonce = (int.from_bytes(os.urandom(3), "little") << 4) + (_CALLS[0] % 16) + 1
    ims = [fused_inputs(inp, c, cs, A, lnp, nonce) for c in cores]
    res = run_bass_kernel_spmd(_prog("fused", build_fused), ims, core_ids=cores)
    out = np.stack([np.concatenate([res.results[2 * b]["t_out"], res.results[2 * b + 1]["t_out"]]) for b in range(B)])
    return out.astype(np.float32)
```

```python
import os
import numpy as np
import concourse.bass as bass
import concourse.mybir as mybir
from concourse.bass_utils import run_bass_kernel_spmd

F32 = mybir.dt.float32
BF16 = mybir.dt.bfloat16
AF = mybir.ActivationFunctionType
ALU = mybir.AluOpType
AX = mybir.AxisListType

NCORES = 8
D = 1024
ALPHA = float((2 * 2) ** 0.25)
LN_EPS = 1e-5


class Buf:
    __slots__ = ("name", "lw", "rs")

    def __init__(self, name):
        self.name = name
        self.lw = None
        self.rs = []


class Sched:
    NDMA = 16

    def __init__(self, nc, shared=None):
        self.nc = nc
        self.ops = []
        self.shared = shared

    def add(self, eng, fn, reads=(), writes=(), dma=False):
        i = len(self.ops)
        deps = set()
        for b in reads:
            if b.lw is not None:
                deps.add(b.lw)
        for b in writes:
            if b.lw is not None:
                deps.add(b.lw)
            deps.update(b.rs)
        for b in reads:
            b.rs.append(i)
        for b in writes:
            b.lw = i
            b.rs = []
        self.ops.append([eng, fn, deps, dma])
        return i

    def pe(self, fn, reads=(), writes=()):
        return self.add("pe", fn, reads, writes)

    def act(self, fn, reads=(), writes=()):
        return self.add("act", fn, reads, writes)

    def dve(self, fn, reads=(), writes=()):
        return self.add("dve", fn, reads, writes)

    def pool(self, fn, reads=(), writes=()):
        return self.add("pool", fn, reads, writes)

    def dma(self, fn, reads=(), writes=(), q="sp"):
        return self.add(q, fn, reads, writes, dma=True)

    def emit(self, stack):
        nc = self.nc
        ops = self.ops
        n = len(ops)
        engs = ["pe", "act", "dve", "pool", "sp"]
        has_dep = [False] * n
        red = []
        pos = [0] * n
        _c = {}
        for i, op in enumerate(ops):
            _c[op[0]] = _c.get(op[0], 0) + 1
            pos[i] = _c[op[0]]
        for i, (eng, fn, deps, dma) in enumerate(ops):
            latest = {}
            dl = []
            for j in deps:
                if ops[j][3]:
                    dl.append(j)
                else:
                    e = ops[j][0]
                    if e not in latest or latest[e] < j:
                        latest[e] = j
            for e, j in latest.items():
                if e == eng and not dma:
                    if eng == "pe":
                        continue
                    if eng in ("act", "dve") and pos[i] - pos[j] >= 2:
                        continue
                dl.append(j)
            red.append(dl)
            for j in dl:
                has_dep[j] = True
        last = {}
        for i, op in enumerate(ops):
            last[op[0]] = i
        for e, i in last.items():
            has_dep[i] = True
        sh = self.shared
        if sh is None:
            sh = {}
        if "sem_eng" not in sh:
            stack = sh.get("stack", stack)
            sh["sem_eng"] = {e: stack.enter_context(nc.semaphore("s_" + e)) for e in engs}
            sh["sem_dma"] = [stack.enter_context(nc.semaphore("s_dma%d" % k)) for k in range(2 * self.NDMA)]
            sh["cnt"] = {e: 0 for e in engs}
            sh["kd"] = [0, 0]
            sh["dma_final"] = {}
        sem_eng, sem_dma = sh["sem_eng"], sh["sem_dma"]
        sig = [None] * n
        prevw = [None] * n
        cnt = dict(sh["cnt"])
        start_cnt = dict(sh["cnt"])
        kd = list(sh["kd"])
        for i, (eng, fn, deps, dma) in enumerate(ops):
            if dma:
                pl = 1 if eng == "pool" else 0
                s = pl * self.NDMA + kd[pl] % self.NDMA
                g = kd[pl] // self.NDMA + 1
                sig[i] = (("d", s), 16 * g)
                if g > 1:
                    prevw[i] = (("d", s), 16 * (g - 1))
                kd[pl] += 1
            elif has_dep[i]:
                cnt[eng] += 1
                sig[i] = (("e", eng), cnt[eng])
        dma_final = dict(sh["dma_final"])
        start_dma = dict(sh["dma_final"])
        for i in range(n):
            if ops[i][3]:
                dma_final[sig[i][0]] = sig[i][1]
        sh["cnt"] = dict(cnt)
        sh["kd"] = kd
        sh["dma_final"] = dict(dma_final)

        def semh(key):
            return sem_dma[key[1]] if key[0] == "d" else sem_eng[key[1]]

        per_eng = {e: [] for e in engs}
        for i, op in enumerate(ops):
            per_eng[op[0]].append(i)

        def run_engine(ename, e):
            waited = {("e", en): v for en, v in start_cnt.items()}
            waited.update(start_dma)
            for i in per_eng[ename]:
                _, fn, _, dma = ops[i]
                need = {}
                for j in red[i]:
                    k, v = sig[j]
                    if need.get(k, 0) < v:
                        need[k] = v
                if prevw[i] is not None:
                    k, v = prevw[i]
                    if need.get(k, 0) < v:
                        need[k] = v
                for k, v in need.items():
                    if waited.get(k, 0) < v:
                        e.wait_ge(semh(k), v)
                        waited[k] = v
                ins = fn(e)
                if sig[i] is not None:
                    k, v = sig[i]
                    ins.then_inc(semh(k), 16 if dma else 1)
            for k, v in dma_final.items():
                if waited.get(k, 0) < v:
                    e.wait_ge(semh(k), v)
            for en in engs:
                if en != ename and cnt[en] > waited.get(("e", en), 0):
                    e.wait_ge(sem_eng[en], cnt[en])

        with nc.Block() as block:
            @block.tensor
            def _(e):
                run_engine("pe", e)

            @block.scalar
            def _(e):
                run_engine("act", e)

            @block.vector
            def _(e):
                run_engine("dve", e)

            @block.gpsimd
            def _(e):
                run_engine("pool", e)

            @block.sync
            def _(e):
                run_engine("sp", e)


class Ctx:
    def __init__(self, nc, stack, shared=None, pfx="", io=None):
        self.nc = nc
        self.stack = stack
        self.S = Sched(nc, shared)
        self.n = 0
        self.pfx = pfx
        self.io = io or {}

    def sb(self, shape, dt, name=None):
        self.n += 1
        name = "sb_" + self.pfx + (name or ("t%d" % self.n))
        t = self.stack.enter_context(self.nc.sbuf_tensor(name, list(shape), dt))
        return t, Buf(name)

    def ps(self, shape, dt, name=None):
        self.n += 1
        name = "ps_" + self.pfx + (name or ("p%d" % self.n))
        t = self.stack.enter_context(self.nc.psum_tensor(name, list(shape), dt))
        return t, Buf(name)

    def din(self, name, shape, dt=F32):
        if name in self.io:
            return self.io[name], Buf(name)
        return self.nc.dram_tensor(self.pfx + name, list(shape), dt, kind="ExternalInput").ap(), Buf(name)

    def dout(self, name, shape, dt=F32):
        if name in self.io:
            return self.io[name], Buf(name)
        return self.nc.dram_tensor(self.pfx + name, list(shape), dt, kind="ExternalOutput").ap(), Buf(name)

    def dscr(self, name, shape, dt=F32):
        return self.nc.dram_tensor(name, list(shape), dt, kind="Internal").ap(), Buf(name)


NT_TAIL = 33
DFF = 2816
DFE = 3584
NEXP = 8


def ln_tile(C, src, srcb, dst, dstb, gt, gtb, bt, btb, tmp):
    S = C.S
    st6, st6b, mv, mvb, rstd, rstdb = tmp
    for h in range(2):
        S.dve(lambda e, h=h: e.bn_stats(out=st6[:, h, :], in_=src[:, h * 512:(h + 1) * 512]), [srcb], [st6b])
    S.dve(lambda e: e.bn_aggr(out=mv[:], in_=st6[:].rearrange("p a b -> p (a b)")), [st6b], [mvb])
    S.act(lambda e: e.activation(out=rstd[:], in_=mv[:, 1:2], func=AF.Ln, bias=LN_EPS, scale=1.0), [mvb], [rstdb])
    S.act(lambda e: e.activation(out=rstd[:], in_=rstd[:], func=AF.Exp, scale=-0.5), [rstdb], [rstdb])
    S.dve(lambda e: e.tensor_scalar(out=dst, in0=src, scalar1=mv[:, 0:1], scalar2=rstd[:, 0:1],
                                    op0=ALU.subtract, op1=ALU.mult), [srcb, mvb, rstdb], [dstb])
    S.pool(lambda e: e.tensor_tensor(out=dst, in0=dst, in1=gt[:], op=ALU.mult), [dstb, gtb], [dstb])
    S.pool(lambda e: e.tensor_tensor(out=dst, in0=dst, in1=bt[:], op=ALU.add), [dstb, btb], [dstb])


def build_tail(nc, stack, stop=None, shared=None, pfx="", io=None, dyn=False):
    C = Ctx(nc, stack, shared, pfx, io)
    S = C.S
    NT = NT_TAIL
    if dyn:
        xpad, xpadb = C.din("xpad", [SEQ + 128, D])
        shmix, shmixb = C.din("shmix", [2, 2, SEQ + 128, 256])
        xin, xinb = C.dscr(pfx + "xts", [NT * 128, D])
        mxs, omixb = C.dscr(pfx + "mxs", [4, NT * 128, 256])
        omix = None

        def dyn_rows(ap):
            return ap[bass.ds((nc.partition_id() % 2) * 4096, NT * 128), :]
        S.dma(lambda e: e.dma_start(out=xin, in_=dyn_rows(xpad)), [xpadb], [xinb], q="act")
        for k in range(4):
            S.dma(lambda e, k=k: e.dma_start(out=mxs[k], in_=dyn_rows(shmix[k % 2, k // 2])), [shmixb], [omixb], q=("act" if k == 0 else "pool"))
    else:
        xin, xinb = C.din("xin", [NT * 128, D])
        omix, omixb = C.din("omix", [NT * 128, D])
    wout_d, woutb = C.din("w_out", [D, D])
    lnp_d, lnpb = C.din("lnp", [9, D])
    wg_d, wgb = C.din("ffn_wg", [D, DFF])
    wu_d, wub = C.din("ffn_wu", [D, DFF])
    wd_d, wdb = C.din("ffn_wd", [DFF, D])
    pw_d, pwb = C.din("pool_w", [4, 256, 256])
    rw_d, rwb = C.din("router_w", [D, NEXP])
    eg_d, egb = C.din("exp_wg", [NEXP, D, DFE])
    eu_d, eub = C.din("exp_wu", [NEXP, D, DFE])
    ed_d, edb = C.din("exp_wd", [NEXP, DFE, D])
    ap_d, apb = C.din("apool", [4, 4, 128, 128])
    id_d, idb = C.din("ident", [128, 128])
    x2s, x2sb = C.dout("x2s", [NT * 128, D])
    out_d, outb = C.dout("out", [(NT - 1) * 128, D])

    ident, identb = C.sb([128, 128], F32, "ident")
    S.dma(lambda e: e.dma_start(out=ident[:], in_=id_d), [idb], [identb])
    lnpt = [C.sb([128, D], F32, "lnp%d" % i) for i in range(5)]
    lnp = {}

    def load_lnp(rows):
        for j, i in enumerate(rows):
            t, b = lnpt[j]
            S.dma(lambda e, t=t, i=i: e.dma_start(out=t[:], in_=lnp_d[i:i + 1, :].to_broadcast([128, D])), [lnpb], [b])
            lnp[i] = (t, b)
    load_lnp([0, 1, 2, 3])
    wout, woutsb = C.sb([128, 8, D], BF16, "wout")
    S.dma(lambda e: e.dma_start(out=wout[:], in_=wout_d.rearrange("(k p) n -> p k n", p=128)), [woutb], [woutsb], q="pool")
    apool, apoolb = C.sb([128, 16, 128], F32, "apool")
    S.dma(lambda e: e.dma_start(out=apool[:], in_=ap_d.rearrange("a w p t -> p (a w) t")), [apb], [apoolb])
    poolw, poolwb = C.sb([128, 8, 256], F32, "poolw")
    S.dma(lambda e: e.dma_start(out=poolw[:], in_=pw_d.rearrange("g (k p) d -> p (g k) d", p=128)), [pwb], [poolwb])
    rw, rwsb = C.sb([128, 8, NEXP], F32, "rw")
    S.dma(lambda e: e.dma_start(out=rw[:], in_=rw_d.rearrange("(k p) n -> p k n", p=128)), [rwb], [rwsb])

    NP = 11
    yacc, yaccb = [], []
    for t in range(NP):
        a, b = C.sb([128, D], F32, "yacc%d" % t)
        yacc.append(a)
        yaccb.append(b)
    xT, xTb = C.sb([128, 8, NP * 128], BF16, "xT")
    gates, gatesb = C.sb([128, NP, NEXP], F32, "gates")
    hT = [C.sb([128, 4, NP * 128], BF16, "hT%d" % i) for i in range(1)]
    wgs = [C.sb([128, 8, 512], BF16, "wgs%d" % i) for i in range(2)]
    wus = [C.sb([128, 8, 512], BF16, "wus%d" % i) for i in range(2)]
    wds = [C.sb([128, 4, D], BF16, "wds%d" % i) for i in range(2)]
    sgt = [C.sb([128, 512], BF16, "sg%d" % i) for i in range(2)]
    NSL = 1
    ta = [C.sb([128, D], F32, "ta%d" % i) for i in range(NSL)]
    tb = [C.sb([128, D], F32, "tb%d" % i) for i in range(NSL)]
    tc_ = [C.sb([128, D], F32, "tc%d" % i) for i in range(NSL)]
    tTb = [C.sb([128, 8, 128], BF16, "tTb%d" % i) for i in range(NSL)]
    tTf = [C.sb([128, 8, 128], F32, "tTf%d" % i) for i in range(NSL)]
    lntmp = []
    for i in range(NSL):
        a = C.sb([128, 2, 6], F32)
        b = C.sb([128, 2], F32)
        c = C.sb([128, 1], F32)
        lntmp.append((a[0], a[1], b[0], b[1], c[0], c[1]))
    sm = [dict(mx=C.sb([128, 8], F32), e=C.sb([128, 8], F32), m=C.sb([128, 8], F32), d=C.sb([128, 1], F32),
               nb=C.sb([128, 1], F32), lg=C.sb([128, 8], F32)) for i in range(NSL)]
    psA = [C.ps([128, 512], F32, "psA%d" % i) for i in range(2)]
    psB = [C.ps([128, 512], F32, "psB%d" % i) for i in range(2)]
    psD = [C.ps([128, 512], F32, "psD%d" % i) for i in range(4)]
    cnt = {"t": 0, "g": 0, "ab": 0, "d": 0}

    def transpose_to(src, srcb, dst_bf=None, dst_bfb=None, dst_f=None, dst_fb=None, dst_bf_ap=None):
        for half in range(2):
            p, pb = psD[cnt["d"] % 4]
            cnt["d"] += 1
            for k in range(4):
                kk = half * 4 + k
                S.pe(lambda e, p=p, k=k, kk=kk: e.transpose(out=p[:, k * 128:(k + 1) * 128], in_=src[:, kk * 128:(kk + 1) * 128],
                                                           identity=ident[:]), [srcb, identb], [pb])
            if dst_bf_ap is not None:
                S.act(lambda e, p=p, half=half: e.copy(out=dst_bf_ap(half), in_=p[:].rearrange("p (a b) -> p a b", a=4)), [pb], [dst_bfb, pb])
            if dst_f is not None:
                S.dve(lambda e, p=p, half=half: e.tensor_copy(out=dst_f[:, half * 4:(half + 1) * 4, :],
                                                              in_=p[:].rearrange("p (a b) -> p a b", a=4)), [pb], [dst_fb])

    def swiglu_pass(ntl, wgd, wgdb, wud, wudb, wdd, wddb, nfc, ne, use_gates):
        ntok = ntl * 128
        blocks = [(s, min(512, ntok - s)) for s in range(0, ntok, 512)]
        for ex in range(ne):
            for c0 in range(0, nfc, 4):
                gc = min(4, nfc - c0)
                slot = cnt["g"] % 2
                cnt["g"] += 1
                wg_t, wg_b = wgs[slot]
                wu_t, wu_b = wus[slot]
                wd_t, wd_b = wds[slot]
                h_t, h_b = hT[0]
                if ne > 1:
                    sg_, su_, sd_ = wgd[ex], wud[ex], wdd[ex]
                else:
                    sg_, su_, sd_ = wgd, wud, wdd
                S.dma(lambda e, wg_t=wg_t, sg_=sg_, c0=c0, gc=gc: e.dma_start(
                    out=wg_t[:, :, 0:gc * 128], in_=sg_[:, c0 * 128:(c0 + gc) * 128].rearrange("(k p) f -> p k f", p=128)),
                    [wgdb], [wg_b], q="pool")
                S.dma(lambda e, wu_t=wu_t, su_=su_, c0=c0, gc=gc: e.dma_start(
                    out=wu_t[:, :, 0:gc * 128], in_=su_[:, c0 * 128:(c0 + gc) * 128].rearrange("(k p) f -> p k f", p=128)),
                    [wudb], [wu_b], q="pool")
                S.dma(lambda e, wd_t=wd_t, sd_=sd_, c0=c0, gc=gc: e.dma_start(
                    out=wd_t[:, 0:gc, :], in_=sd_[c0 * 128:(c0 + gc) * 128, :].rearrange("(c p) d -> p c d", p=128)),
                    [wddb], [wd_b], q="pool")
                for (s0, bw) in blocks:
                    for c in range(gc):
                        ab = cnt["ab"] % 2
                        cnt["ab"] += 1
                        pa, pab = psA[ab]
                        pb_, pbb = psB[ab]
                        sgx, sgb = sgt[ab]
                        for k in range(8):
                            S.pe(lambda e, pa=pa, k=k, c=c, s0=s0, bw=bw, wg_t=wg_t: e.matmul(
                                pa[:, 0:bw], lhsT=wg_t[:, k, c * 128:(c + 1) * 128], rhs=xT[:, k, s0:s0 + bw],
                                start=(k == 0), stop=(k == 7)), [wg_b, xTb], [pab])
                        for k in range(8):
                            S.pe(lambda e, pb_=pb_, k=k, c=c, s0=s0, bw=bw, wu_t=wu_t: e.matmul(
                                pb_[:, 0:bw], lhsT=wu_t[:, k, c * 128:(c + 1) * 128], rhs=xT[:, k, s0:s0 + bw],
                                start=(k == 0), stop=(k == 7)), [wu_b, xTb], [pbb])
                        S.act(lambda e, pa=pa, sgx=sgx, bw=bw: e.activation(out=sgx[:, 0:bw], in_=pa[:, 0:bw], func=AF.Silu),
                              [pab], [sgb])
                        S.dve(lambda e, sgx=sgx, pb_=pb_, h_t=h_t, c=c, s0=s0, bw=bw: e.tensor_tensor(
                            out=h_t[:, c, s0:s0 + bw], in0=sgx[:, 0:bw], in1=pb_[:, 0:bw], op=ALU.mult), [sgb, pbb], [h_b])
                for t in range(ntl):
                    for half in range(2):
                        p, pb2 = psD[cnt["d"] % 4]
                        cnt["d"] += 1
                        for c in range(gc):
                            S.pe(lambda e, p=p, c=c, t=t, half=half, h_t=h_t, wd_t=wd_t, gc=gc: e.matmul(
                                p[:], lhsT=h_t[:, c, t * 128:(t + 1) * 128], rhs=wd_t[:, c, half * 512:(half + 1) * 512],
                                start=(c == 0), stop=(c == gc - 1)), [h_b, wd_b], [pb2])
                        ya = yacc[t]
                        if use_gates:
                            S.dve(lambda e, p=p, ya=ya, t=t, ex=ex, half=half: e.scalar_tensor_tensor(
                                out=ya[:, half * 512:(half + 1) * 512], in0=p[:], scalar=gates[:, t, ex:ex + 1],
                                in1=ya[:, half * 512:(half + 1) * 512], op0=ALU.mult, op1=ALU.add),
                                [pb2, gatesb, yaccb[t]], [yaccb[t]])
                        else:
                            S.dve(lambda e, p=p, ya=ya, half=half: e.tensor_tensor(
                                out=ya[:, half * 512:(half + 1) * 512], in0=p[:], in1=ya[:, half * 512:(half + 1) * 512],
                                op=ALU.add), [pb2, yaccb[t]], [yaccb[t]])

    partsA = [(s, min(NP, NT - s)) for s in range(0, NT, NP)]
    for (t0, ntl) in partsA:
        for tl in range(ntl):
            ti = t0 + tl
            sl = cnt["t"] % NSL
            cnt["t"] += 1
            (om, omb), (xt_, xtb_), (xn, xnb) = ta[sl], tb[sl], tc_[sl]
            oT, oTb = tTb[sl]
            if dyn:
                for k in range(4):
                    S.dma(lambda e, om=om, ti=ti, k=k: e.dma_start(out=om[:, k * 256:(k + 1) * 256], in_=mxs[k, ti * 128:(ti + 1) * 128, :]), [omixb], [omb])
            else:
                S.dma(lambda e, om=om, ti=ti: e.dma_start(out=om[:], in_=omix[ti * 128:(ti + 1) * 128, :]), [omixb], [omb])
            S.dma(lambda e, xt_=xt_, ti=ti: e.dma_start(out=xt_[:], in_=xin[ti * 128:(ti + 1) * 128, :]), [xinb], [xtb_])
            transpose_to(om, omb, dst_bfb=oTb, dst_bf_ap=lambda half, oT=oT: oT[:, half * 4:(half + 1) * 4, :])
            for half in range(2):
                p, pb2 = psD[cnt["d"] % 4]
                cnt["d"] += 1
                for k in range(8):
                    S.pe(lambda e, p=p, k=k, half=half, oT=oT: e.matmul(p[:], lhsT=oT[:, k, :], rhs=wout[:, k, half * 512:(half + 1) * 512],
                                                                        start=(k == 0), stop=(k == 7)), [oTb, woutsb], [pb2])
                S.dve(lambda e, p=p, half=half, xt_=xt_: e.scalar_tensor_tensor(
                    out=xt_[:, half * 512:(half + 1) * 512], in0=xt_[:, half * 512:(half + 1) * 512], scalar=ALPHA, in1=p[:],
                    op0=ALU.mult, op1=ALU.add), [pb2, xtb_], [xtb_])
            ln_tile(C, xt_[:], xtb_, xn[:], xnb, lnp[0][0], lnp[0][1], lnp[1][0], lnp[1][1], lntmp[sl])
            S.act(lambda e, xn=xn, tl=tl: e.mul(out=yacc[tl][:], in_=xn[:], mul=ALPHA), [xnb], [yaccb[tl]])
            transpose_to(xn, xnb, dst_bfb=xTb, dst_bf_ap=lambda half, tl=tl: xT[:, half * 4:(half + 1) * 4, tl * 128:(tl + 1) * 128])
        if stop == "A1":
            S.emit(stack)
            return
        swiglu_pass(ntl, wg_d, wgb, wu_d, wub, wd_d, wdb, DFF // 128, 1, False)
        if stop == "A2":
            S.emit(stack)
            return
        for tl in range(ntl):
            ti = t0 + tl
            sl = cnt["t"] % NSL
            cnt["t"] += 1
            xn, xnb = tc_[sl]
            ln_tile(C, yacc[tl][:], yaccb[tl], xn[:], xnb, lnp[2][0], lnp[2][1], lnp[3][0], lnp[3][1], lntmp[sl])
            S.dma(lambda e, xn=xn, ti=ti: e.dma_start(out=x2s[ti * 128:(ti + 1) * 128, :], in_=xn[:]), [xnb], [x2sb])

    if stop == "A":
        S.emit(stack)
        return
    partsB = [(s, min(NP, NT - s)) for s in range(1, NT, NP)]
    load_lnp([4, 5, 6, 7, 8])
    pmT = [C.sb([128, 8, 128], F32, "pmT%d" % i) for i in range(NSL)]
    for (t0, ntl) in partsB:
        for tl in range(ntl):
            ti = t0 + tl
            sl = cnt["t"] % NSL
            cnt["t"] += 1
            (xc, xcb), (xp, xpb), (xn, xnb) = ta[sl], tb[sl], tc_[sl]
            pm, pmb = pmT[sl]
            xf, xfb = tTf[sl]
            S.dma(lambda e, xc=xc, ti=ti: e.dma_start(out=xc[:], in_=x2s[ti * 128:(ti + 1) * 128, :]), [x2sb], [xcb])
            S.dma(lambda e, xp=xp, ti=ti: e.dma_start(out=xp[:], in_=x2s[(ti - 1) * 128:ti * 128, :]), [x2sb], [xpb])
            first = (ti == 1)
            kp, kc = (0, 1) if first else (2, 3)
            for half in range(2):
                p, pb2 = psD[cnt["d"] % 4]
                cnt["d"] += 1
                for k in range(4):
                    kk = half * 4 + k
                    gi = kk // 2
                    S.pe(lambda e, p=p, k=k, kk=kk, gi=gi, xp=xp, kp=kp: e.matmul(
                        p[:, k * 128:(k + 1) * 128], lhsT=xp[:, kk * 128:(kk + 1) * 128], rhs=apool[:, kp * 4 + gi, :],
                        start=True, stop=False), [xpb, apoolb], [pb2])
                    S.pe(lambda e, p=p, k=k, kk=kk, gi=gi, xc=xc, kc=kc: e.matmul(
                        p[:, k * 128:(k + 1) * 128], lhsT=xc[:, kk * 128:(kk + 1) * 128], rhs=apool[:, kc * 4 + gi, :],
                        start=False, stop=True), [xcb, apoolb], [pb2])
                S.act(lambda e, p=p, half=half, pm=pm: e.copy(out=pm[:, half * 4:(half + 1) * 4, :],
                                                             in_=p[:].rearrange("p (a b) -> p a b", a=4)), [pb2], [pmb])
            for half in range(2):
                p, pb2 = psD[cnt["d"] % 4]
                cnt["d"] += 1
                for g2 in range(2):
                    gi = half * 2 + g2
                    for k in range(2):
                        S.pe(lambda e, p=p, g2=g2, gi=gi, k=k, pm=pm: e.matmul(
                            p[:, g2 * 256:(g2 + 1) * 256], lhsT=pm[:, gi * 2 + k, :], rhs=poolw[:, gi * 2 + k, :],
                            start=(k == 0), stop=(k == 1)), [pmb, poolwb], [pb2])
                S.dve(lambda e, p=p, half=half, xp=xp: e.tensor_tensor(
                    out=xp[:, half * 512:(half + 1) * 512], in0=p[:], in1=lnp[4][0][:, half * 512:(half + 1) * 512], op=ALU.mult),
                    [pb2, lnp[4][1]], [xpb])
            S.dve(lambda e, xc=xc, xp=xp: e.scalar_tensor_tensor(out=xc[:], in0=xc[:], scalar=ALPHA, in1=xp[:],
                                                                 op0=ALU.mult, op1=ALU.add), [xcb, xpb], [xcb])
            ln_tile(C, xc[:], xcb, xn[:], xnb, lnp[5][0], lnp[5][1], lnp[6][0], lnp[6][1], lntmp[sl])
            S.act(lambda e, xn=xn, tl=tl: e.mul(out=yacc[tl][:], in_=xn[:], mul=ALPHA), [xnb], [yaccb[tl]])
            transpose_to(xn, xnb, dst_bfb=xTb, dst_bf_ap=lambda half, tl=tl: xT[:, half * 4:(half + 1) * 4, tl * 128:(tl + 1) * 128],
                         dst_f=xf, dst_fb=xfb)
            p, pb2 = psD[cnt["d"] % 4]
            cnt["d"] += 1
            for k in range(8):
                S.pe(lambda e, p=p, k=k, xf=xf: e.matmul(p[:, 0:NEXP], lhsT=xf[:, k, :], rhs=rw[:, k, :], start=(k == 0), stop=(k == 7)),
                     [xfb, rwsb], [pb2])
            q = sm[sl]
            lg, lgb = q["lg"]
            mx, mxb = q["mx"]
            ee, eeb = q["e"]
            mm, mmb = q["m"]
            dd, ddb = q["d"]
            nb, nbb = q["nb"]
            S.dve(lambda e, p=p, lg=lg: e.tensor_copy(out=lg[:], in_=p[:, 0:NEXP]), [pb2], [lgb])
            S.dve(lambda e, lg=lg, mx=mx: e.max(out=mx[:], in_=lg[:]), [lgb], [mxb])
            S.dve(lambda e, nb=nb, mx=mx: e.tensor_scalar(out=nb[:], in0=mx[:, 0:1], scalar1=-1.0, scalar2=None, op0=ALU.mult), [mxb], [nbb])
            S.act(lambda e, ee=ee, lg=lg, nb=nb: e.activation(out=ee[:], in_=lg[:], func=AF.Exp, bias=nb[:, 0:1], scale=1.0), [lgb, nbb], [eeb])
            S.act(lambda e, dd=dd, mx=mx, nb=nb: e.activation(out=dd[:], in_=mx[:, 1:2], func=AF.Exp, bias=nb[:, 0:1], scale=1.0), [mxb, nbb], [ddb])
            S.dve(lambda e, dd=dd: e.tensor_scalar(out=dd[:], in0=dd[:], scalar1=1.0, scalar2=None, op0=ALU.add), [ddb], [ddb])
            S.dve(lambda e, dd=dd: e.reciprocal(out=dd[:], in_=dd[:]), [ddb], [ddb])
            S.dve(lambda e, mm=mm, lg=lg, mx=mx: e.tensor_scalar(out=mm[:], in0=lg[:], scalar1=mx[:, 1:2], scalar2=None, op0=ALU.is_ge), [lgb, mxb], [mmb])
            S.dve(lambda e, mm=mm, ee=ee: e.tensor_tensor(out=mm[:], in0=mm[:], in1=ee[:], op=ALU.mult), [mmb, eeb], [mmb])
            S.dve(lambda e, mm=mm, dd=dd, tl=tl: e.tensor_scalar(out=gates[:, tl, :], in0=mm[:], scalar1=dd[:, 0:1], scalar2=None, op0=ALU.mult),
                  [mmb, ddb], [gatesb])
        if stop == "B1":
            S.emit(stack)
            return
        swiglu_pass(ntl, eg_d, egb, eu_d, eub, ed_d, edb, DFE // 128, NEXP, True)
        for tl in range(ntl):
            ti = t0 + tl
            sl = cnt["t"] % NSL
            cnt["t"] += 1
            xn, xnb = tc_[sl]
            ln_tile(C, yacc[tl][:], yaccb[tl], xn[:], xnb, lnp[7][0], lnp[7][1], lnp[8][0], lnp[8][1], lntmp[sl])
            S.dma(lambda e, xn=xn, ti=ti: e.dma_start(out=out_d[(ti - 1) * 128:ti * 128, :], in_=xn[:]), [xnb], [outb])
    S.emit(stack)


def pool_consts():
    A = np.zeros((4, 4, 128, 128), np.float32)
    for wi, w in enumerate((2, 4, 8, 16)):
        for t in range(128):
            for j in range(w):
                tp = t - j
                if tp >= 0:
                    A[3, wi, tp, t] += 1.0 / w
                else:
                    A[2, wi, 128 + tp, t] += 1.0 / w
            A[3, wi, t, t] -= 1.0
            c = min(t + 1, w)
            for j in range(c):
                A[1, wi, t - j, t] += 1.0 / c
            A[1, wi, t, t] -= 1.0
    return A


SEQ = 8192
GD = 128
CH = 64


class PsumRing:
    def __init__(self, C, n=8):
        self.t = [C.ps([128, 512], F32, "bank%d" % i) for i in range(n)]
        self.i = 0

    def __call__(self):
        r = self.t[self.i % len(self.t)]
        self.i += 1
        return r


def load_xT_block(C, PS, xb_d, xbb, t0, ident, identb, xtile, xT, xTb):
    S = C.S
    for j in range(4):
        xt_, xtb_ = xtile[j % len(xtile)]
        S.dma(lambda e, xt_=xt_, j=j: e.dma_start(out=xt_[:], in_=xb_d[t0 + j * 128:t0 + (j + 1) * 128, :]), [xbb], [xtb_])
        for half in range(2):
            p, pb = PS()
            for k in range(4):
                kk = half * 4 + k
                S.pe(lambda e, p=p, k=k, kk=kk, xt_=xt_: e.transpose(out=p[:, k * 128:(k + 1) * 128], in_=xt_[:, kk * 128:(kk + 1) * 128],
                                                                     identity=ident[:]), [xtb_, identb], [pb])
            S.act(lambda e, p=p, half=half, j=j: e.copy(out=xT[:, half * 4:(half + 1) * 4, j * 128:(j + 1) * 128],
                                                        in_=p[:].rearrange("p (a b) -> p a b", a=4)), [pb], [xTb])


def build_gdn(nc, stack, nblk=SEQ // 512, shared=None, pfx="", io=None, ocol=0, NH=2):
    C = Ctx(nc, stack, shared, pfx, io)
    S = C.S
    PS = PsumRing(C)
    NC3 = 3 * NH
    xb_d, xbb = C.din("xb", [SEQ, D])
    wqkv_d, wqkvb = C.din("wqkv", [D, NC3 * 128])
    wz_d, wzb = C.din("wz", [D, NH * 128])
    wbg_d, wbgb = C.din("wbg", [D, 2 * NH])
    convw_d, convwb = C.din("convw", [NC3 * 128, 4])
    hp_d, hpb = C.din("hparm", [1, 2 * NH])
    nw_d, nwb = C.din("normw", [1, 128])
    cst_d, cstb = C.din("gcst", [6, 128, 128])
    ob_d, obb = C.dout("o_b", [SEQ, NH * 128])

    cst, cstsb = C.sb([128, 6, 128], F32, "cst")
    S.dma(lambda e: e.dma_start(out=cst[:], in_=cst_d.rearrange("a p t -> p a t")), [cstb], [cstsb])
    ident = cst[:, 0, :]
    identb = cstsb
    tri = cst[0:64, 1, 0:64]
    ones = cst[:, 2, :]
    mneg = cst[0:64, 3, 0:64]
    negst = cst[0:64, 4, 0:64]
    id64 = cst[0:64, 0, 0:64]
    wqkv, wqkvsb = C.sb([128, 8, NC3 * 128], BF16, "wqkv")
    S.dma(lambda e: e.dma_start(out=wqkv[:], in_=wqkv_d.rearrange("(k p) n -> p k n", p=128)), [wqkvb], [wqkvsb], q="pool")
    wz, wzsb = C.sb([128, 8, NH * 128], BF16, "wz")
    S.dma(lambda e: e.dma_start(out=wz[:], in_=wz_d.rearrange("(k p) n -> p k n", p=128)), [wzb], [wzsb], q="pool")
    wbg, wbgsb = C.sb([128, 8, 2 * NH], BF16, "wbg")
    S.dma(lambda e: e.dma_start(out=wbg[:], in_=wbg_d.rearrange("(k p) n -> p k n", p=128)), [wbgb], [wbgsb], q="pool")
    convw, convwsb = C.sb([128, NC3, 4], F32, "convw")
    S.dma(lambda e: e.dma_start(out=convw[:], in_=convw_d.rearrange("(c p) j -> p c j", p=128)), [convwb], [convwsb])
    hp, hpsb = C.sb([64, 2 * NH], F32, "hp")
    S.dma(lambda e: e.dma_start(out=hp[:], in_=hp_d.to_broadcast([64, 2 * NH])), [hpb], [hpsb])
    nw, nwsb = C.sb([64, 128], F32, "nw")
    S.dma(lambda e: e.dma_start(out=nw[:], in_=nw_d.to_broadcast([64, 128])), [nwb], [nwsb])
    nalog, nalogb = C.sb([64, NH], F32, "nalog")
    S.act(lambda e: e.activation(out=nalog[:], in_=hp[:, 0:NH], func=AF.Exp), [hpsb], [nalogb])
    S.dve(lambda e: e.tensor_scalar(out=nalog[:], in0=nalog[:], scalar1=-1.0, scalar2=None, op0=ALU.mult), [nalogb], [nalogb])

    xtile = [C.sb([128, D], F32, "xtile%d" % i) for i in range(2)]
    xT, xTb = C.sb([128, 8, 512], BF16, "xT")
    cin, cinb = C.sb([128, NC3, 515], F32, "cin")
    S.pool(lambda e: e.memset(cin[:].rearrange("p a b -> p (a b)"), 0.0), [], [cinb])
    cacc = [C.sb([128, 512], F32, "cacc%d" % i) for i in range(2)]
    qkv = [C.sb([128, 512], F32, "qkv%d" % i) for i in range(NC3)]
    sq = [C.sb([128, 512], F32, "sq%d" % i) for i in range(2)]
    rn = [C.sb([128, 512], F32, "rn%d" % i) for i in range(2)]
    zs, zsb = C.sb([64, 8, NH * 128], F32, "zs")
    bg, bgb = C.sb([64, 8, 2 * NH], F32, "bg")
    beta, betab = C.sb([64, 8, NH], F32, "beta")
    gg, ggb = C.sb([64, 8, NH], F32, "gg")
    gc, gcb = C.sb([64, 8, NH], F32, "gc")
    ngc, ngcb = C.sb([64, 8, NH], F32, "ngc")
    egc, egcb = C.sb([64, 8, NH], F32, "egc")
    negc, negcb = C.sb([64, 8, NH], F32, "negc")
    eglg, eglgb = C.sb([64, 8, NH], F32, "eglg")
    egl, eglb = C.sb([128, 8, NH], F32, "egl")
    St = [C.sb([128, 128], F32, "state%d" % h) for h in range(NH)]
    for h in range(NH):
        S.pool(lambda e, h=h: e.memset(St[h][0][:], 0.0), [], [St[h][1]])
    CG = 2
    NCH = 2 * CG * NH

    def ring(shape, name, n):
        return [C.sb(shape, F32, "%s%d" % (name, i)) for i in range(n)]
    NT_ = CG * NH
    ktok = ring([64, 128], "ktok", NT_)
    dgc = ring([64, 64], "dgc", NT_)
    DT = ring([64, 64], "DT", NT_)
    X = ring([64, 64], "X", NT_)
    XT = ring([64, 64], "XT", NT_)
    G1s = ring([64, 64], "G1s", NT_)
    G2s = ring([64, 64], "G2s", NT_)
    vtok = ring([64, 128], "vtok", NCH)
    kpp = ring([64, 128], "kpp", NCH)
    Y = ring([64, 64], "Y", NCH)
    IT = ring([64, 64], "IT", NCH)
    NS_ = NH
    Rr = ring([64, 128], "R", NS_)
    vn = ring([64, 128], "vn", NS_)
    ivt = ring([64, 128], "ivt", NS_)
    ot = ring([64, 128], "ot", NS_)
    ss = ring([64, 1], "ss", NS_)
    osq = ring([64, 128], "osq", NS_)
    ofin = ring([64, NH * 128], "ofin", 2)
    tctr = {"i": 0}

    def drive(gens):
        gens = list(gens)
        while gens:
            for g in list(gens):
                try:
                    next(g)
                except StopIteration:
                    gens.remove(g)

    for blk in range(nblk):
        t0 = blk * 512
        load_xT_block(C, PS, xb_d, xbb, t0, ident, identb, xtile, xT, xTb)
        for c in range(NC3):
            p, pb = PS()
            for k in range(8):
                S.pe(lambda e, p=p, k=k, c=c: e.matmul(p[:], lhsT=wqkv[:, k, c * 128:(c + 1) * 128], rhs=xT[:, k, :],
                                                       start=(k == 0), stop=(k == 7)), [wqkvsb, xTb], [pb])
            S.act(lambda e, p=p, c=c: e.copy(out=cin[:, c, 3:515], in_=p[:]), [pb], [cinb])
        for c in range(NC3):
            ca, cab = cacc[c % 2]
            eng = S.dve
            eng(lambda e, ca=ca, c=c: e.tensor_scalar(out=ca[:], in0=cin[:, c, 0:512], scalar1=convw[:, c, 0:1], scalar2=None, op0=ALU.mult),
                [cinb, convwsb], [cab])
            for j in range(1, 4):
                eng(lambda e, ca=ca, c=c, j=j: e.scalar_tensor_tensor(out=ca[:], in0=cin[:, c, j:j + 512], scalar=convw[:, c, j:j + 1], in1=ca[:],
                                                                       op0=ALU.mult, op1=ALU.add), [cinb, convwsb, cab], [cab])
            S.act(lambda e, ca=ca, c=c: e.activation(out=qkv[c][0][:], in_=ca[:], func=AF.Silu), [cab], [qkv[c][1]])
        S.pool(lambda e: e.tensor_copy(out=cin[:, :, 0:3], in_=cin[:, :, 512:515]), [cinb], [cinb])
        for c in range(2 * NH):
            sq_, sqb = sq[c % 2]
            rn_, rnb = rn[c % 2]
            S.pool(lambda e, sq_=sq_, c=c: e.tensor_tensor(out=sq_[:], in0=qkv[c][0][:], in1=qkv[c][0][:], op=ALU.mult), [qkv[c][1]], [sqb])
            p, pb = PS()
            S.pe(lambda e, p=p, sq_=sq_: e.matmul(p[:], lhsT=ones, rhs=sq_[:], start=True, stop=True), [sqb, cstsb], [pb])
            S.act(lambda e, p=p, rn_=rn_: e.activation(out=rn_[:], in_=p[:], func=AF.Ln, bias=1e-6, scale=1.0), [pb], [rnb])
            S.act(lambda e, rn_=rn_: e.activation(out=rn_[:], in_=rn_[:], func=AF.Exp, scale=-0.5), [rnb], [rnb])
            sc = float(GD ** -0.5) if c < NH else 1.0
            S.dve(lambda e, rn_=rn_, c=c, sc=sc: e.scalar_tensor_tensor(out=qkv[c][0][:], in0=qkv[c][0][:], scalar=sc, in1=rn_[:],
                                                                        op0=ALU.mult, op1=ALU.mult), [qkv[c][1], rnb], [qkv[c][1]])
        for cc in range(8):
            p, pb = PS()
            for k in range(8):
                S.pe(lambda e, p=p, k=k, cc=cc: e.matmul(p[0:64, 0:NH * 128], lhsT=xT[:, k, cc * 64:(cc + 1) * 64], rhs=wz[:, k, :],
                                                         start=(k == 0), stop=(k == 7)), [xTb, wzsb], [pb])
            S.act(lambda e, p=p, cc=cc: e.activation(out=zs[:, cc, :], in_=p[0:64, 0:NH * 128], func=AF.Silu), [pb], [zsb])
            p2, pb2 = PS()
            for k in range(8):
                S.pe(lambda e, p2=p2, k=k, cc=cc: e.matmul(p2[0:64, 0:2 * NH], lhsT=xT[:, k, cc * 64:(cc + 1) * 64], rhs=wbg[:, k, :],
                                                           start=(k == 0), stop=(k == 7)), [xTb, wbgsb], [pb2])
            S.dve(lambda e, p2=p2, cc=cc: e.tensor_copy(out=bg[:, cc, :], in_=p2[0:64, 0:2 * NH]), [pb2], [bgb])
        S.act(lambda e: e.activation(out=beta[:], in_=bg[:, :, 0:NH], func=AF.Sigmoid), [bgb], [betab])
        for h in range(NH):
            S.act(lambda e, h=h: e.activation(out=gg[:, :, h], in_=bg[:, :, NH + h], func=AF.Exp, bias=hp[:, NH + h:NH + h + 1], scale=1.0),
                  [bgb, hpsb], [ggb])
        S.act(lambda e: e.activation(out=gg[:], in_=gg[:], func=AF.Ln, bias=1.0, scale=1.0), [ggb], [ggb])
        for h in range(NH):
            S.dve(lambda e, h=h: e.tensor_scalar(out=gg[:, :, h], in0=gg[:, :, h], scalar1=nalog[:, h:h + 1], scalar2=None, op0=ALU.mult),
                  [ggb, nalogb], [ggb])
        ggf = gg[:].rearrange("p a b -> p (a b)")
        NG = 8 * NH
        p, pb = PS()
        S.pe(lambda e, p=p: e.matmul(p[0:64, 0:NG], lhsT=tri, rhs=ggf, start=True, stop=True), [ggb, cstsb], [pb])
        S.dve(lambda e, p=p: e.tensor_copy(out=gc[:].rearrange("p a b -> p (a b)"), in_=p[0:64, 0:NG]), [pb], [gcb])
        S.dve(lambda e: e.tensor_scalar(out=ngc[:], in0=gc[:], scalar1=-1.0, scalar2=None, op0=ALU.mult), [gcb], [ngcb])
        S.act(lambda e: e.activation(out=egc[:], in_=gc[:], func=AF.Exp), [gcb], [egcb])
        S.dve(lambda e: e.tensor_scalar(out=negc[:], in0=egc[:], scalar1=-1.0, scalar2=None, op0=ALU.mult), [egcb], [negcb])
        p, pb = PS()
        S.pe(lambda e, p=p: e.matmul(p[:, 0:NG], lhsT=ones[0:64, :], rhs=ggf, start=True, stop=True), [ggb, cstsb], [pb])
        S.act(lambda e, p=p: e.activation(out=egl[:].rearrange("p a b -> p (a b)"), in_=p[:, 0:NG], func=AF.Exp), [pb], [eglb, pb])
        S.dve(lambda e, p=p: e.tensor_tensor(out=eglg[:].rearrange("p a b -> p (a b)"), in0=p[0:64, 0:NG],
                                             in1=gc[:].rearrange("p a b -> p (a b)"), op=ALU.subtract), [pb, gcb], [eglgb])
        S.act(lambda e: e.activation(out=eglg[:], in_=eglg[:], func=AF.Exp), [eglgb], [eglgb])

        def pre(cc, h):
            csl = slice(cc * 64, (cc + 1) * 64)
            ch = (cc % (2 * CG)) * NH + h
            r = tctr["i"]
            tctr["i"] += 1
            qT, qTb = qkv[h]
            kT, kTb = qkv[NH + h]
            vT, vTb = qkv[2 * NH + h]
            kt, ktb = ktok[r % NT_]
            dg, dgb = dgc[r % NT_]
            dt_, dtb = DT[r % NT_]
            x_, xb_ = X[r % NT_]
            xt_, xtb_ = XT[r % NT_]
            g1, g1b = G1s[r % NT_]
            g2, g2b = G2s[r % NT_]
            vt, vtb = vtok[ch]
            kp_, kpb = kpp[ch]
            y_, yb_ = Y[ch]
            it_, itb = IT[ch]
            bcol = beta[:, cc, h:h + 1]
            p, pb = PS()
            S.pe(lambda e: e.transpose(out=p[0:64, 0:128], in_=kT[:, csl], identity=ident), [kTb, cstsb], [pb])
            S.dve(lambda e: e.tensor_copy(out=kt[:], in_=p[0:64, 0:128]), [pb], [ktb])
            p2, pb2 = PS()
            S.pe(lambda e: e.transpose(out=p2[0:64, 0:128], in_=vT[:, csl], identity=ident), [vTb, cstsb], [pb2])
            S.act(lambda e: e.copy(out=vt[:], in_=p2[0:64, 0:128]), [pb2], [vtb])
            S.pool(lambda e: e.tensor_scalar(out=dg[:], in0=id64, scalar1=gc[:, cc, h:h + 1], scalar2=None, op0=ALU.mult), [gcb, cstsb], [dgb])
            yield
            p3, pb3 = PS()
            S.pe(lambda e: e.matmul(p3[0:64, 0:64], lhsT=ones[0:64, 0:64], rhs=dg[:], start=True, stop=False), [dgb, cstsb], [pb3])
            S.pe(lambda e: e.matmul(p3[0:64, 0:64], lhsT=id64, rhs=mneg, start=False, stop=True), [cstsb], [pb3])
            S.act(lambda e: e.activation(out=dt_[:], in_=p3[0:64, 0:64], func=AF.Exp, bias=ngc[:, cc, h:h + 1], scale=1.0), [pb3, ngcb], [dtb])
            p4, pb4 = PS()
            S.pe(lambda e: e.matmul(p4[0:64, 0:64], lhsT=kT[:, csl], rhs=kT[:, csl], start=True, stop=True), [kTb], [pb4])
            S.dve(lambda e: e.tensor_scalar(out=g1[:], in0=p4[0:64, 0:64], scalar1=bcol, scalar2=None, op0=ALU.mult), [pb4, betab], [g1b])
            p5, pb5 = PS()
            S.pe(lambda e: e.matmul(p5[0:64, 0:64], lhsT=kT[:, csl], rhs=qT[:, csl], start=True, stop=True), [kTb, qTb], [pb5])
            S.dve(lambda e: e.tensor_copy(out=g2[:], in_=p5[0:64, 0:64]), [pb5], [g2b])
            S.pool(lambda e: e.tensor_scalar(out=kp_[:], in0=kt[:], scalar1=eglg[:, cc, h:h + 1], scalar2=None, op0=ALU.mult), [ktb, eglgb], [kpb])
            yield
            S.dve(lambda e: e.tensor_tensor(out=x_[:], in0=g1[:], in1=dt_[:], op=ALU.mult), [g1b, dtb], [xb_])
            S.dve(lambda e: e.tensor_tensor(out=x_[:], in0=x_[:], in1=negst, op=ALU.mult), [xb_, cstsb], [xb_])
            S.pool(lambda e: e.tensor_tensor(out=it_[:], in0=g2[:], in1=dt_[:], op=ALU.mult), [g2b, dtb], [itb])
            yield
            p6, pb6 = PS()
            S.pe(lambda e: e.transpose(out=p6[0:64, 0:64], in_=x_[:], identity=id64), [xb_, cstsb], [pb6])
            S.act(lambda e: e.copy(out=xt_[:], in_=p6[0:64, 0:64]), [pb6], [xtb_])
            S.pool(lambda e: e.tensor_tensor(out=y_[:], in0=x_[:], in1=id64, op=ALU.add), [xb_, cstsb], [yb_])
            yield
            for m in range(5):
                pt, ptb = PS()
                S.pe(lambda e, pt=pt: e.matmul(pt[0:64, 0:64], lhsT=x_[:], rhs=xt_[:], start=True, stop=True), [xb_, xtb_], [ptb])
                if m < 4:
                    pa, pab = PS()
                    S.pe(lambda e, pa=pa: e.matmul(pa[0:64, 0:64], lhsT=xt_[:], rhs=x_[:], start=True, stop=True), [xb_, xtb_], [pab])
                    S.dve(lambda e, pa=pa: e.tensor_copy(out=x_[:], in_=pa[0:64, 0:64]), [pab], [xb_])
                S.act(lambda e, pt=pt: e.copy(out=xt_[:], in_=pt[0:64, 0:64]), [ptb], [xtb_])
                yield
                py, pyb = PS()
                S.pe(lambda e, py=py: e.matmul(py[0:64, 0:64], lhsT=xt_[:], rhs=y_[:], start=True, stop=True), [xtb_, yb_], [pyb])
                S.dve(lambda e, py=py: e.tensor_tensor(out=y_[:], in0=py[0:64, 0:64], in1=y_[:], op=ALU.add), [pyb, yb_], [yb_])
                yield

        def scan(cc, h, of, ofb):
            csl = slice(cc * 64, (cc + 1) * 64)
            ch = (cc % (2 * CG)) * NH + h
            qT, qTb = qkv[h]
            kT, kTb = qkv[NH + h]
            s_, sb_ = St[h]
            vt, vtb = vtok[ch]
            kp_, kpb = kpp[ch]
            y_, yb_ = Y[ch]
            it_, itb = IT[ch]
            R_, Rb = Rr[h]
            vn_, vnb = vn[h]
            qs_, qsb = ivt[h]
            o_, ob_ = ot[h]
            ss_, ssb = ss[h]
            oq_, oqb = osq[h]
            bcol = beta[:, cc, h:h + 1]
            p, pb = PS()
            S.pe(lambda e: e.matmul(p[0:64, 0:128], lhsT=kT[:, csl], rhs=s_[:], start=True, stop=True), [kTb, sb_], [pb])
            S.dve(lambda e: e.scalar_tensor_tensor(out=R_[:], in0=p[0:64, 0:128], scalar=negc[:, cc, h:h + 1], in1=vt[:], op0=ALU.mult, op1=ALU.add),
                  [pb, negcb, vtb], [Rb])
            pq, pqb = PS()
            S.pe(lambda e: e.matmul(pq[0:64, 0:128], lhsT=qT[:, csl], rhs=s_[:], start=True, stop=True), [qTb, sb_], [pqb])
            S.dve(lambda e: e.tensor_scalar(out=qs_[:], in0=pq[0:64, 0:128], scalar1=egc[:, cc, h:h + 1], scalar2=None, op0=ALU.mult), [pqb, egcb], [qsb])
            yield
            p2, pb2 = PS()
            S.pe(lambda e: e.matmul(p2[0:64, 0:128], lhsT=y_[:], rhs=R_[:], start=True, stop=True), [yb_, Rb], [pb2])
            S.dve(lambda e: e.tensor_scalar(out=vn_[:], in0=p2[0:64, 0:128], scalar1=bcol, scalar2=None, op0=ALU.mult), [pb2, betab], [vnb])
            yield
            p3, pb3 = PS()
            S.pe(lambda e: e.matmul(p3[:, 0:128], lhsT=kp_[:], rhs=vn_[:], start=True, stop=True), [kpb, vnb], [pb3])
            S.dve(lambda e: e.scalar_tensor_tensor(out=s_[:], in0=s_[:], scalar=egl[:, cc, h:h + 1], in1=p3[:, 0:128], op0=ALU.mult, op1=ALU.add),
                  [pb3, eglb, sb_], [sb_])
            p4, pb4 = PS()
            S.pe(lambda e: e.matmul(p4[0:64, 0:128], lhsT=it_[:], rhs=vn_[:], start=True, stop=True), [itb, vnb], [pb4])
            S.dve(lambda e: e.tensor_tensor(out=o_[:], in0=p4[0:64, 0:128], in1=qs_[:], op=ALU.add), [pb4, qsb], [ob_])
            yield
            S.pool(lambda e: e.tensor_tensor(out=oq_[:], in0=o_[:], in1=o_[:], op=ALU.mult), [ob_], [oqb])
            yield
            S.dve(lambda e: e.reduce_sum(out=ss_[:], in_=oq_[:], axis=AX.X), [oqb], [ssb])
            yield
            S.act(lambda e: e.activation(out=ss_[:], in_=ss_[:], func=AF.Ln, bias=1e-6, scale=1.0 / GD), [ssb], [ssb])
            S.act(lambda e: e.activation(out=ss_[:], in_=ss_[:], func=AF.Exp, scale=-0.5), [ssb], [ssb])
            yield
            S.dve(lambda e: e.scalar_tensor_tensor(out=o_[:], in0=o_[:], scalar=ss_[:, 0:1], in1=nw[:], op0=ALU.mult, op1=ALU.mult),
                  [ob_, ssb, nwsb], [ob_])
            yield
            S.pool(lambda e: e.tensor_tensor(out=of[:, h * 128:(h + 1) * 128], in0=o_[:], in1=zs[:, cc, h * 128:(h + 1) * 128], op=ALU.mult),
                   [ob_, zsb], [ofb])

        def scan_seq(ccs, t0=t0):
            for cc in ccs:
                of, ofb = ofin[cc % 2]
                gens = [scan(cc, h, of, ofb) for h in range(NH)]
                while gens:
                    for g in list(gens):
                        try:
                            next(g)
                        except StopIteration:
                            gens.remove(g)
                    yield
                S.dma(lambda e, of=of, cc=cc, t0=t0: e.dma_start(out=ob_d[t0 + cc * 64:t0 + (cc + 1) * 64, ocol:ocol + NH * 128], in_=of[:]), [ofb], [obb])

        NGRP = 8 // CG
        grp = lambda g: list(range(g * CG, (g + 1) * CG))
        drive([pre(cc, h) for cc in grp(0) for h in range(NH)])
        for g in range(NGRP):
            gens = [scan_seq(grp(g))]
            if g + 1 < NGRP:
                gens += [pre(cc, h) for cc in grp(g + 1) for h in range(NH)]
            drive(gens)
    S.emit(stack)


def gdn_consts():
    c = np.zeros((6, 128, 128), np.float32)
    c[0] = np.eye(128)
    k = np.arange(128)
    c[1] = (k[:, None] <= k[None, :]).astype(np.float32)
    c[2] = 1.0
    c[3] = np.where(k[None, :] < k[:, None], -30000.0, 0.0)
    c[4] = np.where(k[None, :] > k[:, None], -1.0, 0.0)
    return c


def gdn_inputs(inp, b, half, heads=None):
    if heads is None:
        heads = [2 * half, 2 * half + 1]
    w_in = inp["ev_w_in"][0]
    o_gq = 512 + 6 * 128 + 24
    o_gk, o_gv, o_gz = o_gq + 512, o_gq + 1024, o_gq + 1536
    o_gb = o_gq + 2048
    o_ga = o_gb + 4
    hc = lambda off: np.concatenate([w_in[:, off + h * 128: off + (h + 1) * 128] for h in heads], 1)
    wqkv = np.concatenate([hc(o_gq), hc(o_gk), hc(o_gv)], 1)
    wz = hc(o_gz)
    wbg = np.concatenate([w_in[:, [o_gb + h for h in heads]], w_in[:, [o_ga + h for h in heads]]], 1)
    cw = inp["ev_conv_w"][0]
    cc = lambda off: np.concatenate([cw[:, off + h * 128: off + (h + 1) * 128] for h in heads], 1)
    convw = np.concatenate([cc(0), cc(512), cc(1024)], 1).T
    hparm = np.concatenate([inp["ev_a_log"][0][heads], inp["ev_dt_bias"][0][heads]])[None, :]
    return {"xb": np.ascontiguousarray(inp["x"][b]), "wqkv": np.ascontiguousarray(wqkv), "wz": np.ascontiguousarray(wz),
            "wbg": np.ascontiguousarray(wbg), "convw": np.ascontiguousarray(convw), "hparm": np.ascontiguousarray(hparm),
            "normw": np.ascontiguousarray(inp["ev_gdn_norm"][0][None, :]), "gcst": gdn_consts()}


def build_nsa(nc, stack, nqblk=SEQ // 512, shared=None, pfx="", io=None, ocol=0):
    C = Ctx(nc, stack, shared, pfx, io)
    S = C.S
    PS = PsumRing(C, 4)
    ACC = [C.ps([128, 512], F32, "acc%d" % i) for i in range(4)]
    xb_d, xbb = C.din("xb", [SEQ, D])
    wq_d, wqb = C.din("wq", [D, 1024])
    wk_d, wkb = C.din("wk", [D, 384])
    wv_d, wvb = C.din("wv", [D, 128])
    wgt_d, wgtb = C.din("wgt", [D, 12])
    cwk_d, cwkb = C.din("cmpwk", [2, 64, 32, 64])
    cwv_d, cwvb = C.din("cmpwv", [128, 32, 64])
    pek_d, pekb = C.din("cmppek", [64, 32])
    pev_d, pevb = C.din("cmppev", [128, 32])
    rope_d, ropeb = C.din("rope", [2, 128, SEQ])
    ropek_d, ropekb = C.din("ropek", [2, 64, 512])
    E_d, Eb = C.din("Eexp", [128, 32, 128])
    mk_d, mkb = C.din("masks", [8, 128, 512])
    cmk_d, cmkb = C.din("cmask", [17, 128, 128])
    ov_d, ovb = C.din("overlap", [512, 128])
    W_d, Wb = C.din("forceW", [128, 256])
    id_d, idb = C.din("ident", [128, 128])
    oa_d, oab = C.dout("o_a", [SEQ, 256])

    ident, identb = C.sb([128, 128], F32, "ident")
    S.dma(lambda e: e.dma_start(out=ident[:], in_=id_d), [idb], [identb])
    if "zero_rows" in C.io:
        for (zdst, zsrc) in C.io["zero_rows"]:
            S.dma(lambda e, zdst=zdst, zsrc=zsrc: e.dma_start(out=zdst, in_=zsrc), [], [oab])
    wq, wqsb = C.sb([128, 8, 1024], BF16, "wq")
    S.dma(lambda e: e.dma_start(out=wq[:], in_=wq_d.rearrange("(k p) n -> p k n", p=128)), [wqb], [wqsb], q="pool")
    wk, wksb = C.sb([128, 8, 384], BF16, "wk")
    S.dma(lambda e: e.dma_start(out=wk[:], in_=wk_d.rearrange("(k p) n -> p k n", p=128)), [wkb], [wksb], q="pool")
    wv, wvsb = C.sb([128, 8, 128], BF16, "wv")
    S.dma(lambda e: e.dma_start(out=wv[:], in_=wv_d.rearrange("(k p) n -> p k n", p=128)), [wvb], [wvsb], q="pool")
    wgt, wgtsb = C.sb([128, 8, 12], BF16, "wgt")
    S.dma(lambda e: e.dma_start(out=wgt[:], in_=wgt_d.rearrange("(k p) n -> p k n", p=128)), [wgtb], [wgtsb], q="pool")
    cwk, cwksb = C.sb([64, 2, 32, 64], BF16, "cwk")
    S.dma(lambda e: e.dma_start(out=cwk[:], in_=cwk_d.rearrange("a d l e -> d a l e")), [cwkb], [cwksb], q="pool")
    cwv, cwvsb = C.sb([128, 32, 64], BF16, "cwv")
    S.dma(lambda e: e.dma_start(out=cwv[:], in_=cwv_d), [cwvb], [cwvsb], q="pool")
    pek, peksb = C.sb([64, 32], BF16, "pek")
    S.dma(lambda e: e.dma_start(out=pek[:], in_=pek_d), [pekb], [peksb], q="pool")
    pev, pevsb = C.sb([128, 32], BF16, "pev")
    S.dma(lambda e: e.dma_start(out=pev[:], in_=pev_d), [pevb], [pevsb], q="pool")
    ropek, ropeksb = C.sb([64, 2, 512], F32, "ropek")
    S.dma(lambda e: e.dma_start(out=ropek[:], in_=ropek_d.rearrange("a d n -> d a n")), [ropekb], [ropeksb])
    Ex, Exb = C.sb([128, 32, 128], BF16, "Ex")
    S.dma(lambda e: e.dma_start(out=Ex[:], in_=E_d), [Eb], [Exb], q="pool")
    mk, mksb = C.sb([128, 8, 512], BF16, "mk")
    S.dma(lambda e: e.dma_start(out=mk[:], in_=mk_d.rearrange("a p q -> p a q")), [mkb], [mksb], q="pool")
    cmk, cmksb = C.sb([128, 17, 128], BF16, "cmk")
    S.dma(lambda e: e.dma_start(out=cmk[:], in_=cmk_d.rearrange("a p q -> p a q")), [cmkb], [cmksb], q="pool")
    Wt, Wsb = C.sb([128, 256], F32, "Wt")
    S.dma(lambda e: e.dma_start(out=Wt[:], in_=W_d), [Wb], [Wsb])

    kT2, kT2b = C.sb([128, SEQ], BF16, "kT2")
    kvcin, kvcinb = C.sb([128, SEQ], BF16, "kvcin")
    vslc, vslcb = C.sb([128, 64, 65], BF16, "vslc")
    vwin, vwinb = C.sb([128, 64, 65], BF16, "vwin")
    S.pool(lambda e: e.memset(vslc[:].rearrange("p a b -> p (a b)"), 1.0), [], [vslcb])
    S.pool(lambda e: e.memset(vwin[:].rearrange("p a b -> p (a b)"), 1.0), [], [vwinb])
    kcT, kcTb = C.sb([64, 512], F32, "kcT")
    vca, vcab = C.sb([128, 4, 193], F32, "vca")
    S.pool(lambda e: e.memset(vca[:].rearrange("p a b -> p (a b)"), 0.0), [], [vcab])
    S.pool(lambda e: e.memset(vca[:, :, 64:65], 1.0), [vcab], [vcab])
    S.dma(lambda e: e.dma_start(out=vca[:, :, 65:193], in_=ov_d.rearrange("(c p) m -> p c m", p=128)), [ovb, vcab], [vcab])

    xtile = [C.sb([128, D], F32, "xtile%d" % i) for i in range(2)]
    xT, xTb = C.sb([128, 8, 512], BF16, "xT")
    rp, rpb = C.sb([128, 2, 512], F32, "rp")
    t1, t1b = C.sb([128, 512], F32, "t1")
    t2, t2b = C.sb([128, 512], F32, "t2")

    def rope_from(pa, pab, pbk, pbkb, dst, dstb):
        S.dve(lambda e: e.tensor_tensor(out=t1[:], in0=pa, in1=rp[:, 0, :], op=ALU.mult), [pab, rpb], [t1b])
        S.dve(lambda e: e.tensor_tensor(out=t2[:], in0=pbk, in1=rp[:, 1, :], op=ALU.mult), [pbkb, rpb], [t2b])
        S.pool(lambda e: e.tensor_tensor(out=dst, in0=t1[:], in1=t2[:], op=ALU.add), [t1b, t2b], [dstb])

    for blk in range(SEQ // 512):
        t0 = blk * 512
        load_xT_block(C, PS, xb_d, xbb, t0, ident, identb, xtile, xT, xTb)
        S.dma(lambda e, t0=t0: e.dma_start(out=rp[:], in_=rope_d[:, :, t0:t0 + 512].rearrange("a d n -> d a n")), [ropeb], [rpb])
        pk = []
        for c in range(3):
            p, pb = ACC[c]
            for k in range(8):
                S.pe(lambda e, p=p, k=k, c=c: e.matmul(p[:], lhsT=wk[:, k, c * 128:(c + 1) * 128], rhs=xT[:, k, :],
                                                       start=(k == 0), stop=(k == 7)), [wksb, xTb], [pb])
            pk.append((p, pb))
        S.act(lambda e, t0=t0: e.copy(out=kvcin[:, t0:t0 + 512], in_=pk[0][0][:]), [pk[0][1]], [kvcinb])
        rope_from(pk[1][0][:], pk[1][1], pk[2][0][:], pk[2][1], kT2[:, t0:t0 + 512], kT2b)
        for j in range(4):
            p, pb = PS()
            for k in range(8):
                S.pe(lambda e, p=p, k=k, j=j: e.matmul(p[:, 0:128], lhsT=xT[:, k, j * 128:(j + 1) * 128], rhs=wv[:, k, :],
                                                       start=(k == 0), stop=(k == 7)), [xTb, wvsb], [pb])
            tix = blk * 4 + j
            S.act(lambda e, p=p, tix=tix: e.copy(out=vslc[:, tix, 0:64], in_=p[:, 0:64]), [pb], [vslcb, pb])
            S.dve(lambda e, p=p, tix=tix: e.tensor_copy(out=vwin[:, tix, 0:64], in_=p[:, 64:128]), [pb], [vwinb])
    kb_, kbb = C.sb([128, 4], F32, "kbias")
    for a in range(2):
        p, pb = PS()
        for l in range(32):
            S.pe(lambda e, p=p, l=l, a=a: e.matmul(p[0:64, 0:1], lhsT=cwk[:, a, l, :], rhs=pek[:, l:l + 1],
                                                   start=(l == 0), stop=(l == 31)), [cwksb, peksb], [pb])
        S.dve(lambda e, p=p, a=a: e.tensor_copy(out=kb_[0:64, a:a + 1], in_=p[0:64, 0:1]), [pb], [kbb])
    p, pb = PS()
    for l in range(32):
        S.pe(lambda e, p=p, l=l: e.matmul(p[0:64, 0:1], lhsT=cwv[64:128, l, :], rhs=pev[64:128, l:l + 1],
                                          start=(l == 0), stop=(l == 31)), [cwvsb, pevsb], [pb])
    S.dve(lambda e, p=p: e.tensor_copy(out=kb_[0:64, 2:3], in_=p[0:64, 0:1]), [pb], [kbb])
    kc0, kc0b = C.sb([64, 512], F32, "kc0")
    kc1, kc1b = C.sb([64, 512], F32, "kc1")
    S.pool(lambda e: e.memset(kc0[:], 0.0), [], [kc0b])
    S.pool(lambda e: e.memset(kc1[:], 0.0), [], [kc1b])
    for a, (dst, dstb) in enumerate([(kc0, kc0b), (kc1, kc1b)]):
        p, pb = PS()
        for l in range(32):
            S.pe(lambda e, p=p, l=l, a=a: e.matmul(p[0:64, 0:511], lhsT=cwk[:, a, l, :], rhs=kvcin[0:64, l:l + 16 * 510 + 1:16],
                                                   start=(l == 0), stop=(l == 31)), [cwksb, kvcinb], [pb])
        S.dve(lambda e, p=p, dst=dst, a=a: e.tensor_scalar(out=dst[:, 0:511], in0=p[0:64, 0:511], scalar1=kb_[0:64, a:a + 1], scalar2=None, op0=ALU.add),
              [pb, kbb], [dstb])
    S.dve(lambda e: e.tensor_tensor(out=kc0[:], in0=kc0[:], in1=ropek[:, 0, :], op=ALU.mult), [kc0b, ropeksb], [kc0b])
    S.dve(lambda e: e.tensor_tensor(out=kc1[:], in0=kc1[:], in1=ropek[:, 1, :], op=ALU.mult), [kc1b, ropeksb], [kc1b])
    S.dve(lambda e: e.tensor_tensor(out=kcT[:], in0=kc0[:], in1=kc1[:], op=ALU.add), [kc0b, kc1b], [kcTb])
    vbT, vbTb = C.sb([64, 128], F32, "vbT")
    S.pool(lambda e: e.memset(vbT[:], 0.0), [], [vbTb])
    S.dve(lambda e: e.tensor_scalar(out=vbT[:], in0=vbT[:], scalar1=kb_[0:64, 2:3], scalar2=None, op0=ALU.add), [vbTb, kbb], [vbTb])
    vbr, vbrb = C.sb([128, 64], F32, "vbr")
    p, pb = PS()
    S.pe(lambda e, p=p: e.transpose(out=p[:, 0:64], in_=vbT[:], identity=ident[0:64, 0:64]), [vbTb, identb], [pb])
    S.dve(lambda e, p=p: e.tensor_copy(out=vbr[:], in_=p[:, 0:64]), [pb], [vbrb])
    for c in range(4):
        nn = 128 if c < 3 else 127
        p, pb = PS()
        for l in range(32):
            s0 = l + 16 * 128 * c
            S.pe(lambda e, p=p, l=l, s0=s0, nn=nn: e.matmul(p[0:nn, 0:64], lhsT=kvcin[64:128, s0:s0 + 16 * (nn - 1) + 1:16], rhs=cwv[64:128, l, :],
                                                            start=(l == 0), stop=(l == 31)), [cwvsb, kvcinb], [pb])
        S.dve(lambda e, p=p, c=c, nn=nn: e.tensor_tensor(out=vca[0:nn, c, 0:64], in0=p[0:nn, 0:64], in1=vbr[0:nn, :], op=ALU.add), [pb, vbrb, vcab], [vcab])

    qT = [C.sb([128, 512], F32, "qT%d" % h) for h in range(4)]
    qTh = [C.sb([128, 512], BF16, "qTh%d" % h) for h in range(4)]
    gts, gtsb = C.sb([128, 4, 12], F32, "gts")
    PTc = [C.sb([128, 512], F32, "PTc%d" % i) for i in range(4)]
    rz = [C.sb([128, 1], F32, "rz%d" % i) for i in range(4)]
    oc, ocb = C.sb([128, 4, 4, 64], F32, "oc")
    imps = [C.sb([128, 128], F32, "imp%d" % i) for i in range(2)]
    sc2, sc2b = C.sb([128, 128], F32, "sc2")
    m8a, m8ab = C.sb([128, 8], F32, "m8a")
    m8b, m8bb = C.sb([128, 8], F32, "m8b")
    sel, selb = C.sb([128, 128], F32, "sel")
    selT, selTb = C.sb([128, 512], BF16, "selT")
    maskT = [C.sb([128, 512], BF16, "maskT%d" % i) for i in range(3)]
    PT = [C.sb([128, 512], BF16, "PT%d" % i) for i in range(6)]
    osT = [C.sb([65, 512], F32, "osT%d" % i) for i in range(2)]
    fs = [C.sb([128, 1], F32, "fs%d" % i) for i in range(2)]
    oa, oab_ = C.sb([128, 4, 256], F32, "oa")
    ctr = {"pt": 0, "m": 0, "o": 0}
    SCALE = 0.125

    for qblk in range(nqblk):
        t0 = qblk * 512
        load_xT_block(C, PS, xb_d, xbb, t0, ident, identb, xtile, xT, xTb)
        S.dma(lambda e, t0=t0: e.dma_start(out=rp[:], in_=rope_d[:, :, t0:t0 + 512].rearrange("a d n -> d a n")), [ropeb], [rpb])
        for h in range(4):
            pa, pab = PS()
            pb_, pbb = PS()
            for k in range(8):
                S.pe(lambda e, pa=pa, k=k, h=h: e.matmul(pa[:], lhsT=wq[:, k, h * 256:h * 256 + 128], rhs=xT[:, k, :],
                                                         start=(k == 0), stop=(k == 7)), [wqsb, xTb], [pab])
            for k in range(8):
                S.pe(lambda e, pb_=pb_, k=k, h=h: e.matmul(pb_[:], lhsT=wq[:, k, h * 256 + 128:h * 256 + 256], rhs=xT[:, k, :],
                                                           start=(k == 0), stop=(k == 7)), [wqsb, xTb], [pbb])
            rope_from(pa[:], pab, pb_[:], pbb, qT[h][0][:], qT[h][1])
            S.act(lambda e, h=h: e.copy(out=qTh[h][0][:], in_=qT[h][0][:]), [qT[h][1]], [qTh[h][1]])
        for j in range(4):
            p, pb = PS()
            for k in range(8):
                S.pe(lambda e, p=p, k=k, j=j: e.matmul(p[:, 0:12], lhsT=xT[:, k, j * 128:(j + 1) * 128], rhs=wgt[:, k, :],
                                                       start=(k == 0), stop=(k == 7)), [xTb, wgtsb], [pb])
            S.act(lambda e, p=p, j=j: e.activation(out=gts[:, j, :], in_=p[:, 0:12], func=AF.Sigmoid), [pb], [gtsb])
        csteps = [(j, h) for j in range(4) for h in range(4)]
        cinfo = {}

        def chunks_of(qb):
            out = []
            for c in range(4):
                delta = 128 * c - 8 * qb
                if delta >= 7:
                    continue
                out.append((c, None if delta <= -129 else (delta + 128) // 8))
            return out

        def cstageA(j, h):
            qb = qblk * 4 + j
            chunks = chunks_of(qb)
            i = ctr["pt"]
            ctr["pt"] += 1
            ptc, ptcb = PTc[i % len(PTc)]
            rz_, rzb = rz[i % len(rz)]
            p, pb = PS()
            for (c, mi) in chunks:
                S.pe(lambda e, p=p, c=c, h=h, j=j: e.matmul(p[:, c * 128:(c + 1) * 128], lhsT=kcT[:, c * 128:(c + 1) * 128],
                                                            rhs=qT[h][0][0:64, j * 128:(j + 1) * 128], start=True, stop=True),
                     [kcTb, qT[h][1]], [pb])
            nc_ = len(chunks) * 128
            S.act(lambda e, p=p, ptc=ptc, nc_=nc_: e.activation(out=ptc[:, 0:nc_], in_=p[:, 0:nc_], func=AF.Exp, scale=SCALE), [pb], [ptcb])
            for (c, mi) in chunks:
                if mi is not None:
                    S.dve(lambda e, ptc=ptc, c=c, mi=mi: e.tensor_tensor(out=ptc[:, c * 128:(c + 1) * 128], in0=ptc[:, c * 128:(c + 1) * 128],
                                                                         in1=cmk[:, mi, :], op=ALU.mult), [ptcb, cmksb], [ptcb])
            cinfo[(j, h)] = (chunks, ptc, ptcb, rz_, rzb)

        def cstageB(j, h):
            qb = qblk * 4 + j
            chunks, ptc, ptcb, rz_, rzb = cinfo[(j, h)]
            po, pob = PS()
            for ci, (c, mi) in enumerate(chunks):
                S.pe(lambda e, po=po, ptc=ptc, c=c, ci=ci, n=len(chunks): e.matmul(po[:, 0:193], lhsT=ptc[:, c * 128:(c + 1) * 128], rhs=vca[:, c, :],
                                                                                 start=(ci == 0), stop=(ci == n - 1)), [ptcb, vcab], [pob])
            S.dve(lambda e, po=po, rz_=rz_: e.tensor_scalar(out=rz_[:], in0=po[:, 64:65], scalar1=1e-30, scalar2=None, op0=ALU.max), [pob], [rzb])
            S.dve(lambda e, rz_=rz_: e.reciprocal(out=rz_[:], in_=rz_[:]), [rzb], [rzb])
            S.dve(lambda e, po=po, rz_=rz_, j=j, h=h: e.tensor_scalar(out=oc[:, j, h, :], in0=po[:, 0:64], scalar1=rz_[:, 0:1], scalar2=None,
                                                                    op0=ALU.mult), [pob, rzb], [ocb])
            im_, imb_ = imps[j % 2]
            if h == 0:
                S.dve(lambda e, po=po, rz_=rz_, im_=im_: e.tensor_scalar(out=im_[:], in0=po[:, 65:193], scalar1=rz_[:, 0:1], scalar2=None, op0=ALU.mult),
                      [pob, rzb], [imb_])
            else:
                S.dve(lambda e, po=po, rz_=rz_, im_=im_: e.scalar_tensor_tensor(out=im_[:], in0=po[:, 65:193], scalar=rz_[:, 0:1], in1=im_[:],
                                                                                op0=ALU.mult, op1=ALU.add), [pob, rzb, imb_], [imb_])
            if h == 3:
                S.dve(lambda e, qb=qb, im_=im_: e.tensor_tensor(out=im_[:], in0=im_[:], in1=Wt[:, 128 - 2 * qb:256 - 2 * qb], op=ALU.max), [imb_, Wsb], [imb_])
                S.pool(lambda e, im_=im_: e.memset(im_[:, 0:1], 1e6), [imb_], [imb_])
                S.dve(lambda e, im_=im_: e.max(out=m8a[:], in_=im_[:]), [imb_], [m8ab])
                S.dve(lambda e, im_=im_: e.match_replace(out=sc2[:], in_to_replace=m8a[:], in_values=im_[:], imm_value=-2.0), [m8ab, imb_], [sc2b])
                S.dve(lambda e: e.max(out=m8b[:], in_=sc2[:]), [sc2b], [m8bb])
                S.dve(lambda e, im_=im_: e.tensor_scalar(out=sel[:], in0=im_[:], scalar1=m8b[:, 7:8], scalar2=None, op0=ALU.is_ge), [imb_, m8bb], [selb])
                pst, pstb = PS()
                S.pe(lambda e, pst=pst: e.transpose(out=pst[:, 0:128], in_=sel[:], identity=ident[:]), [selb, identb], [pstb])
                S.act(lambda e, pst=pst, j=j: e.copy(out=selT[:, j * 128:(j + 1) * 128], in_=pst[:, 0:128]), [pstb], [selTb])

        LC = 2
        for i in range(len(csteps) + LC):
            if i < len(csteps):
                cstageA(*csteps[i])
            if i >= LC:
                cstageB(*csteps[i - LC])

        for br in range(2):
            if br == 0:
                kcs = list(range(0, 4 * qblk + 4))
                VV, VVb = vslc, vslcb
                r0 = 0
            else:
                kcs = [kc for kc in range(4 * qblk - 4, 4 * qblk + 4) if kc >= 0]
                VV, VVb = vwin, vwinb
                r0 = 64
            pend = []
            LS = 3

            def flush_one():
                (h, kc, pt_, ptb_, ki, n) = pend.pop(0)
                S.pe(lambda e, h=h, kc=kc, pt_=pt_, VV=VV, ki=ki, n=n: e.matmul(ACC[h][0][0:65, :], lhsT=VV[:, kc, :], rhs=pt_[:],
                                                                              start=(ki == 0), stop=(ki == n - 1)), [VVb, ptb_], [ACC[h][1]])
            for ki, kc in enumerate(kcs):
                dk = kc - 4 * qblk
                if br == 0:
                    mt, mtb = maskT[ctr["m"] % len(maskT)]
                    ctr["m"] += 1
                    pm, pmb = PS()
                    base = 0 if (2 * kc) < 64 else 64
                    v = kc % 32
                    S.pe(lambda e, pm=pm, base=base, v=v: e.matmul(pm[:], lhsT=Ex[base:base + 64, v, :], rhs=selT[base:base + 64, :], start=True, stop=True),
                         [Exb, selTb], [pmb])
                    if dk >= 0:
                        S.dve(lambda e, pm=pm, mt=mt, dk=dk: e.tensor_tensor(out=mt[:], in0=pm[:], in1=mk[:, 4 + dk, :], op=ALU.mult), [pmb, mksb], [mtb])
                    else:
                        S.dve(lambda e, pm=pm, mt=mt: e.tensor_copy(out=mt[:], in_=pm[:]), [pmb], [mtb])
                    mask_ap, mask_b = mt[:], mtb
                else:
                    mask_ap, mask_b = mk[:, dk + 4, :], mksb
                for h in range(4):
                    pt_, ptb_ = PT[ctr["pt"] % len(PT)]
                    ctr["pt"] += 1
                    ps_, psb_ = PS()
                    S.pe(lambda e, ps_=ps_, kc=kc, h=h, r0=r0: e.matmul(ps_[:], lhsT=kT2[r0:r0 + 64, kc * 128:(kc + 1) * 128], rhs=qTh[h][0][r0:r0 + 64, :],
                                                                       start=True, stop=True), [kT2b, qTh[h][1]], [psb_])
                    S.act(lambda e, ps_=ps_, pt_=pt_: e.activation(out=pt_[:], in_=ps_[:], func=AF.Exp, scale=SCALE), [psb_], [ptb_])
                    S.dve(lambda e, pt_=pt_, mask_ap=mask_ap: e.tensor_tensor(out=pt_[:], in0=pt_[:], in1=mask_ap, op=ALU.mult), [ptb_, mask_b], [ptb_])
                    pend.append((h, kc, pt_, ptb_, ki, len(kcs)))
                    if len(pend) > LS:
                        flush_one()
            while pend:
                flush_one()
            for h in range(4):
                ot_, otb_ = osT[h % 2]
                S.act(lambda e, h=h, ot_=ot_: e.copy(out=ot_[:], in_=ACC[h][0][0:65, :]), [ACC[h][1]], [otb_])
                for j in range(4):
                    i = ctr["o"]
                    ctr["o"] += 1
                    fs_, fsb = fs[i % 2]
                    p, pb = PS()
                    S.pe(lambda e, p=p, ot_=ot_, j=j: e.transpose(out=p[:, 0:65], in_=ot_[:, j * 128:(j + 1) * 128], identity=ident[0:65, 0:65]),
                         [otb_, identb], [pb])
                    S.dve(lambda e, p=p, fs_=fs_: e.tensor_scalar(out=fs_[:], in0=p[:, 64:65], scalar1=1e-30, scalar2=None, op0=ALU.max), [pb], [fsb])
                    S.dve(lambda e, fs_=fs_: e.reciprocal(out=fs_[:], in_=fs_[:]), [fsb], [fsb])
                    gi = h * 3 + 1 + br
                    S.dve(lambda e, fs_=fs_, j=j, gi=gi: e.tensor_tensor(out=fs_[:], in0=fs_[:], in1=gts[:, j, gi:gi + 1], op=ALU.mult), [fsb, gtsb], [fsb])
                    dsl = oa[:, j, h * 64:(h + 1) * 64]
                    if br == 0:
                        S.pool(lambda e, j=j, h=h, dsl=dsl: e.tensor_scalar(out=dsl, in0=oc[:, j, h, :], scalar1=gts[:, j, h * 3:h * 3 + 1], scalar2=None,
                                                                           op0=ALU.mult), [ocb, gtsb, oab_], [oab_])
                    S.dve(lambda e, p=p, fs_=fs_, dsl=dsl: e.scalar_tensor_tensor(out=dsl, in0=p[:, 0:64], scalar=fs_[:, 0:1], in1=dsl,
                                                                                 op0=ALU.mult, op1=ALU.add), [pb, fsb, oab_], [oab_])
        for j in range(4):
            S.dma(lambda e, j=j, t0=t0: e.dma_start(out=oa_d[t0 + j * 128:t0 + (j + 1) * 128, ocol:ocol + 256], in_=oa[:, j, :]), [oab_], [oab])
    S.emit(stack)


def nsa_consts():
    c = {}
    half = 32
    inv = np.power(10000.0, -np.arange(half, dtype=np.float32) / half).astype(np.float32)
    pos = np.arange(SEQ, dtype=np.float32)
    ang = pos[None, :] * inv[:, None]
    cos, sin = np.cos(ang).astype(np.float32), np.sin(ang).astype(np.float32)
    cosC = np.concatenate([cos, cos], 0)
    sinS = np.concatenate([-sin, sin], 0)
    c["rope"] = np.stack([np.concatenate([cosC, cosC], 0), np.concatenate([sinS, sinS], 0)]).astype(np.float32)
    posk = (np.arange(512, dtype=np.float32) * 16 + 15.5)
    angk = posk[None, :] * inv[:, None]
    ck, sk = np.cos(angk).astype(np.float32), np.sin(angk).astype(np.float32)
    c["ropek"] = np.stack([np.concatenate([ck, ck], 0), np.concatenate([-sk, sk], 0)]).astype(np.float32)
    p = np.arange(128)
    E = np.zeros((128, 32, 128), np.float32)
    for v in range(32):
        for k in range(128):
            r = 2 * v + k // 64
            if r < 64:
                E[r, v, k] = 1.0
                E[64 + r, v, k] = 1.0
    c["Eexp"] = E
    q = np.arange(512)
    mk = np.zeros((8, 128, 512), np.float32)
    for i in range(8):
        kp = 128 * (i - 4) + p[:, None]
        rel = q[None, :] - kp
        mk[i] = ((rel >= 0) & (rel < 512)).astype(np.float32)
    c["masks"] = mk
    ql = np.arange(128)
    cm = np.zeros((17, 128, 128), np.float32)
    for mi in range(17):
        delta = mi * 8 - 128
        npr = p[:, None] + delta
        cm[mi] = (16 * npr + 31 <= ql[None, :]).astype(np.float32)
    c["cmask"] = cm
    n = np.arange(512)
    m = np.arange(128)
    ov = ((16 * n[:, None] <= 64 * m[None, :] + 63) & (16 * n[:, None] + 31 >= 64 * m[None, :])).astype(np.float32)
    ov[511] = 0.0
    c["overlap"] = ov
    W = np.full((128, 256), -1.0, np.float32)
    for qq in range(128):
        rels = (0, -1) if qq < 64 else (0, 1)
        for r in rels:
            W[qq, 128 + r] = 1e6
    c["forceW"] = W
    c["ident"] = np.eye(128, dtype=np.float32)
    return c


def nsa_inputs(inp, b, hkv, consts):
    w_in = inp["ev_w_in"][0]
    sw = lambda a: np.concatenate([a[..., 32:], a[..., :32]], -1)
    cols = []
    for g in range(4):
        h = hkv * 4 + g
        qh = w_in[:, h * 64:(h + 1) * 64]
        cols += [qh, qh, sw(qh), sw(qh)]
    wq = np.concatenate(cols, 1)
    kv = lambda i: w_in[:, 512 + i * 128 + hkv * 64: 512 + i * 128 + (hkv + 1) * 64]
    wk = np.concatenate([kv(0), kv(1), kv(2), kv(4), sw(kv(2)), sw(kv(4))], 1)
    wv = np.concatenate([kv(3), kv(5)], 1)
    og = 512 + 6 * 128
    wgt = w_in[:, og + hkv * 12: og + (hkv + 1) * 12]
    wkc = inp["ev_cmp_w_k"][0]
    wvc = inp["ev_cmp_w_v"][0]
    cmpwk = np.stack([wkc.transpose(1, 0, 2), sw(wkc).transpose(1, 0, 2)])
    cmpwv = np.concatenate([np.zeros((64, 32, 64), np.float32), wvc.transpose(1, 0, 2)], 0)
    pek = inp["ev_cmp_pe_k"][0].T
    pev = np.concatenate([np.zeros((64, 32), np.float32), inp["ev_cmp_pe_v"][0].T], 0)
    d = {"xb": np.ascontiguousarray(inp["x"][b]), "wq": np.ascontiguousarray(wq), "wk": np.ascontiguousarray(wk),
         "wv": np.ascontiguousarray(wv), "wgt": np.ascontiguousarray(wgt), "cmpwk": np.ascontiguousarray(cmpwk),
         "cmpwv": np.ascontiguousarray(cmpwv), "cmppek": np.ascontiguousarray(pek), "cmppev": np.ascontiguousarray(pev)}
    d.update(consts)
    return d


NSA_SHARED = {"rope": [2, 128, SEQ], "ropek": [2, 64, 512], "Eexp": [128, 32, 128], "masks": [8, 128, 512],
              "cmask": [17, 128, 128], "overlap": [512, 128], "forceW": [128, 256], "ident": [128, 128]}
I32 = mybir.dt.int32


def build_fused(nc, stack):
    from contextlib import ExitStack
    shared = {"stack": stack}
    xpad = nc.dram_tensor("xpad", [SEQ + 128, D], F32, kind="ExternalInput").ap()
    nonce = nc.dram_tensor("nonce", [1, 16], I32, kind="ExternalInput").ap()
    mixh = nc.dram_tensor("mixh", [2, SEQ + 128, 256], F32, kind="Internal").ap()
    shmix = nc.dram_tensor("shmix", [2, 2, SEQ + 128, 256], F32, kind="Internal", addr_space="Shared").ap()
    flag = nc.dram_tensor("shflag", [2, 16], I32, kind="Internal", addr_space="Shared").ap()
    io = {"xb": xpad[128:SEQ + 128, :], "gcst": nc.dram_tensor("gcst", [6, 128, 128], F32, kind="ExternalInput").ap()}
    for k, shp in NSA_SHARED.items():
        io[k] = nc.dram_tensor(k, shp, F32, kind="ExternalInput").ap()
    with ExitStack() as st:
        d = dict(io)
        d["o_a"] = mixh[0, 128:SEQ + 128, :]
        d["zero_rows"] = [(mixh[0, 0:128, :], xpad[0:128, 0:256]), (mixh[1, 0:128, :], xpad[0:128, 0:256])]
        build_nsa(nc, st, shared=shared, pfx="n_", io=d, ocol=0)
    with ExitStack() as st:
        d = {"xb": io["xb"], "gcst": io["gcst"], "o_b": mixh[1, 128:SEQ + 128, :]}
        build_gdn(nc, st, shared=shared, pfx="g_", io=d, ocol=0, NH=2)
    with ExitStack() as st:
        C = Ctx(nc, st, shared, "x_")
        S = C.S
        mb, sb_, fb, nb = Buf("mixh"), Buf("shmix"), Buf("flag"), Buf("nonce")
        S.dma(lambda e: e.dma_start(out=shmix[bass.ds(nc.partition_id() % 2, 1), :, :, :], in_=mixh), [mb], [sb_])
        S.dma(lambda e: e.dma_start(out=flag[bass.ds(nc.partition_id() % 2, 1), :], in_=nonce), [nb, sb_], [fb])

        def poll(e):
            with e.register("pf") as f, e.register("pn") as n, e.register("pd") as dd:
                e.reg_load(n, nonce[0:1, 0:1])
                other = flag[bass.ds(1 - nc.partition_id() % 2, 1), 0:1]
                e.reg_load(f, other)
                e.reg_sub(dd, f, n)
                with e.While(dd):
                    e.reg_load(f, other)
                    e.reg_sub(dd, f, n)
            return e.nop()
        S.add("sp", poll, [fb], [fb, sb_])
        S.emit(st)
    with ExitStack() as st:
        d = {"xpad": xpad, "shmix": shmix, "ident": io["ident"]}
        build_tail(nc, st, shared=shared, pfx="t_", io=d, dyn=True)


_PROGS = {}


def _prog(name, builder):
    if name not in _PROGS:
        from contextlib import ExitStack
        nc = bass.Bass("TRN2", target_bir_lowering=False)
        with ExitStack() as st:
            builder(nc, st)
        _PROGS[name] = nc
    return _PROGS[name]


def fused_inputs(inp, c, cs, A, lnp, nonce):
    b, r = c // 2, c % 2
    d = {"xpad": np.concatenate([np.zeros((128, D), np.float32), inp["x"][b]]), "gcst": gdn_consts(),
         "nonce": np.full((1, 16), nonce, np.int32)}
    d.update(cs)
    for k, v in nsa_inputs(inp, b, r, {}).items():
        if k != "xb":
            d["n_" + k] = v
    for k, v in gdn_inputs(inp, b, r).items():
        if k not in ("xb", "gcst"):
            d["g_" + k] = v
    Ac = A
    if r == 1:
        Ac = A.copy()
        Ac[0] = A[2]
        Ac[1] = A[3]
    t = {"w_out": inp["ev_w_out"][0], "lnp": lnp, "ffn_wg": inp["ev_ffn_wg"][0], "ffn_wu": inp["ev_ffn_wu"][0],
         "ffn_wd": inp["ev_ffn_wd"][0], "pool_w": inp["od_pool_w"][0], "router_w": inp["od_router_w"][0],
         "exp_wg": inp["od_exp_wg"][0], "exp_wu": inp["od_exp_wu"][0], "exp_wd": inp["od_exp_wd"][0], "apool": Ac}
    for k, v in t.items():
        d["t_" + k] = v
    return d


_CALLS = [0]


def kernel(**inp):
    inp = {k: np.asarray(v) for k, v in inp.items()}
    B = inp["x"].shape[0]
    cores = list(range(NCORES))
    cs = nsa_consts()
    A = pool_consts()
    lnp = np.stack([inp[k][0] for k in ["ev_ln1_g", "ev_ln1_b", "ev_ln2_g", "ev_ln2_b", "od_pool_scale",
                                        "od_ln1_g", "od_ln1_b", "od_ln2_g", "od_ln2_b"]])
    _CALLS[0] += 1
    nonce = (int.from_bytes(os.urandom(3), "little") << 4) + (_CALLS[0] % 16) + 1
    ims = [fused_inputs(inp, c, cs, A, lnp, nonce) for c in cores]
    res = run_bass_kernel_spmd(_prog("fused", build_fused), ims, core_ids=cores)
    out = np.stack([np.concatenate([res.results[2 * b]["t_out"], res.results[2 * b + 1]["t_out"]]) for b in range(B)])
    return out.astype(np.float32)
```
